# Optimizing a Trainium2 kernel written in Bass

```python
import jax
import jax.numpy as jnp
from jax import lax
import numpy as np

D_MODEL = 1024
BATCH = 4
SEQ = 4096
DEPTH = 4

D_MIX = D_MODEL
CONV_CH = D_MIX // 4
CONV_K = 31
CONV_IN = 2 * CONV_CH
MLA_NOPE = 64
MLA_ROPE = 32
MLA_V = 64
MLA_QK = MLA_NOPE + MLA_ROPE
MLA_OUT = D_MIX // 2
MLA_HEADS = MLA_OUT // MLA_V
MLA_Q_LORA = D_MODEL // 4
MLA_KV_LORA = D_MODEL // 8
MLA_IN = MLA_Q_LORA + MLA_KV_LORA + MLA_ROPE
ROPE_THETA = 10000.0
Q_BLOCK = 128
RWKV_HEAD = 64
RWKV_CH = D_MIX - CONV_CH - MLA_OUT
RWKV_HEADS = RWKV_CH // RWKV_HEAD
DECAY_LORA = 64
ICLR_LORA = 64
GATE_LORA = 128
RWKV_IN = 3 * RWKV_CH + DECAY_LORA + ICLR_LORA + GATE_LORA
IN_COLS = CONV_IN + MLA_IN + RWKV_IN
D_FF = ((8 * D_MODEL // 3 + 127) // 128) * 128
N_EXPERTS = 8
TOP_K = 2
MOE_FF = 7 * D_MODEL // 2
MOE_BLOCK = 256
N_DENSE = (DEPTH + 1) // 2
N_MOE = DEPTH // 2
N_MOD = 6
RMS_EPS = 1e-6
LN_EPS = 1e-5
RWKV_GN_EPS = 64e-5

kernel_name = 'hybrid_parallel_group_block'


def _rms_norm(x, gain):
    xf = x.astype(jnp.float32)
    y = xf * lax.rsqrt(jnp.mean(xf * xf, axis=-1, keepdims=True) + RMS_EPS)
    return (y * gain.astype(jnp.float32)).astype(x.dtype)


def _layer_norm(x, gain, bias, eps):
    xf = x.astype(jnp.float32)
    xc = xf - jnp.mean(xf, axis=-1, keepdims=True)
    var = jnp.mean(xc * xc, axis=-1, keepdims=True)
    y = xc * lax.rsqrt(var + eps) * gain.astype(jnp.float32) + bias.astype(jnp.float32)
    return y.astype(x.dtype)


def _rope(x, cos, sin):
    xf = x.astype(jnp.float32)
    x1, x2 = jnp.split(xf, 2, axis=-1)
    y = jnp.concatenate([x1 * cos - x2 * sin, x1 * sin + x2 * cos], axis=-1)
    return y.astype(x.dtype)


def _conformer_conv(u, conv_w, conv_b, ln_g, ln_b):
    a, g = jnp.split(u, 2, axis=-1)
    z = a * jax.nn.sigmoid(g)
    z = lax.conv_general_dilated(z, conv_w[:, None, :].astype(z.dtype), window_strides=(1,), padding=[(CONV_K - 1, 0)], dimension_numbers=('NWC', 'WIO', 'NWC'), feature_group_count=CONV_CH)
    z = z + conv_b
    return jax.nn.silu(_layer_norm(z, ln_g, ln_b, LN_EPS))


def _causal_attention(q, k, v):
    b, t, h, dq = q.shape
    nb = t // Q_BLOCK
    scale = float(dq) ** -0.5
    q_blocks = q.reshape(b, nb, Q_BLOCK, h, dq).swapaxes(0, 1)
    k_pos = jnp.arange(t)

    def block(args):
        q_blk, start = args
        s = jnp.einsum('bqhd,bkhd->bhqk', q_blk, k, preferred_element_type=jnp.float32) * scale
        q_pos = start + jnp.arange(Q_BLOCK)
        s = jnp.where(k_pos[None, :] <= q_pos[:, None], s, -jnp.inf)
        p = jax.nn.softmax(s, axis=-1).astype(v.dtype)
        return jnp.einsum('bhqk,bkhd->bqhd', p, v)

    o = lax.map(block, (q_blocks, jnp.arange(nb) * Q_BLOCK))
    return o.swapaxes(0, 1).reshape(b, t, h, v.shape[-1])


def _mla(u, cos, sin, q_norm_g, w_uq, kv_norm_g, w_ukv, qk_q_g, qk_k_g):
    b, t, _ = u.shape
    c_q, c_kv, k_rope = jnp.split(u, [MLA_Q_LORA, MLA_Q_LORA + MLA_KV_LORA], axis=-1)
    q = (_rms_norm(c_q, q_norm_g) @ w_uq).reshape(b, t, MLA_HEADS, MLA_QK)
    kv = (_rms_norm(c_kv, kv_norm_g) @ w_ukv).reshape(b, t, MLA_HEADS, MLA_NOPE + MLA_V)
    k_nope, v = jnp.split(kv, [MLA_NOPE], axis=-1)
    k_rope = jnp.broadcast_to(k_rope[:, :, None, :], (b, t, MLA_HEADS, MLA_ROPE))
    k = jnp.concatenate([k_nope, k_rope], axis=-1)
    q = _rms_norm(q, qk_q_g)
    k = _rms_norm(k, qk_k_g)
    q = jnp.concatenate([q[..., :MLA_NOPE], _rope(q[..., MLA_NOPE:], cos, sin)], axis=-1)
    k = jnp.concatenate([k[..., :MLA_NOPE], _rope(k[..., MLA_NOPE:], cos, sin)], axis=-1)
    o = _causal_attention(q, k, v)
    return o.reshape(b, t, MLA_OUT)


def _wkv7_scan(r, w, k, v, a, b):
    bsz = r.shape[0]

    def step(s, inp):
        r_t, w_t, k_t, v_t, a_t, b_t = inp
        sa = jnp.einsum('bhij,bhj->bhi', s, a_t)
        s = s * w_t[:, :, None, :] + sa[..., None] * b_t[:, :, None, :] + v_t[..., None] * k_t[:, :, None, :]
        return s, jnp.einsum('bhij,bhj->bhi', s, r_t)

    s0 = jnp.zeros((bsz, RWKV_HEADS, RWKV_HEAD, RWKV_HEAD), jnp.float32)
    xs = [z.swapaxes(0, 1) for z in (r, w, k, v, a, b)]
    _, y = lax.scan(step, s0, xs)
    return y.swapaxes(0, 1)


def _rwkv7_mix(u, mu, w0, w_up, a0, a_up, g_up, k_k, k_a, r_k, ln_g, ln_b):
    dt = u.dtype
    bsz, t, _ = u.shape
    f32 = jnp.float32
    uf = u.astype(f32)
    prev = jnp.pad(uf, ((0, 0), (1, 0), (0, 0)))[:, :-1]
    uf = uf + (prev - uf) * mu.astype(f32)
    s1 = RWKV_CH
    s3 = 3 * RWKV_CH
    r, k, v, wd, ad, gd = jnp.split(uf, [s1, 2 * s1, s3, s3 + DECAY_LORA, s3 + DECAY_LORA + ICLR_LORA], axis=-1)
    w_log = -jax.nn.softplus(-(w0.astype(f32) + jnp.tanh(wd) @ w_up.astype(f32))) - 0.5
    decay = jnp.exp(-jnp.exp(w_log))
    a = jax.nn.sigmoid(a0.astype(f32) + ad @ a_up.astype(f32))
    g = jax.nn.sigmoid(gd) @ g_up.astype(f32)
    hs = (bsz, t, RWKV_HEADS, RWKV_HEAD)
    kk = (k * k_k.astype(f32)).reshape(hs)
    kk = kk / jnp.maximum(jnp.linalg.norm(kk, axis=-1, keepdims=True), 1e-12)
    k = k * (1.0 + (a - 1.0) * k_a.astype(f32))
    r, k, v, decay, a = [z.reshape(hs) for z in (r, k, v, decay, a)]
    y = _wkv7_scan(r, decay, k, v, -kk, kk * a)
    y = _layer_norm(y, ln_g.reshape(RWKV_HEADS, RWKV_HEAD), ln_b.reshape(RWKV_HEADS, RWKV_HEAD), RWKV_GN_EPS)
    y = y + jnp.sum(r * k * r_k.astype(f32), axis=-1, keepdims=True) * v
    return (y.reshape(bsz, t, RWKV_CH) * g).astype(dt)


def _swiglu(h, w_gate, w_up, w_down):
    return (jax.nn.silu(h @ w_gate) * (h @ w_up)) @ w_down


def _moe_swiglu(h, router, w_gate, w_up, w_down):
    b, t, d = h.shape
    xf = h.reshape(-1, d)
    n_tok = xf.shape[0]
    probs = jax.nn.softmax((xf @ router).astype(jnp.float32), axis=-1)
    top_p, top_e = lax.top_k(probs, TOP_K)
    top_p = top_p / jnp.sum(top_p, axis=-1, keepdims=True)
    flat_e = top_e.reshape(-1)
    flat_p = top_p.reshape(-1)
    n_assign = n_tok * TOP_K
    order = jnp.argsort(flat_e)
    sorted_e = flat_e[order]
    counts = jnp.bincount(flat_e, length=N_EXPERTS)
    starts = jnp.cumsum(counts) - counts
    padded = (counts + MOE_BLOCK - 1) // MOE_BLOCK * MOE_BLOCK
    pad_ends = jnp.cumsum(padded)
    pad_starts = pad_ends - padded
    dest = pad_starts[sorted_e] + jnp.arange(n_assign) - starts[sorted_e]
    cap = -(-n_assign // MOE_BLOCK) * MOE_BLOCK + N_EXPERTS * MOE_BLOCK
    n_blk = cap // MOE_BLOCK
    slot_tok = jnp.zeros((cap,), jnp.int32).at[dest].set((order // TOP_K).astype(jnp.int32))
    slot_w = jnp.zeros((cap,), jnp.float32).at[dest].set(flat_p[order])
    blk_e = jnp.minimum(jnp.searchsorted(pad_ends, jnp.arange(n_blk) * MOE_BLOCK, side='right'), N_EXPERTS - 1)
    xs = xf[slot_tok].reshape(n_blk, MOE_BLOCK, d)

    def expert_block(args):
        xb, e = args
        return _swiglu(xb, w_gate[e], w_up[e], w_down[e])

    ys = lax.map(expert_block, (xs, blk_e)).reshape(cap, d)
    out = jnp.zeros((n_tok, d), jnp.float32).at[slot_tok].add(ys.astype(jnp.float32) * slot_w[:, None])
    return out.astype(h.dtype).reshape(b, t, d)


def setup_inputs(seed: int = 0) -> dict:
    key = jax.random.key(seed)
    ks = iter(jax.random.split(key, 48))
    L = DEPTH

    def nrm(shape, scale):
        return jax.random.normal(next(ks), shape, jnp.float32) * scale

    def gain(shape):
        return 1.0 + nrm(shape, 0.05)

    x = nrm((BATCH, SEQ, D_MODEL), 1.0)
    c = nrm((BATCH, D_MODEL), 1.0)
    positions = jnp.arange(SEQ, dtype=jnp.int32)[None, :] + jax.random.randint(next(ks), (BATCH, 1), 0, SEQ, dtype=jnp.int32)
    return {
        'x': x,
        'c': c,
        'positions': positions,
        'ada_w': nrm((D_MODEL, N_MOD * D_MODEL), 0.5 * D_MODEL ** -0.5),
        'ada_b': nrm((N_MOD * D_MODEL,), 0.02),
        'ada_layer_bias': nrm((L, N_MOD * D_MODEL), 0.02),
        'norm1_g': gain((L, D_MODEL)),
        'norm2_g': gain((L, D_MODEL)),
        'w_in': nrm((L, D_MODEL, IN_COLS), D_MODEL ** -0.5),
        'w_out': nrm((L, D_MIX, D_MODEL), D_MIX ** -0.5),
        'conv_w': nrm((L, CONV_K, CONV_CH), CONV_K ** -0.5),
        'conv_b': nrm((L, CONV_CH), 0.02),
        'conv_ln_g': gain((L, CONV_CH)),
        'conv_ln_b': nrm((L, CONV_CH), 0.02),
        'mla_q_norm_g': gain((L, MLA_Q_LORA)),
        'mla_w_uq': nrm((L, MLA_Q_LORA, MLA_HEADS * MLA_QK), MLA_Q_LORA ** -0.5),
        'mla_kv_norm_g': gain((L, MLA_KV_LORA)),
        'mla_w_ukv': nrm((L, MLA_KV_LORA, MLA_HEADS * (MLA_NOPE + MLA_V)), MLA_KV_LORA ** -0.5),
        'qk_norm_q': gain((L, MLA_QK)),
        'qk_norm_k': gain((L, MLA_QK)),
        'rwkv_mu': jax.random.uniform(next(ks), (L, RWKV_IN), jnp.float32, 0.0, 1.0),
        'rwkv_w0': nrm((L, RWKV_CH), 0.5) - 0.5,
        'rwkv_w_up': nrm((L, DECAY_LORA, RWKV_CH), 0.1),
        'rwkv_a0': nrm((L, RWKV_CH), 0.5),
        'rwkv_a_up': nrm((L, ICLR_LORA, RWKV_CH), 0.5 * ICLR_LORA ** -0.5),
        'rwkv_g_up': nrm((L, GATE_LORA, RWKV_CH), GATE_LORA ** -0.5),
        'rwkv_k_k': 0.85 + nrm((L, RWKV_CH), 0.05),
        'rwkv_k_a': gain((L, RWKV_CH)),
        'rwkv_r_k': nrm((L, RWKV_HEADS, RWKV_HEAD), 0.1),
        'rwkv_ln_g': gain((L, RWKV_CH)),
        'rwkv_ln_b': nrm((L, RWKV_CH), 0.02),
        'ffn_w_gate': nrm((N_DENSE, D_MODEL, D_FF), D_MODEL ** -0.5),
        'ffn_w_up': nrm((N_DENSE, D_MODEL, D_FF), D_MODEL ** -0.5),
        'ffn_w_down': nrm((N_DENSE, D_FF, D_MODEL), D_FF ** -0.5),
        'moe_router': nrm((N_MOE, D_MODEL, N_EXPERTS), D_MODEL ** -0.5),
        'moe_w_gate': nrm((N_MOE, N_EXPERTS, D_MODEL, MOE_FF), D_MODEL ** -0.5),
        'moe_w_up': nrm((N_MOE, N_EXPERTS, D_MODEL, MOE_FF), D_MODEL ** -0.5),
        'moe_w_down': nrm((N_MOE, N_EXPERTS, MOE_FF, D_MODEL), MOE_FF ** -0.5),
    }


def reference(x, c, positions, ada_w, ada_b, ada_layer_bias, norm1_g, norm2_g, w_in, w_out, conv_w, conv_b, conv_ln_g, conv_ln_b, mla_q_norm_g, mla_w_uq, mla_kv_norm_g, mla_w_ukv, qk_norm_q, qk_norm_k, rwkv_mu, rwkv_w0, rwkv_w_up, rwkv_a0, rwkv_a_up, rwkv_g_up, rwkv_k_k, rwkv_k_a, rwkv_r_k, rwkv_ln_g, rwkv_ln_b, ffn_w_gate, ffn_w_up, ffn_w_down, moe_router, moe_w_gate, moe_w_up, moe_w_down):
    inv_freq = ROPE_THETA ** (-jnp.arange(0, MLA_ROPE, 2, dtype=jnp.float32) / MLA_ROPE)
    ang = positions.astype(jnp.float32)[..., None] * inv_freq
    cos = jnp.cos(ang)[:, :, None, :]
    sin = jnp.sin(ang)[:, :, None, :]
    cond = jax.nn.silu(c) @ ada_w + ada_b
    for layer in range(DEPTH):
        mod = cond + ada_layer_bias[layer]
        sh1, sc1, gt1, sh2, sc2, gt2 = [m[:, None, :] for m in jnp.split(mod, N_MOD, axis=-1)]
        h = _rms_norm(x, norm1_g[layer]) * (1.0 + sc1) + sh1
        u = h @ w_in[layer]
        u_conv, u_mla, u_rwkv = jnp.split(u, [CONV_IN, CONV_IN + MLA_IN], axis=-1)
        y_conv = _conformer_conv(u_conv, conv_w[layer], conv_b[layer], conv_ln_g[layer], conv_ln_b[layer])
        y_mla = _mla(u_mla, cos, sin, mla_q_norm_g[layer], mla_w_uq[layer], mla_kv_norm_g[layer], mla_w_ukv[layer], qk_norm_q[layer], qk_norm_k[layer])
        y_rwkv = _rwkv7_mix(u_rwkv, rwkv_mu[layer], rwkv_w0[layer], rwkv_w_up[layer], rwkv_a0[layer], rwkv_a_up[layer], rwkv_g_up[layer], rwkv_k_k[layer], rwkv_k_a[layer], rwkv_r_k[layer], rwkv_ln_g[layer], rwkv_ln_b[layer])
        mixed = jnp.concatenate([y_conv, y_mla, y_rwkv], axis=-1)
        x = x + gt1 * (mixed @ w_out[layer])
        h = _rms_norm(x, norm2_g[layer]) * (1.0 + sc2) + sh2
        i = layer // 2
        if layer % 2 == 0:
            f = _swiglu(h, ffn_w_gate[i], ffn_w_up[i], ffn_w_down[i])
        else:
            f = _moe_swiglu(h, moe_router[i], moe_w_gate[i], moe_w_up[i], moe_w_down[i])
        x = x + gt2 * f
    return x
```

```python
import os
import contextlib
import numpy as np
import concourse.bass as bass
import concourse.mybir as mybir
from concourse.bass_utils import run_bass_kernel_spmd

F32 = mybir.dt.float32
BF16 = mybir.dt.bfloat16
I32 = mybir.dt.int32
AF = mybir.ActivationFunctionType
ALU = mybir.AluOpType

D = 1024
T = 4096
L = 4
TT = 512
NT = T // TT
DFF = 2816
MFF = 3584
NE = 8
NCH_IN = 16
RMS_EPS = 1e-6
LN_EPS = 1e-5
GN_EPS = 64e-5
SCALE_QK = 96.0 ** -0.5
N_CORES = 4

DBG = os.environ.get("KDBG", "")


class Prog:
    CE = ("vector", "scalar", "gpsimd", "tensor")

    def __init__(self, nc):
        self.nc = nc
        self.eng = {"vector": nc.vector, "scalar": nc.scalar, "gpsimd": nc.gpsimd,
                    "tensor": nc.tensor, "sync": nc.sync}
        self.sem = {e: nc.alloc_semaphore("p_" + e) for e in self.CE}
        self.cnt = {e: 0 for e in self.CE}
        self.dsem = {q: [nc.alloc_semaphore(f"d_{q}{i}") for i in range(8)] for q in ("sync", "gpsimd")}
        self.dcnt = {}
        self.drr = {"sync": 0, "gpsimd": 0}
        self.semobj = {}
        for e in self.CE:
            self.semobj[self.sem[e].name if hasattr(self.sem[e], "name") else e] = self.sem[e]
        self.seen = {e: {} for e in self.eng}
        self.lastw = {}
        self.readers = {}
        self.nops = 0

    def _need(self, eng, deps):
        for (sid, sem, val) in deps:
            if self.seen[eng].get(sid, 0) < val:
                self.eng[eng].wait_ge(sem, val)
                self.seen[eng][sid] = val
                self.nops += 1

    def _collect(self, reads, writes):
        deps = {}
        def add(d):
            if d is None:
                return
            sid, sem, val = d
            if sid not in deps or deps[sid][2] < val:
                deps[sid] = d
        for k in reads:
            add(self.lastw.get(k))
        for k in writes:
            add(self.lastw.get(k))
            for d in self.readers.get(k, {}).values():
                add(d)
        return list(deps.values())

    def _record(self, reads, writes, dep):
        sid = dep[0]
        for k in reads:
            r = self.readers.setdefault(k, {})
            r[sid] = dep
        for k in writes:
            self.lastw[k] = dep
            self.readers[k] = {}

    def op(self, eng, fn, reads=(), writes=()):
        deps = self._collect(reads, writes)
        if eng == "tensor":
            deps = [d for d in deps if d[0] != "tensor"]
        self._need(eng, deps)
        ins = fn(self.eng[eng])
        self.cnt[eng] += 1
        ins.then_inc(self.sem[eng], 1)
        self.nops += 1
        dep = (eng, self.sem[eng], self.cnt[eng])
        self.seen[eng][eng] = max(self.seen[eng].get(eng, 0), 0)
        self._record(reads, writes, dep)
        return dep

    def dma(self, q, out, in_, reads=(), writes=()):
        deps = self._collect(reads, writes)
        i = self.drr[q]
        self.drr[q] = (i + 1) % len(self.dsem[q])
        sem = self.dsem[q][i]
        sid = f"d_{q}{i}"
        n = self.dcnt.get(sid, 0)
        if n > 0:
            deps.append((sid, sem, 16 * n))
        self._need(q, deps)
        self.eng[q].dma_start(out=out, in_=in_).then_inc(sem, 16)
        self.nops += 1
        self.dcnt[sid] = n + 1
        dep = (sid, sem, 16 * (n + 1))
        self._record(reads, writes, dep)
        return dep

    def barrier(self):
        alld = [(e, self.sem[e], self.cnt[e]) for e in self.CE if self.cnt[e] > 0]
        for q in self.dsem:
            for i, sem in enumerate(self.dsem[q]):
                sid = f"d_{q}{i}"
                if self.dcnt.get(sid, 0) > 0:
                    alld.append((sid, sem, 16 * self.dcnt[sid]))
        for e in self.eng:
            self._need(e, [d for d in alld if d[0] != e])
        self.lastw = {}
        self.readers = {}


def V(P, fn, r=(), w=()):
    return P.op("vector", fn, r, w)


def A(P, fn, r=(), w=()):
    return P.op("scalar", fn, r, w)


def G(P, fn, r=(), w=()):
    return P.op("gpsimd", fn, r, w)


def PE(P, fn, r=(), w=()):
    return P.op("tensor", fn, r, w)


class Ctx:
    pass


def mm_group(P, out, pairs, r, w):
    def fn(e):
        ins = None
        n = len(pairs)
        for i, (l, rr) in enumerate(pairs):
            ins = e.matmul(out, l, rr, start=(i == 0), stop=(i == n - 1))
        return ins
    return PE(P, fn, r, w)


C_IDENT, C_ONES, C_BD, C_SU, C_IU, C_SL, C_MROW, C_RMAT, C_SEL65, C_INVF, C_SEL8, C_W = (
    0, 128, 256, 384, 512, 640, 768, 1280, 1376, 1440, 1472, 2496)


def make_consts():
    c = np.zeros((128, C_W), np.float32)
    r = np.arange(128)
    c[:, C_IDENT:C_IDENT + 128] = np.eye(128)
    c[:, C_ONES:C_ONES + 128] = 1.0
    bd = (r[:, None] // 64) == (r[None, :] // 64)
    c[:, C_BD:C_BD + 128] = bd
    su = r[:, None] < r[None, :]
    iu = r[:, None] <= r[None, :]
    c[:, C_SU:C_SU + 128] = su
    c[:, C_IU:C_IU + 128] = iu
    c[:, C_SL:C_SL + 128] = su.T
    c[:, C_MROW:C_MROW + 512] = np.concatenate([su, iu, su, iu], axis=1)
    rm = np.zeros((96, 96), np.float32)
    for i in range(16):
        rm[80 + i, 64 + i] = -1.0
        rm[64 + i, 80 + i] = 1.0
    c[0:96, C_RMAT:C_RMAT + 96] = rm
    c[64, C_SEL65:C_SEL65 + 64] = 1.0
    inv = (10000.0 ** (-np.arange(0, 32, 2, dtype=np.float32) / np.float32(32))).astype(np.float32)
    c[64:80, C_INVF] = inv
    c[80:96, C_INVF] = inv
    for e in range(8):
        c[e, C_SEL8 + e * 128:C_SEL8 + (e + 1) * 128] = 1.0
    return c


def pk(v, n):
    return np.ascontiguousarray(np.asarray(v, np.float32).reshape(n, 128).T)


def prep_shared(I):
    S = {}
    S["ada_w"] = np.ascontiguousarray(I["ada_w"], np.float32)
    S["ada_b"] = pk(I["ada_b"], 48)
    S["ada_lb"] = np.stack([pk(I["ada_layer_bias"][l], 48) for l in range(L)])
    S["n1g"] = np.stack([pk(I["norm1_g"][l], 8) for l in range(L)])
    S["n2g"] = np.stack([pk(I["norm2_g"][l], 8) for l in range(L)])
    w = I["w_in"]
    wi = np.zeros((L, D, NCH_IN * 128), np.float32)
    wi[:, :, 0:512] = w[:, :, 0:512]
    wi[:, :, 512:768] = w[:, :, 512:768]
    wi[:, :, 768:896] = w[:, :, 768:896]
    wi[:, :, 896 + 64:896 + 96] = w[:, :, 896:928]
    rb = 928
    wi[:, :, 1024:1792] = w[:, :, rb:rb + 768]
    wi[:, :, 1792:1920] = w[:, :, rb + 768:rb + 896]
    wi[:, :, 1920:2048] = w[:, :, rb + 896:rb + 1024]
    S["w_in"] = wi
    S["w_out"] = np.ascontiguousarray(I["w_out"], np.float32)
    S["conv_w"] = np.ascontiguousarray(I["conv_w"].reshape(L, 31, 2, 128).transpose(0, 2, 3, 1), np.float32)
    S["conv_p"] = np.stack([np.concatenate([pk(I["conv_b"][l], 2), pk(I["conv_ln_g"][l], 2),
                                            pk(I["conv_ln_b"][l], 2)], axis=1) for l in range(L)])
    mp = np.zeros((L, 128, 5), np.float32)
    for l in range(L):
        mp[l, :, 0:2] = pk(I["mla_q_norm_g"][l], 2)
        mp[l, :, 2] = I["mla_kv_norm_g"][l]
        mp[l, 0:96, 3] = I["qk_norm_q"][l]
        mp[l, 0:96, 4] = I["qk_norm_k"][l]
    S["mla_p"] = mp
    S["w_uq"] = np.ascontiguousarray(I["mla_w_uq"], np.float32)
    wkv = I["mla_w_ukv"].reshape(L, 128, 8, 128)
    S["w_k"] = np.ascontiguousarray(wkv[:, :, :, 0:64].reshape(L, 128, 512), np.float32)
    S["w_v"] = np.ascontiguousarray(wkv[:, :, :, 64:128].reshape(L, 128, 512), np.float32)
    S["rw_mu"] = np.stack([pk(I["rwkv_mu"][l], 8) for l in range(L)])
    S["rw_p"] = np.stack([np.concatenate([pk(I[k][l].reshape(-1), 2) for k in
                                          ("rwkv_w0", "rwkv_a0", "rwkv_k_k", "rwkv_k_a", "rwkv_r_k",
                                           "rwkv_ln_g", "rwkv_ln_b")], axis=1) for l in range(L)])
    S["rw_wup"] = np.ascontiguousarray(np.concatenate([I["rwkv_w_up"], I["rwkv_a_up"]], axis=1), np.float32)
    S["rw_gup"] = np.ascontiguousarray(I["rwkv_g_up"], np.float32)
    S["ffn_g"] = np.ascontiguousarray(I["ffn_w_gate"], np.float32)
    S["ffn_u"] = np.ascontiguousarray(I["ffn_w_up"], np.float32)
    S["ffn_d"] = np.ascontiguousarray(I["ffn_w_down"], np.float32)
    S["moe_r"] = np.ascontiguousarray(I["moe_router"].reshape(2, 8, 128, 8).transpose(0, 2, 1, 3).reshape(2, 128, 64),
                                      np.float32)
    S["moe_g"] = np.ascontiguousarray(I["moe_w_gate"], np.float32)
    S["moe_u"] = np.ascontiguousarray(I["moe_w_up"], np.float32)
    S["moe_d"] = np.ascontiguousarray(I["moe_w_down"], np.float32)
    S["cst"] = make_consts()
    return S


def prep_core(I, b):
    return {"xT": np.ascontiguousarray(I["x"][b].T, np.float32),
            "c_pk": pk(I["c"][b], 8),
            "pos": np.ascontiguousarray(I["positions"][b].reshape(1, T), np.int32)}


def build(n_layers=L, phases="ABCDEF", dbg_out=()):
    nc = bass.Bass("TRN2", target_bir_lowering=False)
    P = Prog(nc)

    def din(name, shape, dt=F32):
        return nc.dram_tensor(name, list(shape), dt, kind="ExternalInput").ap()

    def dscr(name, shape, dt=F32):
        kind = "ExternalOutput" if name in dbg_out else "Internal"
        return nc.dram_tensor(name, list(shape), dt, kind=kind).ap()

    xT_in = din("xT", [D, T])
    c_in = din("c_pk", [128, 8])
    pos_in = din("pos", [1, T], I32)
    ada_w = din("ada_w", [D, 6 * D])
    ada_b = din("ada_b", [128, 48])
    ada_lb = din("ada_lb", [L, 128, 48])
    n1g = din("n1g", [L, 128, 8])
    n2g = din("n2g", [L, 128, 8])
    w_in = din("w_in", [L, D, NCH_IN * 128])
    w_out = din("w_out", [L, D, D])
    conv_w = din("conv_w", [L, 2, 128, 31])
    conv_p = din("conv_p", [L, 128, 6])
    mla_p = din("mla_p", [L, 128, 5])
    w_uq = din("w_uq", [L, 256, 768])
    w_k = din("w_k", [L, 128, 512])
    w_v = din("w_v", [L, 128, 512])
    rw_mu = din("rw_mu", [L, 128, 8])
    rw_p = din("rw_p", [L, 128, 14])
    rw_wup = din("rw_wup", [L, 128, 256])
    rw_gup = din("rw_gup", [L, 128, 256])
    if "F" in phases:
        ffn_g = din("ffn_g", [2, D, DFF])
        ffn_u = din("ffn_u", [2, D, DFF])
        ffn_d = din("ffn_d", [2, DFF, D])
        moe_r = din("moe_r", [2, 128, 64])
        moe_g = din("moe_g", [2, NE, D, MFF])
        moe_u = din("moe_u", [2, NE, D, MFF])
        moe_d = din("moe_d", [2, NE, MFF, D])
    cst = din("cst", [128, C_W])

    xS = nc.dram_tensor("outT", [D, T], F32, kind="ExternalOutput").ap()
    uS = dscr("uS", [NCH_IN * 128, T])
    mixS = dscr("mixS", [D, T], BF16)
    csS = dscr("csS", [2, 96, T])
    qS = dscr("qS", [8, 96, T], BF16)
    kS = dscr("kS", [8, 96, T], BF16)

    es = contextlib.ExitStack()

    uid = [0]

    def sb(name, shape, dt=F32, stack=None):
        uid[0] += 1
        return (stack or es).enter_context(nc.sbuf_tensor(f"{name}_{uid[0]}", list(shape), dt))

    pb = [es.enter_context(nc.psum_tensor(f"pb{i}", [128, 512], F32)) for i in range(8)]

    def PB(i):
        return ("pb", i)

    cstt = sb("cstt", [128, C_W])
    P.dma("sync", cstt[:, :], cst[:, :], writes=["cst"])
    ident = cstt[:, C_IDENT:C_IDENT + 128]
    ones_f = cstt[:, C_ONES:C_ONES + 128]
    bd_ones = cstt[:, C_BD:C_BD + 128]
    m_sl = cstt[:, C_SL:C_SL + 128]
    m_row = cstt[:, C_MROW:C_MROW + 512]
    rmat = cstt[0:96, C_RMAT:C_RMAT + 96]
    sel65 = cstt[0:65, C_SEL65:C_SEL65 + 64]
    invf = cstt[0:96, C_INVF:C_INVF + 1]
    cbf = sb("cbf", [128, 384], BF16)
    V(P, lambda e: e.tensor_copy(out=cbf[:, 0:256], in_=cstt[:, 0:256]), ["cst"], ["cbf"])
    V(P, lambda e: e.tensor_copy(out=cbf[:, 256:384], in_=cstt[:, C_IU:C_IU + 128]), ["cst"], ["cbf"])
    ident_b = cbf[:, 0:128]
    ones_b = cbf[:, 128:256]
    tri_b = cbf[:, 256:384]
    zero_t = sb("zero_t", [128, 128])
    V(P, lambda e: e.memset(zero_t[:, :], 0.0), [], ["zero"])

    mod = sb("mod", [128, L, 48])
    modp = sb("modp", [128, L, 16])

    with contextlib.ExitStack() as st:
        cpk = sb("cpk", [128, 8], stack=st)
        scs = sb("scs", [128, 8], stack=st)
        P.dma("sync", cpk[:, :], c_in[:, :], writes=["cpk"])
        A(P, lambda e: e.activation(out=scs[:, :], in_=cpk[:, :], func=AF.Silu), ["cpk"], ["scs"])
        aw = [sb(f"aw{i}", [128, 6 * D], stack=st) for i in range(2)]
        for k in range(8):
            t = aw[k % 2]
            kk = f"aw{k % 2}"
            for j in range(4):
                P.dma("sync" if j % 2 == 0 else "gpsimd", t[:, j * 1536:(j + 1) * 1536],
                      ada_w[k * 128:(k + 1) * 128, j * 1536:(j + 1) * 1536], writes=[kk])

            def fn(e, t=t, k=k):
                ins = None
                for m in range(48):
                    ins = e.matmul(pb[0][:, k * 48 + m:k * 48 + m + 1], t[:, m * 128:(m + 1) * 128],
                                   scs[:, k:k + 1], start=True, stop=True)
                return ins
            PE(P, fn, [kk, "scs"], [PB(0)])
        cond = sb("cond", [128, 48], stack=st)
        ab = sb("ab", [128, 48], stack=st)
        alb = sb("alb", [128, L, 48], stack=st)
        P.dma("sync", ab[:, :], ada_b[:, :], writes=["ab"])
        for l in range(L):
            P.dma("sync", alb[:, l, :], ada_lb[l], writes=["alb"])
        V(P, lambda e: e.tensor_tensor(out=cond[:, :], in0=pb[0][:, 0:48], in1=ab[:, :], op=ALU.add),
          [PB(0), "ab"], ["cond"])
        for k in range(1, 8):
            V(P, lambda e, k=k: e.tensor_tensor(out=cond[:, :], in0=pb[0][:, k * 48:(k + 1) * 48], in1=cond[:, :],
                                                op=ALU.add), [PB(0), "cond"], ["cond"])
        g1t = sb("g1t", [128, L, 8], stack=st)
        g2t = sb("g2t", [128, L, 8], stack=st)
        for l in range(L):
            P.dma("sync", g1t[:, l, :], n1g[l], writes=["g1t"])
            P.dma("sync", g2t[:, l, :], n2g[l], writes=["g2t"])
        for l in range(L):
            V(P, lambda e, l=l: e.tensor_tensor(out=mod[:, l, :], in0=cond[:, :], in1=alb[:, l, :], op=ALU.add),
              ["cond", "alb"], ["mod"])
            V(P, lambda e, l=l: e.scalar_tensor_tensor(out=modp[:, l, 0:8], in0=mod[:, l, 8:16], scalar=1.0,
                                                       in1=g1t[:, l, :], op0=ALU.add, op1=ALU.mult),
              ["mod", "g1t"], ["modp"])
            V(P, lambda e, l=l: e.scalar_tensor_tensor(out=modp[:, l, 8:16], in0=mod[:, l, 32:40], scalar=1.0,
                                                       in1=g2t[:, l, :], op0=ALU.add, op1=ALU.mult),
              ["mod", "g2t"], ["modp"])
        posi = sb("posi", [96, T], I32, stack=st)
        ang = sb("ang", [96, T], stack=st)
        tb = sb("tb", [96, T], stack=st)
        P.dma("sync", posi[:, :], pos_in[0:1, :].partition_broadcast(96), writes=["posi"])
        V(P, lambda e: e.tensor_copy(out=ang[:, :], in_=posi[:, :]), ["posi"], ["ang"])
        V(P, lambda e: e.tensor_scalar(out=ang[:, :], in0=ang[:, :], scalar1=invf, scalar2=None, op0=ALU.mult),
          ["ang", "cst"], ["ang"])
        TWO_PI = 2.0 * np.pi
        ki = sb("ki", [96, T], I32, stack=st)
        kf = sb("kf", [96, T], stack=st)
        for ci, shift in ((1, 0.0), (0, np.pi / 2)):
            V(P, lambda e, shift=shift: e.tensor_scalar(out=tb[:, :], in0=ang[:, :], scalar1=float(shift),
                                                        scalar2=float(1.0 / TWO_PI), op0=ALU.add, op1=ALU.mult),
              ["ang"], ["tb"])
            V(P, lambda e: e.tensor_copy(out=ki[:, :], in_=tb[:, :]), ["tb"], ["ki"])
            V(P, lambda e: e.tensor_copy(out=kf[:, :], in_=ki[:, :]), ["ki"], ["kf"])
            V(P, lambda e, shift=shift: e.tensor_scalar(out=tb[:, :], in0=ang[:, :], scalar1=float(shift),
                                                        scalar2=None, op0=ALU.add), ["ang"], ["tb"])
            V(P, lambda e: e.scalar_tensor_tensor(out=tb[:, :], in0=kf[:, :], scalar=float(-TWO_PI), in1=tb[:, :],
                                                  op0=ALU.mult, op1=ALU.add), ["kf", "tb"], ["tb"])
            V(P, lambda e: e.tensor_scalar(out=kf[:, :], in0=tb[:, :], scalar1=float(np.pi), scalar2=None,
                                           op0=ALU.is_gt), ["tb"], ["kf"])
            V(P, lambda e: e.scalar_tensor_tensor(out=tb[:, :], in0=kf[:, :], scalar=float(-TWO_PI), in1=tb[:, :],
                                                  op0=ALU.mult, op1=ALU.add), ["kf", "tb"], ["tb"])
            V(P, lambda e: e.tensor_scalar(out=kf[:, :], in0=tb[:, :], scalar1=float(-np.pi), scalar2=None,
                                           op0=ALU.is_lt), ["tb"], ["kf"])
            V(P, lambda e: e.scalar_tensor_tensor(out=tb[:, :], in0=kf[:, :], scalar=float(TWO_PI), in1=tb[:, :],
                                                  op0=ALU.mult, op1=ALU.add), ["kf", "tb"], ["tb"])
            V(P, lambda e: e.tensor_scalar(out=tb[:, :], in0=tb[:, :], scalar1=3.1415925, scalar2=-3.1415925,
                                           op0=ALU.min, op1=ALU.max), ["tb"], ["tb"])
            A(P, lambda e: e.activation(out=tb[:, :], in_=tb[:, :], func=AF.Sin), ["tb"], ["tb"])
            P.dma("sync", csS[ci], tb[:, :], reads=["tb"], writes=[("csS", ci)])
        xc = [sb(f"xc{i}", [128, T], stack=st) for i in range(2)]
        for k in range(8):
            t = xc[k % 2]
            q = "sync" if k % 2 == 0 else "gpsimd"
            P.dma(q, t[:, :], xT_in[k * 128:(k + 1) * 128, :], writes=[f"xc{k % 2}"])
            P.dma(q, xS[k * 128:(k + 1) * 128, :], t[:, :], reads=[f"xc{k % 2}"], writes=[("xS", k)])
        P.barrier()

    ring = {}

    def rr(name, n):
        i = ring.get(name, 0)
        ring[name] = i + 1
        return i % n

    def load_cast(dst3, dkey, src2d, nk, ncols, stg, stgname, cast_eng="gpsimd"):
        per = max(1, 2048 // ncols)
        k = 0
        while k < nk:
            kk = min(per, nk - k)
            i = rr(stgname, len(stg))
            s = stg[i]
            skey = (stgname, i)
            sv = s[:, 0:kk * ncols].rearrange("p (k c) -> p k c", k=kk)
            q = "sync" if (ring[stgname] % 2 == 0) else "gpsimd"
            P.dma(q, sv, src2d[k * 128:(k + kk) * 128, :].rearrange("(k p) c -> p k c", p=128), writes=[skey])
            eng = cast_eng if cast_eng != "alt" else ("gpsimd" if ring[stgname] % 2 == 0 else "vector")
            if eng == "scalar":
                A(P, lambda e, sv=sv, k=k, kk=kk: e.copy(out=dst3[:, k:k + kk, :], in_=sv), [skey], [dkey])
            else:
                P.op(eng, lambda e, sv=sv, k=k, kk=kk: e.tensor_copy(out=dst3[:, k:k + kk, :], in_=sv), [skey], [dkey])
            k += kk

    def rms_h(xt, xkey, hb, hkey, gm, sh, tmp, hf=None, hfkey=None):
        sq, rstd, t2 = tmp["sq"], tmp["rstd"], tmp["t2"]
        xkeys = xkey if isinstance(xkey, list) else [xkey] * 8
        A(P, lambda e: e.activation(out=sq[:, :, :], in_=xt, func=AF.Square), list(set(xkeys)), ["sq"])
        mm_group(P, pb[7][:, :], [(ones_b, sq[:, k, :]) for k in range(8)], ["sq", "cbf"], [PB(7)])
        A(P, lambda e: e.activation(out=rstd[:, :], in_=pb[7][:, :], func=AF.Sqrt, scale=1.0 / D, bias=RMS_EPS),
          [PB(7)], ["rstd"])
        V(P, lambda e: e.reciprocal(out=rstd[:, :], in_=rstd[:, :]), ["rstd"], ["rstd"])
        for k in range(8):
            i = rr("t2", 2)
            V(P, lambda e, k=k, i=i: e.scalar_tensor_tensor(out=t2[i][:, :], in0=xt[:, k, :], scalar=gm[:, k:k + 1],
                                                            in1=rstd[:, :], op0=ALU.mult, op1=ALU.mult),
              [xkeys[k], "rstd", "modp"], [("t2", i)])
            if hf is None:
                A(P, lambda e, k=k, i=i: e.activation(out=hb[:, k, :], in_=t2[i][:, :], func=AF.Identity,
                                                      bias=sh[:, k:k + 1]), [("t2", i), "mod"], [hkey])
            else:
                A(P, lambda e, k=k, i=i: e.activation(out=hf[:, k, :], in_=t2[i][:, :], func=AF.Identity,
                                                      bias=sh[:, k:k + 1]), [("t2", i), "mod"], [hfkey])
                G(P, lambda e, k=k: e.tensor_copy(out=hb[:, k, :], in_=hf[:, k, :]), [hfkey], [hkey])

    EPS_T = {}

    def eps_tile(val):
        return float(val)

    for l in range(n_layers):
        gm1, gm2 = modp[:, l, 0:8], modp[:, l, 8:16]
        sh1, gt1 = mod[:, l, 0:8], mod[:, l, 16:24]
        sh2, gt2 = mod[:, l, 24:32], mod[:, l, 40:48]

        if "A" in phases:
            with contextlib.ExitStack() as st:
                wA = sb("wA", [128, 8, 2048], BF16, stack=st)
                stg = [sb(f"stgA{i}", [128, 2048], stack=st) for i in range(3)]
                load_cast(wA, "wA", w_in[l], 8, 2048, stg, "stgA", cast_eng="alt")
                xtb = [sb(f"xtA{i}", [128, 8, 512], stack=st) for i in range(2)]
                hb = [sb(f"hbA{i}", [128, 8, 512], BF16, stack=st) for i in range(2)]
                tmp = {"sq": sb("sqA", [128, 8, 512], BF16, stack=st), "rstd": sb("rstdA", [128, 512], stack=st),
                       "t2": [sb(f"t2A{i}", [128, 512], stack=st) for i in range(2)]}
                uo = [sb(f"uoA{i}", [128, 512], stack=st) for i in range(4)]
                for n in range(NT):
                    xt = xtb[n % 2]
                    xkey = f"xtA{n % 2}"
                    for k in range(8):
                        P.dma("sync" if k % 2 == 0 else "gpsimd", xt[:, k, :],
                              xS[k * 128:(k + 1) * 128, n * 512:(n + 1) * 512], writes=[xkey])
                    h = hb[n % 2]
                    hkey = f"hbA{n % 2}"
                    rms_h(xt[:, :, :], xkey, h, hkey, gm1, sh1, tmp)
                    for m in range(NCH_IN):
                        bi = m % 4
                        mm_group(P, pb[bi][:, :], [(wA[:, k, m * 128:(m + 1) * 128], h[:, k, :]) for k in range(8)],
                                 ["wA", hkey], [PB(bi)])
                        oi = rr("uo", 4)
                        if m % 2 == 0:
                            V(P, lambda e, bi=bi, oi=oi: e.tensor_copy(out=uo[oi][:, :], in_=pb[bi][:, :]),
                              [PB(bi)], [("uo", oi)])
                        else:
                            A(P, lambda e, bi=bi, oi=oi: e.copy(out=uo[oi][:, :], in_=pb[bi][:, :]),
                              [PB(bi)], [("uo", oi)])
                        P.dma("sync", uS[m * 128:(m + 1) * 128, n * 512:(n + 1) * 512], uo[oi][:, :],
                              reads=[("uo", oi)], writes=[("uS", m, n)])
                P.barrier()

        if "B" in phases:
            with contextlib.ExitStack() as st:
                cw = sb("cw", [128, 2, 31], stack=st)
                cp = sb("cp", [128, 6], stack=st)
                for cc in range(2):
                    P.dma("sync", cw[:, cc, :], conv_w[l, cc], writes=["cw"])
                P.dma("sync", cp[:, :], conv_p[l], writes=["cp"])
                dg = sb("dg", [128, 62, 128], BF16, stack=st)
                for cc in range(2):
                    for k in range(31):
                        P.op("gpsimd" if k % 2 else "vector",
                             lambda e, cc=cc, k=k: e.tensor_scalar(out=dg[:, cc * 31 + k, :], in0=ident,
                                                                   scalar1=cw[:, cc, k:k + 1], scalar2=None,
                                                                   op0=ALU.mult), ["cw", "cst"], [("dg", cc, k)])
                at = [sb(f"atB{i}", [128, 542], stack=st) for i in range(2)]
                gtl = [sb(f"gtB{i}", [128, 542], stack=st) for i in range(2)]
                zb = [sb(f"zbB{i}", [128, 542], BF16, stack=st) for i in range(2)]
                zc = sb("zcB", [128, 2, 512], stack=st)
                xcB = sb("xcB", [128, 2, 512], stack=st)
                sqB = sb("sqB", [128, 2, 512], stack=st)
                rsB = sb("rsB", [128, 512], stack=st)
                tB = sb("tB", [128, 512], stack=st)
                yb = [sb(f"ybB{i}", [128, 512], BF16, stack=st) for i in range(2)]
                for n in range(NT):
                    for cc in range(2):
                        i = rr("atB", 2)
                        a_, g_, z_ = at[i], gtl[i], zb[i]
                        ak, gk, zk = ("atB", i), ("gtB", i), ("zbB", i)
                        if n == 0:
                            V(P, lambda e, a_=a_: e.memset(a_[:, 0:30], 0.0), [], [ak])
                            V(P, lambda e, g_=g_: e.memset(g_[:, 0:30], 0.0), [], [gk])
                            P.dma("sync", a_[:, 30:542], uS[cc * 128:(cc + 1) * 128, 0:512], writes=[ak])
                            P.dma("gpsimd", g_[:, 30:542], uS[(2 + cc) * 128:(3 + cc) * 128, 0:512], writes=[gk])
                        else:
                            P.dma("sync", a_[:, :], uS[cc * 128:(cc + 1) * 128, n * 512 - 30:(n + 1) * 512], writes=[ak])
                            P.dma("gpsimd", g_[:, :], uS[(2 + cc) * 128:(3 + cc) * 128, n * 512 - 30:(n + 1) * 512],
                                  writes=[gk])
                        A(P, lambda e, g_=g_: e.activation(out=g_[:, :], in_=g_[:, :], func=AF.Sigmoid), [gk], [gk])
                        V(P, lambda e, a_=a_, g_=g_, z_=z_: e.tensor_tensor(out=z_[:, :], in0=a_[:, :], in1=g_[:, :],
                                                                            op=ALU.mult), [ak, gk], [zk])
                        mm_group(P, pb[cc][:, :], [(dg[:, cc * 31 + k, :], z_[:, k:k + 512]) for k in range(31)],
                                 [zk] + [("dg", cc, k) for k in range(31)], [PB(cc)])
                        A(P, lambda e, cc=cc: e.activation(out=zc[:, cc, :], in_=pb[cc][:, :], func=AF.Identity,
                                                           bias=cp[:, cc:cc + 1]), [PB(cc), "cp"], ["zcB"])
                    mm_group(P, pb[2][:, :], [(ones_f, zc[:, 0, :]), (ones_f, zc[:, 1, :])], ["zcB", "cst"], [PB(2)])
                    for cc in range(2):
                        V(P, lambda e, cc=cc: e.scalar_tensor_tensor(out=xcB[:, cc, :], in0=pb[2][:, :],
                                                                     scalar=-1.0 / 256, in1=zc[:, cc, :],
                                                                     op0=ALU.mult, op1=ALU.add),
                          [PB(2), "zcB"], ["xcB"])
                    A(P, lambda e: e.activation(out=sqB[:, :, :], in_=xcB[:, :, :], func=AF.Square), ["xcB"], ["sqB"])
                    mm_group(P, pb[3][:, :], [(ones_f, sqB[:, 0, :]), (ones_f, sqB[:, 1, :])], ["sqB", "cst"], [PB(3)])
                    A(P, lambda e: e.activation(out=rsB[:, :], in_=pb[3][:, :], func=AF.Sqrt, scale=1.0 / 256,
                                                bias=LN_EPS), [PB(3)], ["rsB"])
                    V(P, lambda e: e.reciprocal(out=rsB[:, :], in_=rsB[:, :]), ["rsB"], ["rsB"])
                    for cc in range(2):
                        V(P, lambda e, cc=cc: e.scalar_tensor_tensor(out=tB[:, :], in0=xcB[:, cc, :],
                                                                     scalar=cp[:, 2 + cc:3 + cc], in1=rsB[:, :],
                                                                     op0=ALU.mult, op1=ALU.mult),
                          ["xcB", "rsB", "cp"], ["tB"])
                        yi = rr("ybB", 2)
                        A(P, lambda e, cc=cc, yi=yi: e.activation(out=yb[yi][:, :], in_=tB[:, :], func=AF.Silu,
                                                                  bias=cp[:, 4 + cc:5 + cc]), ["tB", "cp"], [("ybB", yi)])
                        P.dma("sync", mixS[cc * 128:(cc + 1) * 128, n * 512:(n + 1) * 512], yb[yi][:, :],
                              reads=[("ybB", yi)], writes=[("mixS", cc, n)])
                P.barrier()

        if "C" in phases:
            with contextlib.ExitStack() as st:
                Vres = sb("Vres", [128, 32, 8, 65], BF16, stack=st)
                V(P, lambda e: e.memset(Vres[:, :, :, 64:65], 1.0), [], ["Vres"])
                with contextlib.ExitStack() as st1:
                    mp = sb("mp", [128, 5], stack=st1)
                    P.dma("sync", mp[:, :], mla_p[l], writes=["mp"])
                    wq = sb("wq", [128, 2, 768], BF16, stack=st1)
                    wk = sb("wk", [128, 1, 512], BF16, stack=st1)
                    wv = sb("wv", [128, 1, 512], BF16, stack=st1)
                    stg = [sb(f"stgC{i}", [128, 2048], stack=st1) for i in range(2)]
                    load_cast(wq, "wq", w_uq[l], 2, 768, stg, "stgC")
                    load_cast(wk, "wk", w_k[l], 1, 512, stg, "stgC")
                    load_cast(wv, "wv", w_v[l], 1, 512, stg, "stgC")
                    cq = sb("cq", [128, 2, 512], stack=st1)
                    ckv = sb("ckv", [128, 512], stack=st1)
                    krt = sb("krt", [128, 512], stack=st1)
                    cst_ = sb("cosC", [96, 512], stack=st1)
                    snt_ = sb("sinC", [96, 512], stack=st1)
                    sqC = sb("sqC", [128, 3, 512], BF16, stack=st1)
                    rqC = sb("rqC", [128, 2, 512], stack=st1)
                    cqn = sb("cqn", [128, 2, 512], BF16, stack=st1)
                    ckvn = sb("ckvn", [128, 512], BF16, stack=st1)
                    raw = [sb(f"rawC{i}", [96, 512], stack=st1) for i in range(4)]
                    sqh = [sb(f"sqhC{i}", [96, 512], stack=st1) for i in range(4)]
                    rsh = [sb(f"rshC{i}", [96, 512], stack=st1) for i in range(4)]
                    qn = [sb(f"qnC{i}", [96, 512], stack=st1) for i in range(4)]
                    t1 = [sb(f"t1C{i}", [96, 512], stack=st1) for i in range(4)]
                    qf = [sb(f"qfC{i}", [96, 512], BF16, stack=st1) for i in range(4)]

                    def norm_rope_multi(chains):
                        for (i, gcol, dst, dkey) in chains:
                            A(P, lambda e: e.activation(out=sqh[i][:, :], in_=raw[i][:, :], func=AF.Square),
                              [("rawC", i)], [("sqhC", i)])
                        for (i, gcol, dst, dkey) in chains:
                            bi = 4 + i
                            PE(P, lambda e: e.matmul(pb[bi][0:96, :], ones_f[0:96, 0:96], sqh[i][:, :], start=True,
                                                     stop=True), [("sqhC", i), "cst"], [PB(bi)])
                        for (i, gcol, dst, dkey) in chains:
                            bi = 4 + i
                            A(P, lambda e: e.activation(out=rsh[i][:, :], in_=pb[bi][0:96, :], func=AF.Sqrt,
                                                        scale=1.0 / 96, bias=RMS_EPS), [PB(bi)], [("rshC", i)])
                        for (i, gcol, dst, dkey) in chains:
                            V(P, lambda e: e.reciprocal(out=rsh[i][:, :], in_=rsh[i][:, :]), [("rshC", i)], [("rshC", i)])
                            V(P, lambda e: e.scalar_tensor_tensor(out=qn[i][:, :], in0=raw[i][:, :], scalar=gcol,
                                                                  in1=rsh[i][:, :], op0=ALU.mult, op1=ALU.mult),
                              [("rawC", i), ("rshC", i), "mp"], [("qnC", i)])
                        for (i, gcol, dst, dkey) in chains:
                            bi = 4 + i
                            PE(P, lambda e: e.matmul(pb[bi][0:96, :], rmat, qn[i][:, :], start=True, stop=True),
                               [("qnC", i), "cst"], [PB(bi)])
                        for (i, gcol, dst, dkey) in chains:
                            bi = 4 + i
                            V(P, lambda e: e.tensor_tensor(out=t1[i][:, :], in0=pb[bi][0:96, :], in1=snt_[:, :],
                                                           op=ALU.mult), [PB(bi), "sinC"], [("t1C", i)])
                            G(P, lambda e: e.tensor_tensor(out=qn[i][:, :], in0=qn[i][:, :], in1=cst_[:, :], op=ALU.mult),
                              [("qnC", i), "cosC"], [("qnC", i)])
                        for (i, gcol, dst, dkey) in chains:
                            G(P, lambda e: e.tensor_tensor(out=qf[i][:, :], in0=qn[i][:, :], in1=t1[i][:, :], op=ALU.add),
                              [("qnC", i), ("t1C", i)], [("qfC", i)])
                            P.dma("sync" if i % 2 == 0 else "gpsimd", dst, qf[i][:, :], reads=[("qfC", i)], writes=[dkey])

                    for n in range(NT):
                        tsl = slice(n * 512, (n + 1) * 512)
                        for k in range(2):
                            P.dma("sync", cq[:, k, :], uS[(4 + k) * 128:(5 + k) * 128, tsl], writes=["cq"])
                        P.dma("gpsimd", ckv[:, :], uS[6 * 128:7 * 128, tsl], writes=["ckv"])
                        P.dma("gpsimd", krt[:, :], uS[7 * 128:8 * 128, tsl], writes=["krt"])
                        P.dma("sync", cst_[:, :], csS[0][:, tsl], writes=["cosC"])
                        P.dma("sync", snt_[:, :], csS[1][:, tsl], writes=["sinC"])
                        A(P, lambda e: e.activation(out=sqC[:, 0:2, :], in_=cq[:, :, :], func=AF.Square), ["cq"], ["sqC"])
                        A(P, lambda e: e.activation(out=sqC[:, 2, :], in_=ckv[:, :], func=AF.Square), ["ckv"], ["sqC"])
                        mm_group(P, pb[0][:, :], [(ones_b, sqC[:, 0, :]), (ones_b, sqC[:, 1, :])], ["sqC", "cbf"], [PB(0)])
                        mm_group(P, pb[1][:, :], [(ones_b, sqC[:, 2, :])], ["sqC", "cbf"], [PB(1)])
                        A(P, lambda e: e.activation(out=rqC[:, 0, :], in_=pb[0][:, :], func=AF.Sqrt, scale=1.0 / 256,
                                                    bias=RMS_EPS), [PB(0)], ["rqC"])
                        A(P, lambda e: e.activation(out=rqC[:, 1, :], in_=pb[1][:, :], func=AF.Sqrt, scale=1.0 / 128,
                                                    bias=RMS_EPS), [PB(1)], ["rqC"])
                        V(P, lambda e: e.reciprocal(out=rqC[:, :, :], in_=rqC[:, :, :]), ["rqC"], ["rqC"])
                        for k in range(2):
                            V(P, lambda e, k=k: e.scalar_tensor_tensor(out=cqn[:, k, :], in0=cq[:, k, :],
                                                                       scalar=mp[:, k:k + 1], in1=rqC[:, 0, :],
                                                                       op0=ALU.mult, op1=ALU.mult),
                              ["cq", "rqC", "mp"], ["cqn"])
                        V(P, lambda e: e.scalar_tensor_tensor(out=ckvn[:, :], in0=ckv[:, :], scalar=mp[:, 2:3],
                                                              in1=rqC[:, 1, :], op0=ALU.mult, op1=ALU.mult),
                          ["ckv", "rqC", "mp"], ["ckvn"])
                        for j in range(4):
                            bi = 2 + (j % 2)
                            PE(P, lambda e, j=j, bi=bi: e.matmul(pb[bi][:, :], ckvn[:, j * 128:(j + 1) * 128], wv[:, 0, :],
                                                                 start=True, stop=True), ["ckvn", "wv"], [PB(bi)])
                            A(P, lambda e, j=j, bi=bi, n=n: e.copy(
                                out=Vres[:, n * 4 + j, :, 0:64],
                                in_=pb[bi][:, :].rearrange("p (h d) -> p h d", h=8)), [PB(bi)], ["Vres"])
                        for hp in range(4):
                            chains = []
                            for hh in range(2):
                                h = 2 * hp + hh
                                iq, ik = 2 * hh, 2 * hh + 1
                                bq, bk = hh, 2 + hh
                                mm_group(P, pb[bq][0:96, :], [(wq[:, k, 96 * h:96 * h + 96], cqn[:, k, :]) for k in range(2)],
                                         ["wq", "cqn"], [PB(bq)])
                                PE(P, lambda e, h=h, bk=bk: e.matmul(pb[bk][0:64, :], wk[:, 0, 64 * h:64 * h + 64], ckvn[:, :],
                                                                     start=True, stop=True), ["wk", "ckvn"], [PB(bk)])
                                A(P, lambda e, iq=iq, bq=bq: e.copy(out=raw[iq][:, :], in_=pb[bq][0:96, :]), [PB(bq)],
                                  [("rawC", iq)])
                                V(P, lambda e, ik=ik, bk=bk: e.tensor_copy(out=raw[ik][0:64, :], in_=pb[bk][0:64, :]), [PB(bk)],
                                  [("rawC", ik)])
                                A(P, lambda e, ik=ik: e.copy(out=raw[ik][64:96, :], in_=krt[64:96, :]), ["krt"], [("rawC", ik)])
                                chains.append((iq, mp[0:96, 3:4], qS[h][:, tsl], ("qS", h, n)))
                                chains.append((ik, mp[0:96, 4:5], kS[h][:, tsl], ("kS", h, n)))
                            norm_rope_multi(chains)
                    P.barrier()
                with contextlib.ExitStack() as st2:
                    KT = [sb(f"KT{i}", [96, T], BF16, stack=st2) for i in range(2)]
                    QT = [sb(f"QT{i}", [96, T], BF16, stack=st2) for i in range(2)]
                    pt = [sb(f"ptC{i}", [128, 512], BF16, stack=st2) for i in range(4)]
                    osb = [sb(f"osb{i}", [65, 512], stack=st2) for i in range(2)]
                    rc = [sb(f"rcC{i}", [64, 512], stack=st2) for i in range(2)]
                    yo = [sb(f"yoC{i}", [64, 512], BF16, stack=st2) for i in range(2)]
                    for h in range(8):
                        kt_, qt_ = KT[h % 2], QT[h % 2]
                        kk_, qk_ = f"KT{h % 2}", f"QT{h % 2}"
                        for half in range(2):
                            hs = slice(half * 2048, (half + 1) * 2048)
                            P.dma("sync", kt_[:, hs], kS[h][:, hs], writes=[kk_])
                            P.dma("gpsimd", qt_[:, hs], qS[h][:, hs], writes=[qk_])
                        blocks = [(qi, kb) for qi in range(NT) for kb in range(4 * qi + 4)]

                        def geom(qi, kb):
                            d = kb - 4 * qi
                            return d, max(0, d) * 128

                        def emit_S(bi_):
                            qi, kb = blocks[bi_]
                            d, c0 = geom(qi, kb)
                            sbk = bi_ % 3
                            PE(P, lambda e: e.matmul(pb[sbk][:, c0:512], kt_[:, kb * 128:(kb + 1) * 128],
                                                     qt_[:, qi * 512 + c0:(qi + 1) * 512], start=True, stop=True),
                               [kk_, qk_], [PB(sbk)])

                        emit_S(0)
                        emit_S(1)
                        for bi_, (qi, kb) in enumerate(blocks):
                            d, c0 = geom(qi, kb)
                            nkb = 4 * qi + 4
                            ob = 4 + (qi % 2)
                            sbk = bi_ % 3
                            pi = bi_ % 4
                            A(P, lambda e: e.activation(out=pt[pi][:, c0:512], in_=pb[sbk][:, c0:512], func=AF.Exp,
                                                        scale=SCALE_QK), [PB(sbk)], [("ptC", pi)])
                            if d >= 0:
                                G(P, lambda e: e.tensor_tensor(out=pt[pi][:, c0:c0 + 128], in0=pt[pi][:, c0:c0 + 128],
                                                               in1=tri_b, op=ALU.mult), [("ptC", pi), "cbf"], [("ptC", pi)])
                            if bi_ + 2 < len(blocks):
                                emit_S(bi_ + 2)
                            PE(P, lambda e: e.matmul(pb[ob][0:65, c0:512], Vres[:, kb, h, :], pt[pi][:, c0:512],
                                                     start=(kb == 0), stop=(kb == nkb - 1)), [("ptC", pi), "Vres"], [PB(ob)])
                            if kb == nkb - 1:
                                oi = qi % 2
                                A(P, lambda e: e.copy(out=osb[oi][:, :], in_=pb[ob][0:65, :]), [PB(ob)], [("osb", oi)])
                                PE(P, lambda e: e.matmul(pb[6 + oi][0:64, :], sel65, osb[oi][:, :], start=True, stop=True),
                                   [("osb", oi), "cst"], [PB(6 + oi)])
                                V(P, lambda e: e.reciprocal(out=rc[oi][:, :], in_=pb[6 + oi][0:64, :]), [PB(6 + oi)],
                                  [("rcC", oi)])
                                V(P, lambda e: e.tensor_tensor(out=yo[oi][:, :], in0=osb[oi][0:64, :], in1=rc[oi][:, :],
                                                               op=ALU.mult), [("osb", oi), ("rcC", oi)], [("yoC", oi)])
                                P.dma("sync", mixS[256 + 64 * h:256 + 64 * (h + 1), qi * 512:(qi + 1) * 512], yo[oi][:, :],
                                      reads=[("yoC", oi)], writes=[("mixS", "m", h, qi)])
                    P.barrier()

        if "D" in phases:
            rwkv_phase(nc, P, l, locals())

        if "E" in phases:
            with contextlib.ExitStack() as st:
                wO = sb("wO", [128, 8, 1024], BF16, stack=st)
                stg = [sb(f"stgE{i}", [128, 2048], stack=st) for i in range(3)]
                load_cast(wO, "wO", w_out[l], 8, 1024, stg, "stgE", cast_eng="alt")
                xtb = [sb(f"xtE{i}", [128, 8, 512], stack=st) for i in range(2)]
                mtb = [sb(f"mtE{i}", [128, 8, 512], BF16, stack=st) for i in range(2)]
                for n in range(NT):
                    xt, mt = xtb[n % 2], mtb[n % 2]
                    xkey, mkey = f"xtE{n % 2}", f"mtE{n % 2}"
                    tsl = slice(n * 512, (n + 1) * 512)
                    for k in range(8):
                        P.dma("sync", xt[:, k, :], xS[k * 128:(k + 1) * 128, tsl], reads=[("xS", k, n)], writes=[(xkey, k)])
                        P.dma("gpsimd", mt[:, k, :], mixS[k * 128:(k + 1) * 128, tsl], writes=[mkey])
                    for m in range(8):
                        bi = m % 4
                        mm_group(P, pb[bi][:, :], [(wO[:, k, m * 128:(m + 1) * 128], mt[:, k, :]) for k in range(8)],
                                 ["wO", mkey], [PB(bi)])
                        V(P, lambda e, m=m, bi=bi, xt=xt: e.scalar_tensor_tensor(
                            out=xt[:, m, :], in0=pb[bi][:, :], scalar=gt1[:, m:m + 1], in1=xt[:, m, :],
                            op0=ALU.mult, op1=ALU.add), [PB(bi), (xkey, m), "mod"], [(xkey, m)])
                        P.dma("sync", xS[m * 128:(m + 1) * 128, tsl], xt[:, m, :], reads=[(xkey, m)],
                              writes=[("xS", m, n)])
                P.barrier()

        if "F" in phases:
            ffn_phase(nc, P, l, locals())

    P.barrier()
    es.close()
    return nc


RW_STOP = os.environ.get('RW_STOP', '')
RW_LVL = int(os.environ.get('RW_LVL', '99'))


USE_R32 = os.environ.get('USE_R32', '1') == '1'


def R32g(ap):
    return ap.bitcast(mybir.dt.float32r) if USE_R32 else ap


def rwkv_phase(nc, P, l, E):
    sb, pb, PB, uS, mixS, rr = E["sb"], E["pb"], E["PB"], E["uS"], E["mixS"], E["rr"]
    ident, ones_f, bd_ones, m_sl, m_row, zero_t = (E["ident"], E["ones_f"], E["bd_ones"], E["m_sl"], E["m_row"],
                                                   E["zero_t"])
    EM05 = float(np.exp(-0.5))
    X_ = mybir.AxisListType.X
    with contextlib.ExitStack() as st:
        mu = sb("muD", [128, 8], stack=st)
        rp = sb("rpD", [128, 14], stack=st)
        wup = sb("wupD", [128, 256], stack=st)
        gup = sb("gupD", [128, 256], stack=st)
        P.dma("sync", mu[:, :], E["rw_mu"][l], writes=["muD"])
        P.dma("sync", rp[:, :], E["rw_p"][l], writes=["rpD"])
        P.dma("sync", wup[:, :], E["rw_wup"][l], writes=["wupD"])
        P.dma("sync", gup[:, :], E["rw_gup"][l], writes=["gupD"])
        omka = sb("omkaD", [128, 2], stack=st)
        V(P, lambda e: e.tensor_scalar(out=omka[:, :], in0=rp[:, 6:8], scalar1=-1.0, scalar2=1.0, op0=ALU.mult,
                                       op1=ALU.add), ["rpD"], ["omkaD"])
        w0, a0, k_k, k_a, r_k, ln_g, ln_b = [rp[:, 2 * i:2 * i + 2] for i in range(7)]
        ST = [[sb(f"ST{c}{i}", [128, 64], stack=st) for i in range(2)] for c in range(2)]
        for c in range(2):
            V(P, lambda e, c=c: e.memset(ST[c][0][:, :], 0.0), [], [("ST", c, 0, 0), ("ST", c, 0, 1)])
        ur = sb("urD", [128, 8, 513], stack=st)
        us = sb("usD", [128, 8, 512], stack=st)
        dl = [sb(f"dlD{i}", [128, 512], stack=st) for i in range(2)]
        tw = sb("twD", [128, 512], stack=st)
        sgd = sb("sgdD", [128, 512], stack=st)

        def t2(name, shape=(128, 512)):
            return [sb(f"{name}{c}", list(shape), stack=st) for c in range(2)]
        def t22(name, shape=(128, 512)):
            return [[sb(f"{name}{pp}{c}", list(shape), stack=st) for c in range(2)] for pp in range(2)]
        dec, asg, gtc, kk, kk2, rn, kkn, tkk, km, bv = (t2("decD"), t2("asgD"), t22("gtcD"), t2("kkD"), t2("kk2D"),
                                                        t2("rnD"), t2("kknD"), None, t2("kmD"), t2("bvD"))
        Gi, Ginv, Ge, bt, kt, rk, bon, ysb, yc, sqy = (t22("GiD"), None, t2("GeD"), t22("btD"), t22("ktD"),
                                                       None, t22("bonD"), t2("ysbD"), None, None)
        ar = t22("arD", (128, 4, 256))
        btT, ktT, vT = t22("btTD", (128, 4, 128)), t22("ktTD", (128, 4, 128)), t22("vTD", (128, 4, 128))
        yo = [sb(f"yoD{c}", [128, 512], BF16, stack=st) for c in range(2)]
        Mm = [sb(f"MmD{q}", [128, 512], stack=st) for q in range(4)]
        FR = mybir.dt.float32r if USE_R32 else F32
        PT0 = [sb(f"PT0D{q}", [128, 128], stack=st) for q in range(4)]
        PTb = [[sb(f"PTD{q}{i}", [128, 128], FR, stack=st) for i in range(2)] for q in range(4)]
        Pmb = [[sb(f"PmD{q}{i}", [128, 128], FR, stack=st) for i in range(2)] for q in range(4)]
        Xb = [[sb(f"XD{q}{i}", [128, 128], FR, stack=st) for i in range(2)] for q in range(4)]
        XF = [sb(f"XFD{q}", [128, 128], stack=st) for q in range(4)]
        Wsb = [sb(f"WsbD{q}", [128, 64], stack=st) for q in range(4)]
        Usb = [sb(f"UsbD{q}", [128, 64], stack=st) for q in range(4)]

        def v4(t):
            return t[:, :].rearrange("p (j t) -> p j t", j=4)

        def prep_gen(n):
            p = n % 2
            if n == 0:
                V(P, lambda e: e.memset(ur[:, :, 0:1], 0.0), [], ["urD"])
                yield
            for j in range(8):
                q_ = "sync" if j % 2 == 0 else "gpsimd"
                if n == 0:
                    P.dma(q_, ur[:, j, 1:513], uS[(8 + j) * 128:(9 + j) * 128, 0:512], writes=["urD"])
                    yield
                else:
                    P.dma(q_, ur[:, j, :], uS[(8 + j) * 128:(9 + j) * 128, n * 512 - 1:(n + 1) * 512], writes=["urD"])
                    yield
            for j in range(8):
                eng = "vector" if j % 2 == 0 else "gpsimd"
                di = j % 2
                P.op(eng, lambda e, j=j, di=di: e.tensor_tensor(out=dl[di][:, :], in0=ur[:, j, 0:512], in1=ur[:, j, 1:513],
                                                                op=ALU.subtract), ["urD"], [("dlD", di)])
                yield
                P.op("vector", lambda e, j=j, di=di: e.scalar_tensor_tensor(out=us[:, j, :], in0=dl[di][:, :],
                                                                       scalar=mu[:, j:j + 1], in1=ur[:, j, 1:513],
                                                                       op0=ALU.mult, op1=ALU.add),
                     [("dlD", di), "urD", "muD"], [("usD", j)])
                yield
            A(P, lambda e: e.activation(out=tw[0:64, :], in_=us[0:64, 6, :], func=AF.Tanh), [("usD", 6)], ["twD"])
            yield
            A(P, lambda e: e.activation(out=sgd[:, :], in_=us[:, 7, :], func=AF.Sigmoid), [("usD", 7)], ["sgdD"])
            yield
            for c in range(2):
                cs = slice(c * 128, (c + 1) * 128)
                kR, kK, kV = ("usD", c), ("usD", 2 + c), ("usD", 4 + c)
                r_, k_, v_ = us[:, c, :], us[:, 2 + c, :], us[:, 4 + c, :]
                PE(P, lambda e: e.matmul(pb[0][:, :], wup[0:64, cs], tw[0:64, :], start=True, stop=True),
                   ["wupD", "twD"], [PB(0)])
                yield
                A(P, lambda e: e.activation(out=dec[c][:, :], in_=pb[0][:, :], func=AF.Sigmoid, bias=w0[:, c:c + 1]),
                  [PB(0), "rpD"], [("decD", c)])
                yield
                A(P, lambda e: e.activation(out=dec[c][:, :], in_=dec[c][:, :], func=AF.Exp, scale=-EM05),
                  [("decD", c)], [("decD", c)])
                yield
                PE(P, lambda e: e.matmul(pb[1][:, :], wup[64:128, cs], us[64:128, 6, :], start=True, stop=True),
                   ["wupD", ("usD", 6)], [PB(1)])
                yield
                A(P, lambda e: e.activation(out=asg[c][:, :], in_=pb[1][:, :], func=AF.Sigmoid, bias=a0[:, c:c + 1]),
                  [PB(1), "rpD"], [("asgD", c)])
                yield
                PE(P, lambda e: e.matmul(pb[2][:, :], gup[:, cs], sgd[:, :], start=True, stop=True),
                   ["gupD", "sgdD"], [PB(2)])
                yield
                A(P, lambda e: e.copy(out=gtc[p][c][:, :], in_=pb[2][:, :]), [PB(2)], [("gtcD", c, p)])
                yield
                V(P, lambda e: e.tensor_scalar(out=kk[c][:, :], in0=k_, scalar1=k_k[:, c:c + 1], scalar2=None,
                                               op0=ALU.mult), [kK, "rpD"], [("kkD", c)])
                yield
                G(P, lambda e: e.tensor_tensor(out=kk2[c][:, :], in0=kk[c][:, :], in1=kk[c][:, :], op=ALU.mult),
                  [("kkD", c)], [("kk2D", c)])
                yield
                PE(P, lambda e: e.matmul(pb[3][:, :], bd_ones, kk2[c][:, :], start=True, stop=True),
                   [("kk2D", c), "cst"], [PB(3)])
                yield
                V(P, lambda e: e.tensor_scalar(out=rn[c][:, :], in0=pb[3][:, :], scalar1=1e-24, scalar2=None,
                                               op0=ALU.max), [PB(3)], [("rnD", c)])
                yield
                A(P, lambda e: e.activation(out=rn[c][:, :], in_=rn[c][:, :], func=AF.Sqrt), [("rnD", c)], [("rnD", c)])
                yield
                V(P, lambda e: e.reciprocal(out=rn[c][:, :], in_=rn[c][:, :]), [("rnD", c)], [("rnD", c)])
                yield
                V(P, lambda e: e.tensor_tensor(out=kkn[c][:, :], in0=kk[c][:, :], in1=rn[c][:, :], op=ALU.mult),
                  [("kkD", c), ("rnD", c)], [("kknD", c)])
                yield
                V(P, lambda e: e.tensor_scalar(out=rn[c][:, :], in0=asg[c][:, :], scalar1=k_a[:, c:c + 1],
                                               scalar2=omka[:, c:c + 1], op0=ALU.mult, op1=ALU.add),
                  [("asgD", c), "rpD", "omkaD"], [("rnD", c)])
                yield
                V(P, lambda e: e.tensor_tensor(out=km[c][:, :], in0=k_, in1=rn[c][:, :], op=ALU.mult),
                  [kK, ("rnD", c)], [("kmD", c)])
                yield
                G(P, lambda e: e.tensor_tensor(out=bv[c][:, :], in0=kkn[c][:, :], in1=asg[c][:, :], op=ALU.mult),
                  [("kknD", c), ("asgD", c)], [("bvD", c)])
                yield
                for j in range(4):
                    tk = slice(j * 128, (j + 1) * 128)
                    V(P, lambda e, tk=tk: e.tensor_tensor_scan(out=Gi[p][c][:, tk], data0=dec[c][:, tk],
                                                               data1=zero_t[:, 0:128], initial=1.0, op0=ALU.mult,
                                                               op1=ALU.add), [("decD", c), "zero"], [("GiD", c, p)])
                    yield
                V(P, lambda e: e.reciprocal(out=dec[c][:, :], in_=Gi[p][c][:, :]), [("GiD", c, p)], [("decD", c)])
                yield
                V(P, lambda e: e.tensor_copy(out=v4(Ge[c])[:, :, 1:128], in_=v4(Gi[p][c])[:, :, 0:127]), [("GiD", c, p)],
                  [("GeD", c)])
                yield
                V(P, lambda e: e.memset(v4(Ge[c])[:, :, 0:1], 1.0), [], [("GeD", c)])
                yield
                V(P, lambda e: e.scalar_tensor_tensor(out=ar[p][c][:, :, 0:128], in0=v4(kkn[c]), scalar=-1.0,
                                                      in1=v4(Ge[c]), op0=ALU.mult, op1=ALU.mult),
                  [("kknD", c), ("GeD", c)], [("arD", c, p)])
                yield
                V(P, lambda e: e.tensor_tensor(out=ar[p][c][:, :, 128:256], in0=r_.rearrange("p (j t) -> p j t", j=4),
                                               in1=v4(Gi[p][c]), op=ALU.mult), [kR, ("GiD", c, p)], [("arD", c, p)])
                yield
                G(P, lambda e: e.tensor_tensor(out=bt[p][c][:, :], in0=bv[c][:, :], in1=dec[c][:, :], op=ALU.mult),
                  [("bvD", c), ("decD", c)], [("btD", c, p)])
                yield
                G(P, lambda e: e.tensor_tensor(out=kt[p][c][:, :], in0=km[c][:, :], in1=dec[c][:, :], op=ALU.mult),
                  [("kmD", c), ("decD", c)], [("ktD", c, p)])
                yield
                V(P, lambda e: e.scalar_tensor_tensor(out=kk2[c][:, :], in0=r_, scalar=r_k[:, c:c + 1], in1=km[c][:, :],
                                                      op0=ALU.mult, op1=ALU.mult), [kR, ("kmD", c), "rpD"], [("kk2D", c)])
                yield
                PE(P, lambda e: e.matmul(pb[4][:, :], bd_ones, kk2[c][:, :], start=True, stop=True),
                   [("kk2D", c), "cst"], [PB(4)])
                yield
                V(P, lambda e: e.tensor_tensor(out=bon[p][c][:, :], in0=pb[4][:, :], in1=v_, op=ALU.mult),
                  [PB(4), kV], [("bonD", c, p)])
                yield
                for bi, (src, skey, dst, dkey) in enumerate(((bt[p][c], ("btD", c, p), btT[p][c], ("btTD", c, p)),
                                                             (kt[p][c], ("ktD", c, p), ktT[p][c], ("ktTD", c, p)),
                                                             (v_, kV, vT[p][c], ("vTD", c, p)))):
                    bank = 5 + bi

                    def fn(e, src=src, bank=bank):
                        ins = None
                        for j in range(4):
                            ins = e.transpose(pb[bank][:, j * 128:(j + 1) * 128], src[:, j * 128:(j + 1) * 128], ident)
                        return ins
                    PE(P, fn, [skey, "cst"], [PB(bank)])
                    yield
                    if bi % 2 == 0:
                        A(P, lambda e, dst=dst, bank=bank: e.copy(out=dst[:, :, :],
                                                                  in_=pb[bank][:, :].rearrange("p (j t) -> p j t", j=4)),
                          [PB(bank)], [dkey])
                        yield
                    else:
                        V(P, lambda e, dst=dst, bank=bank: e.tensor_copy(
                            out=dst[:, :, :], in_=pb[bank][:, :].rearrange("p (j t) -> p j t", j=4)), [PB(bank)], [dkey])
                        yield


        def exhaust(g):
            if g is not None:
                for _ in g:
                    pass

        exhaust(prep_gen(0))
        for n in range(NT):
            p = n % 2
            pgen = [prep_gen(n + 1) if n + 1 < NT else None]

            def adv(k, site='a'):
                if pgen[0] is None or site not in os.environ.get('RW_ADV', 'ac'):
                    return
                for _ in range(k):
                    try:
                        next(pgen[0])
                    except StopIteration:
                        pgen[0] = None
                        return
            for j in range(4 if RW_STOP != 'prep' else 0):
                g = n * 4 + j
                cur, nxt = g % 2, (g + 1) % 2
                tk = slice(j * 128, (j + 1) * 128)
                heads = [(c, hh) for c in range(2) for hh in range(2)]
                for (c, hh) in heads:
                    q = c * 2 + hh
                    sl = slice(64 * hh, 64 * hh + 64)
                    PE(P, lambda e, c=c, sl=sl, q=q: e.matmul(pb[q][:, 0:256], bt[p][c][sl, tk], ar[p][c][sl, j, :],
                                                              start=True, stop=True), [("btD", c, p), ("arD", c, p)], [PB(q)])
                    PE(P, lambda e, c=c, sl=sl, q=q: e.matmul(pb[q][:, 256:512], kt[p][c][sl, tk], ar[p][c][sl, j, :],
                                                              start=True, stop=True), [("ktD", c, p), ("arD", c, p)], [PB(q)])
                    V(P, lambda e, q=q: e.tensor_tensor(out=Mm[q][:, :], in0=pb[q][:, :], in1=m_row, op=ALU.mult),
                      [PB(q), "cst"], [("MmD", q)])
                    PE(P, lambda e, c=c, sl=sl, q=q: e.matmul(pb[q][:, 0:128], ar[p][c][sl, j, 0:128], bt[p][c][sl, tk],
                                                              start=True, stop=True), [("btD", c, p), ("arD", c, p)], [PB(q)])
                    V(P, lambda e, q=q: e.tensor_tensor(out=PT0[q][:, :], in0=pb[q][:, 0:128], in1=m_sl, op=ALU.mult),
                      [PB(q), "cst"], [("PT0D", q)])
                    G(P, lambda e, q=q: e.tensor_tensor(out=Xb[q][0][:, :], in0=Mm[q][:, 0:128], in1=ident, op=ALU.add),
                      [("MmD", q), "cst"], [("XD", q, 0)])
                adv(13, 'a')
                for lev in range(6 if RW_STOP != 'M' else 0):
                    adv(3, 'b')
                    for (c, hh) in heads:
                        q = c * 2 + hh
                        curP = Mm[q][:, 0:128] if lev == 0 else Pmb[q][(lev - 1) % 2][:, :]
                        curPk = ("MmD", q) if lev == 0 else ("PmD", q, (lev - 1) % 2)
                        curPT = PT0[q][:, :] if lev == 0 else PTb[q][(lev - 1) % 2][:, :]
                        curPTk = ("PT0D", q) if lev == 0 else ("PTD", q, (lev - 1) % 2)
                        nPT, nPTk = PTb[q][lev % 2], ("PTD", q, lev % 2)
                        nP, nPk = Pmb[q][lev % 2], ("PmD", q, lev % 2)
                        cX, cXk = Xb[q][lev % 2], ("XD", q, lev % 2)
                        if lev < 5:
                            nX, nXk = Xb[q][(lev + 1) % 2], ("XD", q, (lev + 1) % 2)
                        else:
                            nX, nXk = XF[q], ("XFD", q)
                        R32 = (lambda ap: ap) if lev == 0 else R32g
                        if lev < 5:
                            PE(P, lambda e, q=q, curP=curP, curPT=curPT: e.matmul(pb[q][:, 0:128], R32(curPT), R32(curP),
                                                                                  start=True, stop=True),
                               [curPk, curPTk], [PB(q)])
                        PE(P, lambda e, q=q, curP=curP, curPT=curPT: e.matmul(pb[4 + q][:, 0:128], R32(curP), R32(curPT),
                                                                              start=True, stop=True),
                           [curPk, curPTk], [PB(4 + q)])
                        if lev < 5:
                            V(P, lambda e, q=q, nP=nP: e.tensor_copy(out=nP[:, :], in_=pb[q][:, 0:128]), [PB(q)], [nPk])
                        A(P, lambda e, q=q, nPT=nPT: e.copy(out=nPT[:, :], in_=pb[4 + q][:, 0:128]), [PB(4 + q)], [nPTk])
                        PE(P, lambda e, q=q, nPT=nPT, cX=cX: e.matmul(pb[q][:, 256:384], R32g(nPT[:, :]), R32g(cX[:, :]),
                                                                      start=True, stop=True), [nPTk, cXk], [PB(q)])
                        V(P, lambda e, q=q, cX=cX, nX=nX: e.tensor_tensor(out=nX[:, :], in0=pb[q][:, 256:384],
                                                                          in1=cX[:, :], op=ALU.add),
                          [PB(q), cXk], [nXk])
                if RW_STOP in ('inv', 'M'):
                    continue
                adv(13, 'c')
                for (c, hh) in heads:
                    q = c * 2 + hh
                    sl = slice(64 * hh, 64 * hh + 64)
                    bC = 4 + q
                    S0, S0k = ST[c][cur][sl, :], ("ST", c, cur, hh)
                    mm_group(P, pb[bC][:, 0:64], [(ar[p][c][sl, j, 0:128], S0), (Mm[q][:, 256:384], vT[p][c][:, j, sl])],
                             [("arD", c, p), S0k, ("MmD", q), ("vTD", c, p)], [PB(bC)])
                    V(P, lambda e, q=q, bC=bC: e.tensor_copy(out=Wsb[q][:, :], in_=pb[bC][:, 0:64]), [PB(bC)],
                      [("WsbD", q)])
                for (c, hh) in heads:
                    q = c * 2 + hh
                    bC = 4 + q
                    PE(P, lambda e, q=q, bC=bC: e.matmul(pb[bC][:, 64:128], XF[q][:, :], Wsb[q][:, :], start=True,
                                                         stop=True), [("XFD", q), ("WsbD", q)], [PB(bC)])
                    A(P, lambda e, q=q, bC=bC: e.copy(out=Usb[q][:, :], in_=pb[bC][:, 64:128]), [PB(bC)], [("UsbD", q)])
                for (c, hh) in heads:
                    q = c * 2 + hh
                    sl = slice(64 * hh, 64 * hh + 64)
                    bC = 4 + q
                    S0, S0k = ST[c][cur][sl, :], ("ST", c, cur, hh)
                    mm_group(P, pb[bC][sl, 128:192], [(ident[sl, sl], S0), (btT[p][c][:, j, sl], Usb[q][:, :]),
                                                      (ktT[p][c][:, j, sl], vT[p][c][:, j, sl])],
                             [S0k, ("btTD", c, p), ("UsbD", q), ("ktTD", c, p), ("vTD", c, p), "cst"], [PB(bC)])
                    A(P, lambda e, c=c, sl=sl, bC=bC: e.activation(
                        out=ST[c][nxt][sl, :], in_=pb[bC][sl, 128:192], func=AF.Identity,
                        scale=Gi[p][c][sl, j * 128 + 127:j * 128 + 128]), [PB(bC), ("GiD", c, p)], [("ST", c, nxt, hh)])
                    mm_group(P, pb[q][sl, 256:384], [(S0, ar[p][c][sl, j, 128:256]), (Usb[q][:, :], Mm[q][:, 128:256]),
                                                     (vT[p][c][:, j, sl], Mm[q][:, 384:512])],
                             [S0k, ("arD", c, p), ("UsbD", q), ("MmD", q), ("vTD", c, p)], [PB(q)])
                    V(P, lambda e, c=c, sl=sl, q=q: e.tensor_copy(out=ysb[c][sl, tk], in_=pb[q][sl, 256:384]),
                      [PB(q)], [("ysbD", c)])
            exhaust(pgen[0])
            for c in range(2):
                b1, b2 = c * 2, c * 2 + 1
                PE(P, lambda e, b1=b1: e.matmul(pb[b1][:, :], bd_ones, ysb[c][:, :], start=True, stop=True),
                   [("ysbD", c), "cst"], [PB(b1)])
                V(P, lambda e, b1=b1: e.scalar_tensor_tensor(out=kk[c][:, :], in0=pb[b1][:, :], scalar=-1.0 / 64,
                                                             in1=ysb[c][:, :], op0=ALU.mult, op1=ALU.add),
                  [PB(b1), ("ysbD", c)], [("kkD", c)])
                A(P, lambda e: e.activation(out=kkn[c][:, :], in_=kk[c][:, :], func=AF.Square), [("kkD", c)],
                  [("kknD", c)])
                PE(P, lambda e, b2=b2: e.matmul(pb[b2][:, :], bd_ones, kkn[c][:, :], start=True, stop=True),
                   [("kknD", c), "cst"], [PB(b2)])
                A(P, lambda e, b2=b2: e.activation(out=kkn[c][:, :], in_=pb[b2][:, :], func=AF.Sqrt, scale=1.0 / 64,
                                                   bias=GN_EPS), [PB(b2)], [("kknD", c)])
                V(P, lambda e: e.reciprocal(out=kkn[c][:, :], in_=kkn[c][:, :]), [("kknD", c)], [("kknD", c)])
                V(P, lambda e: e.scalar_tensor_tensor(out=kk[c][:, :], in0=kk[c][:, :], scalar=ln_g[:, c:c + 1],
                                                      in1=kkn[c][:, :], op0=ALU.mult, op1=ALU.mult),
                  [("kkD", c), ("kknD", c), "rpD"], [("kkD", c)])
                V(P, lambda e: e.scalar_tensor_tensor(out=kk[c][:, :], in0=kk[c][:, :], scalar=ln_b[:, c:c + 1],
                                                      in1=bon[p][c][:, :], op0=ALU.add, op1=ALU.add),
                  [("kkD", c), ("bonD", c, p), "rpD"], [("kkD", c)])
                V(P, lambda e: e.tensor_tensor(out=yo[c][:, :], in0=kk[c][:, :], in1=gtc[p][c][:, :], op=ALU.mult),
                  [("kkD", c), ("gtcD", c, p)], [("yoD", c)])
                P.dma("sync", mixS[768 + c * 128:768 + (c + 1) * 128, n * 512:(n + 1) * 512], yo[c][:, :],
                      reads=[("yoD", c)], writes=[("mixS", "r", c, n)])
        P.barrier()


def ffn_phase(nc, P, l, E):
    sb, pb, PB, xS, rr, ring = E["sb"], E["pb"], E["PB"], E["xS"], E["rr"], E["ring"]
    gm2, sh2, gt2 = E["gm2"], E["sh2"], E["gt2"]
    rms_h, load_cast, ident, cstt = E["rms_h"], E["load_cast"], E["ident"], E["cstt"]
    is_moe = (l % 2 == 1)
    li = l // 2
    TB = 1024
    NTB = T // TB
    if is_moe:
        experts = [(E["moe_g"][li, e], E["moe_u"][li, e], E["moe_d"][li, e]) for e in range(NE)]
        ff = MFF
    else:
        experts = [(E["ffn_g"][li], E["ffn_u"][li], E["ffn_d"][li])]
        ff = DFF
    nfg = ff // 256
    with contextlib.ExitStack() as st:
        xacc = sb("xacc", [128, 8, TB], stack=st)
        h2 = sb("h2", [128, 8, TB], BF16, stack=st)
        tmp = {"sq": sb("sqF", [128, 8, 512], BF16, stack=st), "rstd": sb("rstdF", [128, 512], stack=st),
               "t2": [sb(f"t2F{i}", [128, 512], stack=st) for i in range(2)]}
        stg = [sb(f"stgF{i}", [128, 2048], stack=st) for i in range(4)]
        wg = [sb(f"wgF{i}", [128, 8, 256], BF16, stack=st) for i in range(2)]
        wu = [sb(f"wuF{i}", [128, 8, 256], BF16, stack=st) for i in range(2)]
        wd = [sb(f"wdF{i}", [128, 2, 1024], BF16, stack=st) for i in range(3)]
        sg = [sb(f"sgF{i}", [128, 512], stack=st) for i in range(2)]
        a_ = [sb(f"aF{i}", [128, 2, 512], BF16, stack=st) for i in range(3)]
        if is_moe:
            hf = sb("hfF", [128, 8, 512], stack=st)
            rt = sb("rtF", [128, 64], stack=st)
            P.dma("sync", rt[:, :], E["moe_r"][li], writes=["rtF"])
            lg = sb("lgF", [128, 4, 8], stack=st)
            mx8 = sb("mx8F", [128, 4, 8], stack=st)
            ngm = sb("ngmF", [128, 4], stack=st)
            ex = sb("exF", [128, 4, 8], stack=st)
            msk = sb("mskF", [128, 4, 8], stack=st)
            den = sb("denF", [128, 4], stack=st)
            gate = sb("gateF", [128, 4, 8], stack=st)
            gT = sb("gTF", [8, TB], stack=st)
            gbc = [sb(f"gbcF{i}", [128, TB], stack=st) for i in range(2)]
            tt = [sb(f"ttF{i}", [128, 512], stack=st) for i in range(2)]
        cnt = 0
        pending = []
        for tb in range(NTB):
            t0 = tb * TB
            for k in range(8):
                for half in range(2):
                    P.dma("sync" if k % 2 == 0 else "gpsimd", xacc[:, k, half * 512:(half + 1) * 512],
                          xS[k * 128:(k + 1) * 128, t0 + half * 512:t0 + (half + 1) * 512],
                          writes=[("xacc", k, half)])
            for half in range(2):
                hs = slice(half * 512, (half + 1) * 512)
                xk = [("xacc", k, half) for k in range(8)]
                if not is_moe:
                    rms_h(xacc[:, :, hs], xk, h2[:, :, hs], ("h2", half), gm2, sh2, tmp)
                else:
                    rms_h(xacc[:, :, hs], xk, h2[:, :, hs], ("h2", half), gm2, sh2, tmp, hf=hf, hfkey="hfF")
                    for jb in range(4):
                        mm_group(P, pb[6][:, jb * 8:(jb + 1) * 8],
                                 [(hf[:, k, jb * 128:(jb + 1) * 128], rt[:, k * 8:(k + 1) * 8]) for k in range(8)],
                                 ["hfF", "rtF"], [PB(6)])
                    V(P, lambda e: e.tensor_copy(out=lg[:, :, :], in_=pb[6][:, 0:32].rearrange("p (j e) -> p j e", j=4)),
                      [PB(6)], ["lgF"])
                    for jb in range(4):
                        V(P, lambda e, jb=jb: e.max(out=mx8[:, jb, :], in_=lg[:, jb, :]), ["lgF"], ["mx8F"])
                    V(P, lambda e: e.tensor_scalar(out=ngm[:, :], in0=mx8[:, :, 0], scalar1=-1.0, scalar2=None,
                                                   op0=ALU.mult), ["mx8F"], ["ngmF"])
                    for jb in range(4):
                        A(P, lambda e, jb=jb: e.activation(out=ex[:, jb, :], in_=lg[:, jb, :], func=AF.Exp,
                                                           bias=ngm[:, jb:jb + 1]), ["lgF", "ngmF"], ["exF"])
                        V(P, lambda e, jb=jb: e.tensor_scalar(out=msk[:, jb, :], in0=lg[:, jb, :],
                                                              scalar1=mx8[:, jb, 1:2], scalar2=None, op0=ALU.is_ge),
                          ["lgF", "mx8F"], ["mskF"])
                    V(P, lambda e: e.tensor_tensor(out=ex[:, :, :], in0=ex[:, :, :], in1=msk[:, :, :], op=ALU.mult),
                      ["exF", "mskF"], ["exF"])
                    V(P, lambda e: e.tensor_reduce(out=den[:, :], in_=ex[:, :, :], axis=mybir.AxisListType.X,
                                                   op=ALU.add), ["exF"], ["denF"])
                    V(P, lambda e: e.reciprocal(out=den[:, :], in_=den[:, :]), ["denF"], ["denF"])
                    for jb in range(4):
                        V(P, lambda e, jb=jb: e.tensor_scalar(out=gate[:, jb, :], in0=ex[:, jb, :],
                                                              scalar1=den[:, jb:jb + 1], scalar2=None, op0=ALU.mult),
                          ["exF", "denF"], ["gateF"])
                    for jb in range(4):
                        PE(P, lambda e, jb=jb: e.transpose(pb[7][0:8, jb * 128:(jb + 1) * 128], gate[:, jb, :], ident),
                           ["gateF", "cst"], [PB(7)])
                    A(P, lambda e, hs=hs: e.copy(out=gT[:, hs], in_=pb[7][0:8, :]), [PB(7)], ["gTF"])
            for e_i, (Wg, Wu, Wd) in enumerate(experts):
                if is_moe:
                    gb = gbc[e_i % 2]
                    gk = ("gbcF", e_i % 2)
                    for half in range(2):
                        hs = slice(half * 512, (half + 1) * 512)
                        PE(P, lambda e, hs=hs, e_i=e_i: e.matmul(
                            pb[7][:, :], cstt[0:8, C_SEL8 + e_i * 128:C_SEL8 + (e_i + 1) * 128], gT[:, hs],
                            start=True, stop=True), ["gTF", "cst"], [PB(7)])
                        A(P, lambda e, hs=hs, gb=gb: e.copy(out=gb[:, hs], in_=pb[7][:, :]), [PB(7)], [gk])
                for fg in range(nfg):
                    i = rr("wF", 2)
                    fs = slice(fg * 256, (fg + 1) * 256)
                    load_cast(wg[i], ("wgF", i), Wg[:, fs], 8, 256, stg, "stgF", cast_eng="scalar")
                    load_cast(wu[i], ("wuF", i), Wu[:, fs], 8, 256, stg, "stgF", cast_eng="scalar")
                    i3 = rr("wdF3", 3)
                    load_cast(wd[i3], ("wdF", i3), Wd[fs, :], 2, 1024, stg, "stgF", cast_eng="scalar")
                    for half in range(2):
                        hs = slice(half * 512, (half + 1) * 512)
                        ai = rr("aF", 3)
                        for j in range(2):
                            bg = (cnt % 2) * 2
                            cnt += 1
                            js = slice(j * 128, (j + 1) * 128)
                            mm_group(P, pb[bg][:, :], [(wg[i][:, k, js], h2[:, k, hs]) for k in range(8)],
                                     [("wgF", i), ("h2", half)], [PB(bg)])
                            mm_group(P, pb[bg + 1][:, :], [(wu[i][:, k, js], h2[:, k, hs]) for k in range(8)],
                                     [("wuF", i), ("h2", half)], [PB(bg + 1)])
                            si = rr("sgF", 2)
                            A(P, lambda e, si=si, bg=bg: e.activation(out=sg[si][:, :], in_=pb[bg][:, :], func=AF.Silu),
                              [PB(bg)], [("sgF", si)])
                            if not is_moe:
                                V(P, lambda e, si=si, bg=bg, ai=ai, j=j: e.tensor_tensor(
                                    out=a_[ai][:, j, :], in0=sg[si][:, :], in1=pb[bg + 1][:, :], op=ALU.mult),
                                  [("sgF", si), PB(bg + 1)], [("aF", ai, j)])
                            else:
                                ti = rr("ttF", 2)
                                V(P, lambda e, si=si, bg=bg, ti=ti: e.tensor_tensor(
                                    out=tt[ti][:, :], in0=sg[si][:, :], in1=pb[bg + 1][:, :], op=ALU.mult),
                                  [("sgF", si), PB(bg + 1)], [("ttF", ti)])
                                G(P, lambda e, ti=ti, ai=ai, j=j, hs=hs, gb=gb: e.tensor_tensor(
                                    out=a_[ai][:, j, :], in0=tt[ti][:, :], in1=gb[:, hs], op=ALU.mult),
                                  [("ttF", ti), gk], [("aF", ai, j)])
                            if pending:
                                pending[0](range(4 * j, 4 * j + 4))
                                if j == 1:
                                    pending.pop()
                        def down(ms, i=i3, ai=ai, hs=hs, half=half):
                            for m in ms:
                                bd = 4 + (m % 4)
                                mm_group(P, pb[bd][:, :], [(wd[i][:, j, m * 128:(m + 1) * 128], a_[ai][:, j, :])
                                                           for j in range(2)],
                                         [("wdF", i), ("aF", ai, 0), ("aF", ai, 1)], [PB(bd)])
                                V(P, lambda e, m=m, bd=bd, hs=hs: e.scalar_tensor_tensor(
                                    out=xacc[:, m, hs], in0=pb[bd][:, :], scalar=gt2[:, m:m + 1], in1=xacc[:, m, hs],
                                    op0=ALU.mult, op1=ALU.add), [PB(bd), ("xacc", m, half), "mod"], [("xacc", m, half)])
                        pending.append(down)
            if pending:
                pending.pop()(range(8))
            for k in range(8):
                for half in range(2):
                    P.dma("sync" if k % 2 == 0 else "gpsimd",
                          xS[k * 128:(k + 1) * 128, t0 + half * 512:t0 + (half + 1) * 512],
                          xacc[:, k, half * 512:(half + 1) * 512], reads=[("xacc", k, half)],
                          writes=[("xS", k, tb, half)])
        P.barrier()


F_KEYS = ("ffn_g", "ffn_u", "ffn_d", "moe_r", "moe_g", "moe_u", "moe_d")


def kernel(**I):
    shared = prep_shared(I)
    zshared = {k: np.zeros_like(v) for k, v in shared.items()}
    nc = build()
    active = [0, 1, 4, 5]
    in_maps = []
    for c in range(8):
        if c in active:
            in_maps.append(dict(shared, **prep_core(I, active.index(c))))
        else:
            z = prep_core(I, 0)
            in_maps.append(dict(zshared, **{k: np.zeros_like(v) for k, v in z.items()}))
    res = run_bass_kernel_spmd(nc, in_maps, core_ids=list(range(8)))
    out = np.stack([np.asarray(res.results[c]["outT"]).T for c in active])
    return np.ascontiguousarray(out, np.float32)
```

```python
import os
import contextlib
import numpy as np
import concourse.bass as bass
import concourse.mybir as mybir
from concourse.bass_utils import run_bass_kernel_spmd

F32 = mybir.dt.float32
BF16 = mybir.dt.bfloat16
I32 = mybir.dt.int32
AF = mybir.ActivationFunctionType
ALU = mybir.AluOpType

D = 1024
T = 4096
L = 4
TT = 512
NT = T // TT
DFF = 2816
MFF = 3584
NE = 8
NCH_IN = 16
RMS_EPS = 1e-6
LN_EPS = 1e-5
GN_EPS = 64e-5
SCALE_QK = 96.0 ** -0.5
N_CORES = 4

DBG = os.environ.get("KDBG", "")


class Prog:
    CE = ("vector", "scalar", "gpsimd", "tensor")

    def __init__(self, nc):
        self.nc = nc
        self.eng = {"vector": nc.vector, "scalar": nc.scalar, "gpsimd": nc.gpsimd,
                    "tensor": nc.tensor, "sync": nc.sync}
        self.sem = {e: nc.alloc_semaphore("p_" + e) for e in self.CE}
        self.cnt = {e: 0 for e in self.CE}
        self.dsem = {q: [nc.alloc_semaphore(f"d_{q}{i}") for i in range(8)] for q in ("sync", "gpsimd")}
        self.dcnt = {}
        self.drr = {"sync": 0, "gpsimd": 0}
        self.semobj = {}
        for e in self.CE:
            self.semobj[self.sem[e].name if hasattr(self.sem[e], "name") else e] = self.sem[e]
        self.seen = {e: {} for e in self.eng}
        self.lastw = {}
        self.readers = {}
        self.nops = 0

    def _need(self, eng, deps):
        for (sid, sem, val) in deps:
            if self.seen[eng].get(sid, 0) < val:
                self.eng[eng].wait_ge(sem, val)
                self.seen[eng][sid] = val
                self.nops += 1

    def _collect(self, reads, writes):
        deps = {}
        def add(d):
            if d is None:
                return
            sid, sem, val = d
            if sid not in deps or deps[sid][2] < val:
                deps[sid] = d
        for k in reads:
            add(self.lastw.get(k))
        for k in writes:
            add(self.lastw.get(k))
            for d in self.readers.get(k, {}).values():
                add(d)
        return list(deps.values())

    def _record(self, reads, writes, dep):
        sid = dep[0]
        for k in reads:
            r = self.readers.setdefault(k, {})
            r[sid] = dep
        for k in writes:
            self.lastw[k] = dep
            self.readers[k] = {}

    def op(self, eng, fn, reads=(), writes=()):
        deps = self._collect(reads, writes)
        if eng == "tensor":
            deps = [d for d in deps if d[0] != "tensor"]
        self._need(eng, deps)
        ins = fn(self.eng[eng])
        self.cnt[eng] += 1
        ins.then_inc(self.sem[eng], 1)
        self.nops += 1
        dep = (eng, self.sem[eng], self.cnt[eng])
        self.seen[eng][eng] = max(self.seen[eng].get(eng, 0), 0)
        self._record(reads, writes, dep)
        return dep

    def dma(self, q, out, in_, reads=(), writes=()):
        deps = self._collect(reads, writes)
        i = self.drr[q]
        self.drr[q] = (i + 1) % len(self.dsem[q])
        sem = self.dsem[q][i]
        sid = f"d_{q}{i}"
        n = self.dcnt.get(sid, 0)
        if n > 0:
            deps.append((sid, sem, 16 * n))
        self._need(q, deps)
        self.eng[q].dma_start(out=out, in_=in_).then_inc(sem, 16)
        self.nops += 1
        self.dcnt[sid] = n + 1
        dep = (sid, sem, 16 * (n + 1))
        self._record(reads, writes, dep)
        return dep

    def barrier(self):
        alld = [(e, self.sem[e], self.cnt[e]) for e in self.CE if self.cnt[e] > 0]
        for q in self.dsem:
            for i, sem in enumerate(self.dsem[q]):
                sid = f"d_{q}{i}"
                if self.dcnt.get(sid, 0) > 0:
                    alld.append((sid, sem, 16 * self.dcnt[sid]))
        for e in self.eng:
            self._need(e, [d for d in alld if d[0] != e])
        self.lastw = {}
        self.readers = {}


def V(P, fn, r=(), w=()):
    return P.op("vector", fn, r, w)


def A(P, fn, r=(), w=()):
    return P.op("scalar", fn, r, w)


def G(P, fn, r=(), w=()):
    return P.op("gpsimd", fn, r, w)


def PE(P, fn, r=(), w=()):
    return P.op("tensor", fn, r, w)


class Ctx:
    pass


def mm_group(P, out, pairs, r, w):
    def fn(e):
        ins = None
        n = len(pairs)
        for i, (l, rr) in enumerate(pairs):
            ins = e.matmul(out, l, rr, start=(i == 0), stop=(i == n - 1))
        return ins
    return PE(P, fn, r, w)


C_IDENT, C_ONES, C_BD, C_SU, C_IU, C_SL, C_MROW, C_RMAT, C_SEL65, C_INVF, C_SEL8, C_W = (
    0, 128, 256, 384, 512, 640, 768, 1280, 1376, 1440, 1472, 2496)


def make_consts():
    c = np.zeros((128, C_W), np.float32)
    r = np.arange(128)
    c[:, C_IDENT:C_IDENT + 128] = np.eye(128)
    c[:, C_ONES:C_ONES + 128] = 1.0
    bd = (r[:, None] // 64) == (r[None, :] // 64)
    c[:, C_BD:C_BD + 128] = bd
    su = r[:, None] < r[None, :]
    iu = r[:, None] <= r[None, :]
    c[:, C_SU:C_SU + 128] = su
    c[:, C_IU:C_IU + 128] = iu
    c[:, C_SL:C_SL + 128] = su.T
    c[:, C_MROW:C_MROW + 512] = np.concatenate([su, iu, su, iu], axis=1)
    rm = np.zeros((96, 96), np.float32)
    for i in range(16):
        rm[80 + i, 64 + i] = -1.0
        rm[64 + i, 80 + i] = 1.0
    c[0:96, C_RMAT:C_RMAT + 96] = rm
    c[64, C_SEL65:C_SEL65 + 64] = 1.0
    inv = (10000.0 ** (-np.arange(0, 32, 2, dtype=np.float32) / np.float32(32))).astype(np.float32)
    c[64:80, C_INVF] = inv
    c[80:96, C_INVF] = inv
    for e in range(8):
        c[e, C_SEL8 + e * 128:C_SEL8 + (e + 1) * 128] = 1.0
    return c


def pk(v, n):
    return np.ascontiguousarray(np.asarray(v, np.float32).reshape(n, 128).T)


def prep_shared(I):
    S = {}
    S["ada_w"] = np.ascontiguousarray(I["ada_w"], np.float32)
    S["ada_b"] = pk(I["ada_b"], 48)
    S["ada_lb"] = np.stack([pk(I["ada_layer_bias"][l], 48) for l in range(L)])
    S["n1g"] = np.stack([pk(I["norm1_g"][l], 8) for l in range(L)])
    S["n2g"] = np.stack([pk(I["norm2_g"][l], 8) for l in range(L)])
    w = I["w_in"]
    wi = np.zeros((L, D, NCH_IN * 128), np.float32)
    wi[:, :, 0:512] = w[:, :, 0:512]
    wi[:, :, 512:768] = w[:, :, 512:768]
    wi[:, :, 768:896] = w[:, :, 768:896]
    wi[:, :, 896 + 64:896 + 96] = w[:, :, 896:928]
    rb = 928
    wi[:, :, 1024:1792] = w[:, :, rb:rb + 768]
    wi[:, :, 1792:1920] = w[:, :, rb + 768:rb + 896]
    wi[:, :, 1920:2048] = w[:, :, rb + 896:rb + 1024]
    S["w_in"] = wi
    S["w_out"] = np.ascontiguousarray(I["w_out"], np.float32)
    S["conv_w"] = np.ascontiguousarray(I["conv_w"].reshape(L, 31, 2, 128).transpose(0, 2, 3, 1), np.float32)
    S["conv_p"] = np.stack([np.concatenate([pk(I["conv_b"][l], 2), pk(I["conv_ln_g"][l], 2),
                                            pk(I["conv_ln_b"][l], 2)], axis=1) for l in range(L)])
    mp = np.zeros((L, 128, 5), np.float32)
    for l in range(L):
        mp[l, :, 0:2] = pk(I["mla_q_norm_g"][l], 2)
        mp[l, :, 2] = I["mla_kv_norm_g"][l]
        mp[l, 0:96, 3] = I["qk_norm_q"][l]
        mp[l, 0:96, 4] = I["qk_norm_k"][l]
    S["mla_p"] = mp
    S["w_uq"] = np.ascontiguousarray(I["mla_w_uq"], np.float32)
    wkv = I["mla_w_ukv"].reshape(L, 128, 8, 128)
    S["w_k"] = np.ascontiguousarray(wkv[:, :, :, 0:64].reshape(L, 128, 512), np.float32)
    S["w_v"] = np.ascontiguousarray(wkv[:, :, :, 64:128].reshape(L, 128, 512), np.float32)
    S["rw_mu"] = np.stack([pk(I["rwkv_mu"][l], 8) for l in range(L)])
    S["rw_p"] = np.stack([np.concatenate([pk(I[k][l].reshape(-1), 2) for k in
                                          ("rwkv_w0", "rwkv_a0", "rwkv_k_k", "rwkv_k_a", "rwkv_r_k",
                                           "rwkv_ln_g", "rwkv_ln_b")], axis=1) for l in range(L)])
    S["rw_wup"] = np.ascontiguousarray(np.concatenate([I["rwkv_w_up"], I["rwkv_a_up"]], axis=1), np.float32)
    S["rw_gup"] = np.ascontiguousarray(I["rwkv_g_up"], np.float32)
    S["ffn_g"] = np.ascontiguousarray(I["ffn_w_gate"], np.float32)
    S["ffn_u"] = np.ascontiguousarray(I["ffn_w_up"], np.float32)
    S["ffn_d"] = np.ascontiguousarray(I["ffn_w_down"], np.float32)
    S["moe_r"] = np.ascontiguousarray(I["moe_router"].reshape(2, 8, 128, 8).transpose(0, 2, 1, 3).reshape(2, 128, 64),
                                      np.float32)
    S["moe_g"] = np.ascontiguousarray(I["moe_w_gate"], np.float32)
    S["moe_u"] = np.ascontiguousarray(I["moe_w_up"], np.float32)
    S["moe_d"] = np.ascontiguousarray(I["moe_w_down"], np.float32)
    S["cst"] = make_consts()
    return S


def prep_core(I, b):
    return {"xT": np.ascontiguousarray(I["x"][b].T, np.float32),
            "c_pk": pk(I["c"][b], 8),
            "pos": np.ascontiguousarray(I["positions"][b].reshape(1, T), np.int32)}


def build(n_layers=L, phases="ABCDEF", dbg_out=()):
    nc = bass.Bass("TRN2", target_bir_lowering=False)
    P = Prog(nc)

    def din(name, shape, dt=F32):
        return nc.dram_tensor(name, list(shape), dt, kind="ExternalInput").ap()

    def dscr(name, shape, dt=F32):
        kind = "ExternalOutput" if name in dbg_out else "Internal"
        return nc.dram_tensor(name, list(shape), dt, kind=kind).ap()

    xT_in = din("xT", [D, T])
    c_in = din("c_pk", [128, 8])
    pos_in = din("pos", [1, T], I32)
    ada_w = din("ada_w", [D, 6 * D])
    ada_b = din("ada_b", [128, 48])
    ada_lb = din("ada_lb", [L, 128, 48])
    n1g = din("n1g", [L, 128, 8])
    n2g = din("n2g", [L, 128, 8])
    w_in = din("w_in", [L, D, NCH_IN * 128])
    w_out = din("w_out", [L, D, D])
    conv_w = din("conv_w", [L, 2, 128, 31])
    conv_p = din("conv_p", [L, 128, 6])
    mla_p = din("mla_p", [L, 128, 5])
    w_uq = din("w_uq", [L, 256, 768])
    w_k = din("w_k", [L, 128, 512])
    w_v = din("w_v", [L, 128, 512])
    rw_mu = din("rw_mu", [L, 128, 8])
    rw_p = din("rw_p", [L, 128, 14])
    rw_wup = din("rw_wup", [L, 128, 256])
    rw_gup = din("rw_gup", [L, 128, 256])
    if "F" in phases:
        ffn_g = din("ffn_g", [2, D, DFF])
        ffn_u = din("ffn_u", [2, D, DFF])
        ffn_d = din("ffn_d", [2, DFF, D])
        moe_r = din("moe_r", [2, 128, 64])
        moe_g = din("moe_g", [2, NE, D, MFF])
        moe_u = din("moe_u", [2, NE, D, MFF])
        moe_d = din("moe_d", [2, NE, MFF, D])
    cst = din("cst", [128, C_W])

    xS = nc.dram_tensor("outT", [D, T], F32, kind="ExternalOutput").ap()
    uS = dscr("uS", [NCH_IN * 128, T])
    mixS = dscr("mixS", [D, T], BF16)
    csS = dscr("csS", [2, 96, T])
    qS = dscr("qS", [8, 96, T], BF16)
    kS = dscr("kS", [8, 96, T], BF16)

    es = contextlib.ExitStack()

    uid = [0]

    def sb(name, shape, dt=F32, stack=None):
        uid[0] += 1
        return (stack or es).enter_context(nc.sbuf_tensor(f"{name}_{uid[0]}", list(shape), dt))

    pb = [es.enter_context(nc.psum_tensor(f"pb{i}", [128, 512], F32)) for i in range(8)]

    def PB(i):
        return ("pb", i)

    cstt = sb("cstt", [128, C_W])
    P.dma("sync", cstt[:, :], cst[:, :], writes=["cst"])
    ident = cstt[:, C_IDENT:C_IDENT + 128]
    ones_f = cstt[:, C_ONES:C_ONES + 128]
    bd_ones = cstt[:, C_BD:C_BD + 128]
    m_sl = cstt[:, C_SL:C_SL + 128]
    m_row = cstt[:, C_MROW:C_MROW + 512]
    rmat = cstt[0:96, C_RMAT:C_RMAT + 96]
    sel65 = cstt[0:65, C_SEL65:C_SEL65 + 64]
    invf = cstt[0:96, C_INVF:C_INVF + 1]
    cbf = sb("cbf", [128, 384], BF16)
    V(P, lambda e: e.tensor_copy(out=cbf[:, 0:256], in_=cstt[:, 0:256]), ["cst"], ["cbf"])
    V(P, lambda e: e.tensor_copy(out=cbf[:, 256:384], in_=cstt[:, C_IU:C_IU + 128]), ["cst"], ["cbf"])
    ident_b = cbf[:, 0:128]
    ones_b = cbf[:, 128:256]
    tri_b = cbf[:, 256:384]
    zero_t = sb("zero_t", [128, 128])
    V(P, lambda e: e.memset(zero_t[:, :], 0.0), [], ["zero"])

    mod = sb("mod", [128, L, 48])
    modp = sb("modp", [128, L, 16])

    with contextlib.ExitStack() as st:
        cpk = sb("cpk", [128, 8], stack=st)
        scs = sb("scs", [128, 8], stack=st)
        P.dma("sync", cpk[:, :], c_in[:, :], writes=["cpk"])
        A(P, lambda e: e.activation(out=scs[:, :], in_=cpk[:, :], func=AF.Silu), ["cpk"], ["scs"])
        aw = [sb(f"aw{i}", [128, 6 * D], stack=st) for i in range(2)]
        for k in range(8):
            t = aw[k % 2]
            kk = f"aw{k % 2}"
            for j in range(4):
                P.dma("sync" if j % 2 == 0 else "gpsimd", t[:, j * 1536:(j + 1) * 1536],
                      ada_w[k * 128:(k + 1) * 128, j * 1536:(j + 1) * 1536], writes=[kk])

            def fn(e, t=t, k=k):
                ins = None
                for m in range(48):
                    ins = e.matmul(pb[0][:, k * 48 + m:k * 48 + m + 1], t[:, m * 128:(m + 1) * 128],
                                   scs[:, k:k + 1], start=True, stop=True)
                return ins
            PE(P, fn, [kk, "scs"], [PB(0)])
        cond = sb("cond", [128, 48], stack=st)
        ab = sb("ab", [128, 48], stack=st)
        alb = sb("alb", [128, L, 48], stack=st)
        P.dma("sync", ab[:, :], ada_b[:, :], writes=["ab"])
        for l in range(L):
            P.dma("sync", alb[:, l, :], ada_lb[l], writes=["alb"])
        V(P, lambda e: e.tensor_tensor(out=cond[:, :], in0=pb[0][:, 0:48], in1=ab[:, :], op=ALU.add),
          [PB(0), "ab"], ["cond"])
        for k in range(1, 8):
            V(P, lambda e, k=k: e.tensor_tensor(out=cond[:, :], in0=pb[0][:, k * 48:(k + 1) * 48], in1=cond[:, :],
                                                op=ALU.add), [PB(0), "cond"], ["cond"])
        g1t = sb("g1t", [128, L, 8], stack=st)
        g2t = sb("g2t", [128, L, 8], stack=st)
        for l in range(L):
            P.dma("sync", g1t[:, l, :], n1g[l], writes=["g1t"])
            P.dma("sync", g2t[:, l, :], n2g[l], writes=["g2t"])
        for l in range(L):
            V(P, lambda e, l=l: e.tensor_tensor(out=mod[:, l, :], in0=cond[:, :], in1=alb[:, l, :], op=ALU.add),
              ["cond", "alb"], ["mod"])
            V(P, lambda e, l=l: e.scalar_tensor_tensor(out=modp[:, l, 0:8], in0=mod[:, l, 8:16], scalar=1.0,
                                                       in1=g1t[:, l, :], op0=ALU.add, op1=ALU.mult),
              ["mod", "g1t"], ["modp"])
            V(P, lambda e, l=l: e.scalar_tensor_tensor(out=modp[:, l, 8:16], in0=mod[:, l, 32:40], scalar=1.0,
                                                       in1=g2t[:, l, :], op0=ALU.add, op1=ALU.mult),
              ["mod", "g2t"], ["modp"])
        posi = sb("posi", [96, T], I32, stack=st)
        ang = sb("ang", [96, T], stack=st)
        tb = sb("tb", [96, T], stack=st)
        P.dma("sync", posi[:, :], pos_in[0:1, :].partition_broadcast(96), writes=["posi"])
        V(P, lambda e: e.tensor_copy(out=ang[:, :], in_=posi[:, :]), ["posi"], ["ang"])
        V(P, lambda e: e.tensor_scalar(out=ang[:, :], in0=ang[:, :], scalar1=invf, scalar2=None, op0=ALU.mult),
          ["ang", "cst"], ["ang"])
        TWO_PI = 2.0 * np.pi
        ki = sb("ki", [96, T], I32, stack=st)
        kf = sb("kf", [96, T], stack=st)
        for ci, shift in ((1, 0.0), (0, np.pi / 2)):
            V(P, lambda e, shift=shift: e.tensor_scalar(out=tb[:, :], in0=ang[:, :], scalar1=float(shift),
                                                        scalar2=float(1.0 / TWO_PI), op0=ALU.add, op1=ALU.mult),
              ["ang"], ["tb"])
            V(P, lambda e: e.tensor_copy(out=ki[:, :], in_=tb[:, :]), ["tb"], ["ki"])
            V(P, lambda e: e.tensor_copy(out=kf[:, :], in_=ki[:, :]), ["ki"], ["kf"])
            V(P, lambda e, shift=shift: e.tensor_scalar(out=tb[:, :], in0=ang[:, :], scalar1=float(shift),
                                                        scalar2=None, op0=ALU.add), ["ang"], ["tb"])
            V(P, lambda e: e.scalar_tensor_tensor(out=tb[:, :], in0=kf[:, :], scalar=float(-TWO_PI), in1=tb[:, :],
                                                  op0=ALU.mult, op1=ALU.add), ["kf", "tb"], ["tb"])
            V(P, lambda e: e.tensor_scalar(out=kf[:, :], in0=tb[:, :], scalar1=float(np.pi), scalar2=None,
                                           op0=ALU.is_gt), ["tb"], ["kf"])
            V(P, lambda e: e.scalar_tensor_tensor(out=tb[:, :], in0=kf[:, :], scalar=float(-TWO_PI), in1=tb[:, :],
                                                  op0=ALU.mult, op1=ALU.add), ["kf", "tb"], ["tb"])
            V(P, lambda e: e.tensor_scalar(out=kf[:, :], in0=tb[:, :], scalar1=float(-np.pi), scalar2=None,
                                           op0=ALU.is_lt), ["tb"], ["kf"])
            V(P, lambda e: e.scalar_tensor_tensor(out=tb[:, :], in0=kf[:, :], scalar=float(TWO_PI), in1=tb[:, :],
                                                  op0=ALU.mult, op1=ALU.add), ["kf", "tb"], ["tb"])
            V(P, lambda e: e.tensor_scalar(out=tb[:, :], in0=tb[:, :], scalar1=3.1415925, scalar2=-3.1415925,
                                           op0=ALU.min, op1=ALU.max), ["tb"], ["tb"])
            A(P, lambda e: e.activation(out=tb[:, :], in_=tb[:, :], func=AF.Sin), ["tb"], ["tb"])
            P.dma("sync", csS[ci], tb[:, :], reads=["tb"], writes=[("csS", ci)])
        xc = [sb(f"xc{i}", [128, T], stack=st) for i in range(2)]
        for k in range(8):
            t = xc[k % 2]
            q = "sync" if k % 2 == 0 else "gpsimd"
            P.dma(q, t[:, :], xT_in[k * 128:(k + 1) * 128, :], writes=[f"xc{k % 2}"])
            P.dma(q, xS[k * 128:(k + 1) * 128, :], t[:, :], reads=[f"xc{k % 2}"], writes=[("xS", k)])
        P.barrier()

    ring = {}

    def rr(name, n):
        i = ring.get(name, 0)
        ring[name] = i + 1
        return i % n

    def load_cast(dst3, dkey, src2d, nk, ncols, stg, stgname, cast_eng="gpsimd"):
        per = max(1, 2048 // ncols)
        k = 0
        while k < nk:
            kk = min(per, nk - k)
            i = rr(stgname, len(stg))
            s = stg[i]
            skey = (stgname, i)
            sv = s[:, 0:kk * ncols].rearrange("p (k c) -> p k c", k=kk)
            q = "sync" if (ring[stgname] % 2 == 0) else "gpsimd"
            P.dma(q, sv, src2d[k * 128:(k + kk) * 128, :].rearrange("(k p) c -> p k c", p=128), writes=[skey])
            eng = cast_eng if cast_eng != "alt" else ("gpsimd" if ring[stgname] % 2 == 0 else "vector")
            if eng == "scalar":
                A(P, lambda e, sv=sv, k=k, kk=kk: e.copy(out=dst3[:, k:k + kk, :], in_=sv), [skey], [dkey])
            else:
                P.op(eng, lambda e, sv=sv, k=k, kk=kk: e.tensor_copy(out=dst3[:, k:k + kk, :], in_=sv), [skey], [dkey])
            k += kk

    def rms_h(xt, xkey, hb, hkey, gm, sh, tmp, hf=None, hfkey=None):
        sq, rstd, t2 = tmp["sq"], tmp["rstd"], tmp["t2"]
        xkeys = xkey if isinstance(xkey, list) else [xkey] * 8
        A(P, lambda e: e.activation(out=sq[:, :, :], in_=xt, func=AF.Square), list(set(xkeys)), ["sq"])
        mm_group(P, pb[7][:, :], [(ones_b, sq[:, k, :]) for k in range(8)], ["sq", "cbf"], [PB(7)])
        A(P, lambda e: e.activation(out=rstd[:, :], in_=pb[7][:, :], func=AF.Sqrt, scale=1.0 / D, bias=RMS_EPS),
          [PB(7)], ["rstd"])
        V(P, lambda e: e.reciprocal(out=rstd[:, :], in_=rstd[:, :]), ["rstd"], ["rstd"])
        for k in range(8):
            i = rr("t2", 2)
            V(P, lambda e, k=k, i=i: e.scalar_tensor_tensor(out=t2[i][:, :], in0=xt[:, k, :], scalar=gm[:, k:k + 1],
                                                            in1=rstd[:, :], op0=ALU.mult, op1=ALU.mult),
              [xkeys[k], "rstd", "modp"], [("t2", i)])
            if hf is None:
                A(P, lambda e, k=k, i=i: e.activation(out=hb[:, k, :], in_=t2[i][:, :], func=AF.Identity,
                                                      bias=sh[:, k:k + 1]), [("t2", i), "mod"], [hkey])
            else:
                A(P, lambda e, k=k, i=i: e.activation(out=hf[:, k, :], in_=t2[i][:, :], func=AF.Identity,
                                                      bias=sh[:, k:k + 1]), [("t2", i), "mod"], [hfkey])
                G(P, lambda e, k=k: e.tensor_copy(out=hb[:, k, :], in_=hf[:, k, :]), [hfkey], [hkey])

    EPS_T = {}

    def eps_tile(val):
        return float(val)

    for l in range(n_layers):
        gm1, gm2 = modp[:, l, 0:8], modp[:, l, 8:16]
        sh1, gt1 = mod[:, l, 0:8], mod[:, l, 16:24]
        sh2, gt2 = mod[:, l, 24:32], mod[:, l, 40:48]

        if "A" in phases:
            with contextlib.ExitStack() as st:
                wA = sb("wA", [128, 8, 2048], BF16, stack=st)
                stg = [sb(f"stgA{i}", [128, 2048], stack=st) for i in range(3)]
                load_cast(wA, "wA", w_in[l], 8, 2048, stg, "stgA", cast_eng="alt")
                xtb = [sb(f"xtA{i}", [128, 8, 512], stack=st) for i in range(2)]
                hb = [sb(f"hbA{i}", [128, 8, 512], BF16, stack=st) for i in range(2)]
                tmp = {"sq": sb("sqA", [128, 8, 512], BF16, stack=st), "rstd": sb("rstdA", [128, 512], stack=st),
                       "t2": [sb(f"t2A{i}", [128, 512], stack=st) for i in range(2)]}
                uo = [sb(f"uoA{i}", [128, 512], stack=st) for i in range(4)]
                for n in range(NT):
                    xt = xtb[n % 2]
                    xkey = f"xtA{n % 2}"
                    for k in range(8):
                        P.dma("sync" if k % 2 == 0 else "gpsimd", xt[:, k, :],
                              xS[k * 128:(k + 1) * 128, n * 512:(n + 1) * 512], writes=[xkey])
                    h = hb[n % 2]
                    hkey = f"hbA{n % 2}"
                    rms_h(xt[:, :, :], xkey, h, hkey, gm1, sh1, tmp)
                    for m in range(NCH_IN):
                        bi = m % 4
                        mm_group(P, pb[bi][:, :], [(wA[:, k, m * 128:(m + 1) * 128], h[:, k, :]) for k in range(8)],
                                 ["wA", hkey], [PB(bi)])
                        oi = rr("uo", 4)
                        if m % 2 == 0:
                            V(P, lambda e, bi=bi, oi=oi: e.tensor_copy(out=uo[oi][:, :], in_=pb[bi][:, :]),
                              [PB(bi)], [("uo", oi)])
                        else:
                            A(P, lambda e, bi=bi, oi=oi: e.copy(out=uo[oi][:, :], in_=pb[bi][:, :]),
                              [PB(bi)], [("uo", oi)])
                        P.dma("sync", uS[m * 128:(m + 1) * 128, n * 512:(n + 1) * 512], uo[oi][:, :],
                              reads=[("uo", oi)], writes=[("uS", m, n)])
                P.barrier()

        if "B" in phases:
            with contextlib.ExitStack() as st:
                cw = sb("cw", [128, 2, 31], stack=st)
                cp = sb("cp", [128, 6], stack=st)
                for cc in range(2):
                    P.dma("sync", cw[:, cc, :], conv_w[l, cc], writes=["cw"])
                P.dma("sync", cp[:, :], conv_p[l], writes=["cp"])
                dg = sb("dg", [128, 62, 128], BF16, stack=st)
                for cc in range(2):
                    for k in range(31):
                        P.op("gpsimd" if k % 2 else "vector",
                             lambda e, cc=cc, k=k: e.tensor_scalar(out=dg[:, cc * 31 + k, :], in0=ident,
                                                                   scalar1=cw[:, cc, k:k + 1], scalar2=None,
                                                                   op0=ALU.mult), ["cw", "cst"], [("dg", cc, k)])
                at = [sb(f"atB{i}", [128, 542], stack=st) for i in range(2)]
                gtl = [sb(f"gtB{i}", [128, 542], stack=st) for i in range(2)]
                zb = [sb(f"zbB{i}", [128, 542], BF16, stack=st) for i in range(2)]
                zc = sb("zcB", [128, 2, 512], stack=st)
                xcB = sb("xcB", [128, 2, 512], stack=st)
                sqB = sb("sqB", [128, 2, 512], stack=st)
                rsB = sb("rsB", [128, 512], stack=st)
                tB = sb("tB", [128, 512], stack=st)
                yb = [sb(f"ybB{i}", [128, 512], BF16, stack=st) for i in range(2)]
                for n in range(NT):
                    for cc in range(2):
                        i = rr("atB", 2)
                        a_, g_, z_ = at[i], gtl[i], zb[i]
                        ak, gk, zk = ("atB", i), ("gtB", i), ("zbB", i)
                        if n == 0:
                            V(P, lambda e, a_=a_: e.memset(a_[:, 0:30], 0.0), [], [ak])
                            V(P, lambda e, g_=g_: e.memset(g_[:, 0:30], 0.0), [], [gk])
                            P.dma("sync", a_[:, 30:542], uS[cc * 128:(cc + 1) * 128, 0:512], writes=[ak])
                            P.dma("gpsimd", g_[:, 30:542], uS[(2 + cc) * 128:(3 + cc) * 128, 0:512], writes=[gk])
                        else:
                            P.dma("sync", a_[:, :], uS[cc * 128:(cc + 1) * 128, n * 512 - 30:(n + 1) * 512], writes=[ak])
                            P.dma("gpsimd", g_[:, :], uS[(2 + cc) * 128:(3 + cc) * 128, n * 512 - 30:(n + 1) * 512],
                                  writes=[gk])
                        A(P, lambda e, g_=g_: e.activation(out=g_[:, :], in_=g_[:, :], func=AF.Sigmoid), [gk], [gk])
                        V(P, lambda e, a_=a_, g_=g_, z_=z_: e.tensor_tensor(out=z_[:, :], in0=a_[:, :], in1=g_[:, :],
                                                                            op=ALU.mult), [ak, gk], [zk])
                        mm_group(P, pb[cc][:, :], [(dg[:, cc * 31 + k, :], z_[:, k:k + 512]) for k in range(31)],
                                 [zk] + [("dg", cc, k) for k in range(31)], [PB(cc)])
                        A(P, lambda e, cc=cc: e.activation(out=zc[:, cc, :], in_=pb[cc][:, :], func=AF.Identity,
                                                           bias=cp[:, cc:cc + 1]), [PB(cc), "cp"], ["zcB"])
                    mm_group(P, pb[2][:, :], [(ones_f, zc[:, 0, :]), (ones_f, zc[:, 1, :])], ["zcB", "cst"], [PB(2)])
                    for cc in range(2):
                        V(P, lambda e, cc=cc: e.scalar_tensor_tensor(out=xcB[:, cc, :], in0=pb[2][:, :],
                                                                     scalar=-1.0 / 256, in1=zc[:, cc, :],
                                                                     op0=ALU.mult, op1=ALU.add),
                          [PB(2), "zcB"], ["xcB"])
                    A(P, lambda e: e.activation(out=sqB[:, :, :], in_=xcB[:, :, :], func=AF.Square), ["xcB"], ["sqB"])
                    mm_group(P, pb[3][:, :], [(ones_f, sqB[:, 0, :]), (ones_f, sqB[:, 1, :])], ["sqB", "cst"], [PB(3)])
                    A(P, lambda e: e.activation(out=rsB[:, :], in_=pb[3][:, :], func=AF.Sqrt, scale=1.0 / 256,
                                                bias=LN_EPS), [PB(3)], ["rsB"])
                    V(P, lambda e: e.reciprocal(out=rsB[:, :], in_=rsB[:, :]), ["rsB"], ["rsB"])
                    for cc in range(2):
                        V(P, lambda e, cc=cc: e.scalar_tensor_tensor(out=tB[:, :], in0=xcB[:, cc, :],
                                                                     scalar=cp[:, 2 + cc:3 + cc], in1=rsB[:, :],
                                                                     op0=ALU.mult, op1=ALU.mult),
                          ["xcB", "rsB", "cp"], ["tB"])
                        yi = rr("ybB", 2)
                        A(P, lambda e, cc=cc, yi=yi: e.activation(out=yb[yi][:, :], in_=tB[:, :], func=AF.Silu,
                                                                  bias=cp[:, 4 + cc:5 + cc]), ["tB", "cp"], [("ybB", yi)])
                        P.dma("sync", mixS[cc * 128:(cc + 1) * 128, n * 512:(n + 1) * 512], yb[yi][:, :],
                              reads=[("ybB", yi)], writes=[("mixS", cc, n)])
                P.barrier()

        if "C" in phases:
            with contextlib.ExitStack() as st:
                Vres = sb("Vres", [128, 32, 8, 65], BF16, stack=st)
                V(P, lambda e: e.memset(Vres[:, :, :, 64:65], 1.0), [], ["Vres"])
                with contextlib.ExitStack() as st1:
                    mp = sb("mp", [128, 5], stack=st1)
                    P.dma("sync", mp[:, :], mla_p[l], writes=["mp"])
                    wq = sb("wq", [128, 2, 768], BF16, stack=st1)
                    wk = sb("wk", [128, 1, 512], BF16, stack=st1)
                    wv = sb("wv", [128, 1, 512], BF16, stack=st1)
                    stg = [sb(f"stgC{i}", [128, 2048], stack=st1) for i in range(2)]
                    load_cast(wq, "wq", w_uq[l], 2, 768, stg, "stgC")
                    load_cast(wk, "wk", w_k[l], 1, 512, stg, "stgC")
                    load_cast(wv, "wv", w_v[l], 1, 512, stg, "stgC")
                    cq = sb("cq", [128, 2, 512], stack=st1)
                    ckv = sb("ckv", [128, 512], stack=st1)
                    krt = sb("krt", [128, 512], stack=st1)
                    cst_ = sb("cosC", [96, 512], stack=st1)
                    snt_ = sb("sinC", [96, 512], stack=st1)
                    sqC = sb("sqC", [128, 3, 512], BF16, stack=st1)
                    rqC = sb("rqC", [128, 2, 512], stack=st1)
                    cqn = sb("cqn", [128, 2, 512], BF16, stack=st1)
                    ckvn = sb("ckvn", [128, 512], BF16, stack=st1)
                    raw = [sb(f"rawC{i}", [96, 512], stack=st1) for i in range(4)]
                    sqh = [sb(f"sqhC{i}", [96, 512], stack=st1) for i in range(4)]
                    rsh = [sb(f"rshC{i}", [96, 512], stack=st1) for i in range(4)]
                    qn = [sb(f"qnC{i}", [96, 512], stack=st1) for i in range(4)]
                    t1 = [sb(f"t1C{i}", [96, 512], stack=st1) for i in range(4)]
                    qf = [sb(f"qfC{i}", [96, 512], BF16, stack=st1) for i in range(4)]

                    def norm_rope_multi(chains):
                        for (i, gcol, dst, dkey) in chains:
                            A(P, lambda e: e.activation(out=sqh[i][:, :], in_=raw[i][:, :], func=AF.Square),
                              [("rawC", i)], [("sqhC", i)])
                        for (i, gcol, dst, dkey) in chains:
                            bi = 4 + i
                            PE(P, lambda e: e.matmul(pb[bi][0:96, :], ones_f[0:96, 0:96], sqh[i][:, :], start=True,
                                                     stop=True), [("sqhC", i), "cst"], [PB(bi)])
                        for (i, gcol, dst, dkey) in chains:
                            bi = 4 + i
                            A(P, lambda e: e.activation(out=rsh[i][:, :], in_=pb[bi][0:96, :], func=AF.Sqrt,
                                                        scale=1.0 / 96, bias=RMS_EPS), [PB(bi)], [("rshC", i)])
                        for (i, gcol, dst, dkey) in chains:
                            V(P, lambda e: e.reciprocal(out=rsh[i][:, :], in_=rsh[i][:, :]), [("rshC", i)], [("rshC", i)])
                            V(P, lambda e: e.scalar_tensor_tensor(out=qn[i][:, :], in0=raw[i][:, :], scalar=gcol,
                                                                  in1=rsh[i][:, :], op0=ALU.mult, op1=ALU.mult),
                              [("rawC", i), ("rshC", i), "mp"], [("qnC", i)])
                        for (i, gcol, dst, dkey) in chains:
                            bi = 4 + i
                            PE(P, lambda e: e.matmul(pb[bi][0:96, :], rmat, qn[i][:, :], start=True, stop=True),
                               [("qnC", i), "cst"], [PB(bi)])
                        for (i, gcol, dst, dkey) in chains:
                            bi = 4 + i
                            V(P, lambda e: e.tensor_tensor(out=t1[i][:, :], in0=pb[bi][0:96, :], in1=snt_[:, :],
                                                           op=ALU.mult), [PB(bi), "sinC"], [("t1C", i)])
                            G(P, lambda e: e.tensor_tensor(out=qn[i][:, :], in0=qn[i][:, :], in1=cst_[:, :], op=ALU.mult),
                              [("qnC", i), "cosC"], [("qnC", i)])
                        for (i, gcol, dst, dkey) in chains:
                            G(P, lambda e: e.tensor_tensor(out=qf[i][:, :], in0=qn[i][:, :], in1=t1[i][:, :], op=ALU.add),
                              [("qnC", i), ("t1C", i)], [("qfC", i)])
                            P.dma("sync" if i % 2 == 0 else "gpsimd", dst, qf[i][:, :], reads=[("qfC", i)], writes=[dkey])

                    for n in range(NT):
                        tsl = slice(n * 512, (n + 1) * 512)
                        for k in range(2):
                            P.dma("sync", cq[:, k, :], uS[(4 + k) * 128:(5 + k) * 128, tsl], writes=["cq"])
                        P.dma("gpsimd", ckv[:, :], uS[6 * 128:7 * 128, tsl], writes=["ckv"])
                        P.dma("gpsimd", krt[:, :], uS[7 * 128:8 * 128, tsl], writes=["krt"])
                        P.dma("sync", cst_[:, :], csS[0][:, tsl], writes=["cosC"])
                        P.dma("sync", snt_[:, :], csS[1][:, tsl], writes=["sinC"])
                        A(P, lambda e: e.activation(out=sqC[:, 0:2, :], in_=cq[:, :, :], func=AF.Square), ["cq"], ["sqC"])
                        A(P, lambda e: e.activation(out=sqC[:, 2, :], in_=ckv[:, :], func=AF.Square), ["ckv"], ["sqC"])
                        mm_group(P, pb[0][:, :], [(ones_b, sqC[:, 0, :]), (ones_b, sqC[:, 1, :])], ["sqC", "cbf"], [PB(0)])
                        mm_group(P, pb[1][:, :], [(ones_b, sqC[:, 2, :])], ["sqC", "cbf"], [PB(1)])
                        A(P, lambda e: e.activation(out=rqC[:, 0, :], in_=pb[0][:, :], func=AF.Sqrt, scale=1.0 / 256,
                                                    bias=RMS_EPS), [PB(0)], ["rqC"])
                        A(P, lambda e: e.activation(out=rqC[:, 1, :], in_=pb[1][:, :], func=AF.Sqrt, scale=1.0 / 128,
                                                    bias=RMS_EPS), [PB(1)], ["rqC"])
                        V(P, lambda e: e.reciprocal(out=rqC[:, :, :], in_=rqC[:, :, :]), ["rqC"], ["rqC"])
                        for k in range(2):
                            V(P, lambda e, k=k: e.scalar_tensor_tensor(out=cqn[:, k, :], in0=cq[:, k, :],
                                                                       scalar=mp[:, k:k + 1], in1=rqC[:, 0, :],
                                                                       op0=ALU.mult, op1=ALU.mult),
                              ["cq", "rqC", "mp"], ["cqn"])
                        V(P, lambda e: e.scalar_tensor_tensor(out=ckvn[:, :], in0=ckv[:, :], scalar=mp[:, 2:3],
                                                              in1=rqC[:, 1, :], op0=ALU.mult, op1=ALU.mult),
                          ["ckv", "rqC", "mp"], ["ckvn"])
                        for j in range(4):
                            bi = 2 + (j % 2)
                            PE(P, lambda e, j=j, bi=bi: e.matmul(pb[bi][:, :], ckvn[:, j * 128:(j + 1) * 128], wv[:, 0, :],
                                                                 start=True, stop=True), ["ckvn", "wv"], [PB(bi)])
                            A(P, lambda e, j=j, bi=bi, n=n: e.copy(
                                out=Vres[:, n * 4 + j, :, 0:64],
                                in_=pb[bi][:, :].rearrange("p (h d) -> p h d", h=8)), [PB(bi)], ["Vres"])
                        for hp in range(4):
                            chains = []
                            for hh in range(2):
                                h = 2 * hp + hh
                                iq, ik = 2 * hh, 2 * hh + 1
                                bq, bk = hh, 2 + hh
                                mm_group(P, pb[bq][0:96, :], [(wq[:, k, 96 * h:96 * h + 96], cqn[:, k, :]) for k in range(2)],
                                         ["wq", "cqn"], [PB(bq)])
                                PE(P, lambda e, h=h, bk=bk: e.matmul(pb[bk][0:64, :], wk[:, 0, 64 * h:64 * h + 64], ckvn[:, :],
                                                                     start=True, stop=True), ["wk", "ckvn"], [PB(bk)])
                                A(P, lambda e, iq=iq, bq=bq: e.copy(out=raw[iq][:, :], in_=pb[bq][0:96, :]), [PB(bq)],
                                  [("rawC", iq)])
                                V(P, lambda e, ik=ik, bk=bk: e.tensor_copy(out=raw[ik][0:64, :], in_=pb[bk][0:64, :]), [PB(bk)],
                                  [("rawC", ik)])
                                A(P, lambda e, ik=ik: e.copy(out=raw[ik][64:96, :], in_=krt[64:96, :]), ["krt"], [("rawC", ik)])
                                chains.append((iq, mp[0:96, 3:4], qS[h][:, tsl], ("qS", h, n)))
                                chains.append((ik, mp[0:96, 4:5], kS[h][:, tsl], ("kS", h, n)))
                            norm_rope_multi(chains)
                    P.barrier()
                with contextlib.ExitStack() as st2:
                    KT = [sb(f"KT{i}", [96, T], BF16, stack=st2) for i in range(2)]
                    QT = [sb(f"QT{i}", [96, T], BF16, stack=st2) for i in range(2)]
                    pt = [sb(f"ptC{i}", [128, 512], BF16, stack=st2) for i in range(4)]
                    osb = [sb(f"osb{i}", [65, 512], stack=st2) for i in range(2)]
                    rc = [sb(f"rcC{i}", [64, 512], stack=st2) for i in range(2)]
                    yo = [sb(f"yoC{i}", [64, 512], BF16, stack=st2) for i in range(2)]
                    for h in range(8):
                        kt_, qt_ = KT[h % 2], QT[h % 2]
                        kk_, qk_ = f"KT{h % 2}", f"QT{h % 2}"
                        for half in range(2):
                            hs = slice(half * 2048, (half + 1) * 2048)
                            P.dma("sync", kt_[:, hs], kS[h][:, hs], writes=[kk_])
                            P.dma("gpsimd", qt_[:, hs], qS[h][:, hs], writes=[qk_])
                        blocks = [(qi, kb) for qi in range(NT) for kb in range(4 * qi + 4)]

                        def geom(qi, kb):
                            d = kb - 4 * qi
                            return d, max(0, d) * 128

                        def emit_S(bi_):
                            qi, kb = blocks[bi_]
                            d, c0 = geom(qi, kb)
                            sbk = bi_ % 3
                            PE(P, lambda e: e.matmul(pb[sbk][:, c0:512], kt_[:, kb * 128:(kb + 1) * 128],
                                                     qt_[:, qi * 512 + c0:(qi + 1) * 512], start=True, stop=True),
                               [kk_, qk_], [PB(sbk)])

                        emit_S(0)
                        emit_S(1)
                        for bi_, (qi, kb) in enumerate(blocks):
                            d, c0 = geom(qi, kb)
                            nkb = 4 * qi + 4
                            ob = 4 + (qi % 2)
                            sbk = bi_ % 3
                            pi = bi_ % 4
                            A(P, lambda e: e.activation(out=pt[pi][:, c0:512], in_=pb[sbk][:, c0:512], func=AF.Exp,
                                                        scale=SCALE_QK), [PB(sbk)], [("ptC", pi)])
                            if d >= 0:
                                G(P, lambda e: e.tensor_tensor(out=pt[pi][:, c0:c0 + 128], in0=pt[pi][:, c0:c0 + 128],
                                                               in1=tri_b, op=ALU.mult), [("ptC", pi), "cbf"], [("ptC", pi)])
                            if bi_ + 2 < len(blocks):
                                emit_S(bi_ + 2)
                            PE(P, lambda e: e.matmul(pb[ob][0:65, c0:512], Vres[:, kb, h, :], pt[pi][:, c0:512],
                                                     start=(kb == 0), stop=(kb == nkb - 1)), [("ptC", pi), "Vres"], [PB(ob)])
                            if kb == nkb - 1:
                                oi = qi % 2
                                A(P, lambda e: e.copy(out=osb[oi][:, :], in_=pb[ob][0:65, :]), [PB(ob)], [("osb", oi)])
                                PE(P, lambda e: e.matmul(pb[6 + oi][0:64, :], sel65, osb[oi][:, :], start=True, stop=True),
                                   [("osb", oi), "cst"], [PB(6 + oi)])
                                V(P, lambda e: e.reciprocal(out=rc[oi][:, :], in_=pb[6 + oi][0:64, :]), [PB(6 + oi)],
                                  [("rcC", oi)])
                                V(P, lambda e: e.tensor_tensor(out=yo[oi][:, :], in0=osb[oi][0:64, :], in1=rc[oi][:, :],
                                                               op=ALU.mult), [("osb", oi), ("rcC", oi)], [("yoC", oi)])
                                P.dma("sync", mixS[256 + 64 * h:256 + 64 * (h + 1), qi * 512:(qi + 1) * 512], yo[oi][:, :],
                                      reads=[("yoC", oi)], writes=[("mixS", "m", h, qi)])
                    P.barrier()

        if "D" in phases:
            rwkv_phase(nc, P, l, locals())

        if "E" in phases:
            with contextlib.ExitStack() as st:
                wO = sb("wO", [128, 8, 1024], BF16, stack=st)
                stg = [sb(f"stgE{i}", [128, 2048], stack=st) for i in range(3)]
                load_cast(wO, "wO", w_out[l], 8, 1024, stg, "stgE", cast_eng="alt")
                xtb = [sb(f"xtE{i}", [128, 8, 512], stack=st) for i in range(2)]
                mtb = [sb(f"mtE{i}", [128, 8, 512], BF16, stack=st) for i in range(2)]
                for n in range(NT):
                    xt, mt = xtb[n % 2], mtb[n % 2]
                    xkey, mkey = f"xtE{n % 2}", f"mtE{n % 2}"
                    tsl = slice(n * 512, (n + 1) * 512)
                    for k in range(8):
                        P.dma("sync", xt[:, k, :], xS[k * 128:(k + 1) * 128, tsl], reads=[("xS", k, n)], writes=[(xkey, k)])
                        P.dma("gpsimd", mt[:, k, :], mixS[k * 128:(k + 1) * 128, tsl], writes=[mkey])
                    for m in range(8):
                        bi = m % 4
                        mm_group(P, pb[bi][:, :], [(wO[:, k, m * 128:(m + 1) * 128], mt[:, k, :]) for k in range(8)],
                                 ["wO", mkey], [PB(bi)])
                        V(P, lambda e, m=m, bi=bi, xt=xt: e.scalar_tensor_tensor(
                            out=xt[:, m, :], in0=pb[bi][:, :], scalar=gt1[:, m:m + 1], in1=xt[:, m, :],
                            op0=ALU.mult, op1=ALU.add), [PB(bi), (xkey, m), "mod"], [(xkey, m)])
                        P.dma("sync", xS[m * 128:(m + 1) * 128, tsl], xt[:, m, :], reads=[(xkey, m)],
                              writes=[("xS", m, n)])
                P.barrier()

        if "F" in phases:
            ffn_phase(nc, P, l, locals())

    P.barrier()
    es.close()
    return nc


RW_STOP = os.environ.get('RW_STOP', '')
RW_LVL = int(os.environ.get('RW_LVL', '99'))


USE_R32 = os.environ.get('USE_R32', '1') == '1'


def R32g(ap):
    return ap.bitcast(mybir.dt.float32r) if USE_R32 else ap


def rwkv_phase(nc, P, l, E):
    sb, pb, PB, uS, mixS, rr = E["sb"], E["pb"], E["PB"], E["uS"], E["mixS"], E["rr"]
    ident, ones_f, bd_ones, m_sl, m_row, zero_t = (E["ident"], E["ones_f"], E["bd_ones"], E["m_sl"], E["m_row"],
                                                   E["zero_t"])
    EM05 = float(np.exp(-0.5))
    X_ = mybir.AxisListType.X
    with contextlib.ExitStack() as st:
        mu = sb("muD", [128, 8], stack=st)
        rp = sb("rpD", [128, 14], stack=st)
        wup = sb("wupD", [128, 256], stack=st)
        gup = sb("gupD", [128, 256], stack=st)
        P.dma("sync", mu[:, :], E["rw_mu"][l], writes=["muD"])
        P.dma("sync", rp[:, :], E["rw_p"][l], writes=["rpD"])
        P.dma("sync", wup[:, :], E["rw_wup"][l], writes=["wupD"])
        P.dma("sync", gup[:, :], E["rw_gup"][l], writes=["gupD"])
        omka = sb("omkaD", [128, 2], stack=st)
        V(P, lambda e: e.tensor_scalar(out=omka[:, :], in0=rp[:, 6:8], scalar1=-1.0, scalar2=1.0, op0=ALU.mult,
                                       op1=ALU.add), ["rpD"], ["omkaD"])
        w0, a0, k_k, k_a, r_k, ln_g, ln_b = [rp[:, 2 * i:2 * i + 2] for i in range(7)]
        ST = [[sb(f"ST{c}{i}", [128, 64], stack=st) for i in range(2)] for c in range(2)]
        for c in range(2):
            V(P, lambda e, c=c: e.memset(ST[c][0][:, :], 0.0), [], [("ST", c, 0, 0), ("ST", c, 0, 1)])
        ur = sb("urD", [128, 8, 513], stack=st)
        us = sb("usD", [128, 8, 512], stack=st)
        dl = [sb(f"dlD{i}", [128, 512], stack=st) for i in range(2)]
        tw = sb("twD", [128, 512], stack=st)
        sgd = sb("sgdD", [128, 512], stack=st)

        def t2(name, shape=(128, 512)):
            return [sb(f"{name}{c}", list(shape), stack=st) for c in range(2)]
        def t22(name, shape=(128, 512)):
            return [[sb(f"{name}{pp}{c}", list(shape), stack=st) for c in range(2)] for pp in range(2)]
        dec, asg, gtc, kk, kk2, rn, kkn, tkk, km, bv = (t2("decD"), t2("asgD"), t22("gtcD"), t2("kkD"), t2("kk2D"),
                                                        t2("rnD"), t2("kknD"), None, t2("kmD"), t2("bvD"))
        Gi, Ginv, Ge, bt, kt, rk, bon, ysb, yc, sqy = (t22("GiD"), None, t2("GeD"), t22("btD"), t22("ktD"),
                                                       None, t22("bonD"), t2("ysbD"), None, None)
        ar = t22("arD", (128, 4, 256))
        btT, ktT, vT = t22("btTD", (128, 4, 128)), t22("ktTD", (128, 4, 128)), t22("vTD", (128, 4, 128))
        yo = [sb(f"yoD{c}", [128, 512], BF16, stack=st) for c in range(2)]
        Mm = [sb(f"MmD{q}", [128, 512], stack=st) for q in range(4)]
        FR = mybir.dt.float32r if USE_R32 else F32
        PT0 = [sb(f"PT0D{q}", [128, 128], stack=st) for q in range(4)]
        PTb = [[sb(f"PTD{q}{i}", [128, 128], FR, stack=st) for i in range(2)] for q in range(4)]
        Pmb = [[sb(f"PmD{q}{i}", [128, 128], FR, stack=st) for i in range(2)] for q in range(4)]
        Xb = [[sb(f"XD{q}{i}", [128, 128], FR, stack=st) for i in range(2)] for q in range(4)]
        XF = [sb(f"XFD{q}", [128, 128], stack=st) for q in range(4)]
        Wsb = [sb(f"WsbD{q}", [128, 64], stack=st) for q in range(4)]
        Usb = [sb(f"UsbD{q}", [128, 64], stack=st) for q in range(4)]

        def v4(t):
            return t[:, :].rearrange("p (j t) -> p j t", j=4)

        def prep_gen(n):
            p = n % 2
            if n == 0:
                V(P, lambda e: e.memset(ur[:, :, 0:1], 0.0), [], ["urD"])
                yield
            for j in range(8):
                q_ = "sync" if j % 2 == 0 else "gpsimd"
                if n == 0:
                    P.dma(q_, ur[:, j, 1:513], uS[(8 + j) * 128:(9 + j) * 128, 0:512], writes=["urD"])
                    yield
                else:
                    P.dma(q_, ur[:, j, :], uS[(8 + j) * 128:(9 + j) * 128, n * 512 - 1:(n + 1) * 512], writes=["urD"])
                    yield
            for j in range(8):
                eng = "vector" if j % 2 == 0 else "gpsimd"
                di = j % 2
                P.op(eng, lambda e, j=j, di=di: e.tensor_tensor(out=dl[di][:, :], in0=ur[:, j, 0:512], in1=ur[:, j, 1:513],
                                                                op=ALU.subtract), ["urD"], [("dlD", di)])
                yield
                P.op("vector", lambda e, j=j, di=di: e.scalar_tensor_tensor(out=us[:, j, :], in0=dl[di][:, :],
                                                                       scalar=mu[:, j:j + 1], in1=ur[:, j, 1:513],
                                                                       op0=ALU.mult, op1=ALU.add),
                     [("dlD", di), "urD", "muD"], [("usD", j)])
                yield
            A(P, lambda e: e.activation(out=tw[0:64, :], in_=us[0:64, 6, :], func=AF.Tanh), [("usD", 6)], ["twD"])
            yield
            A(P, lambda e: e.activation(out=sgd[:, :], in_=us[:, 7, :], func=AF.Sigmoid), [("usD", 7)], ["sgdD"])
            yield
            for c in range(2):
                cs = slice(c * 128, (c + 1) * 128)
                kR, kK, kV = ("usD", c), ("usD", 2 + c), ("usD", 4 + c)
                r_, k_, v_ = us[:, c, :], us[:, 2 + c, :], us[:, 4 + c, :]
                PE(P, lambda e: e.matmul(pb[0][:, :], wup[0:64, cs], tw[0:64, :], start=True, stop=True),
                   ["wupD", "twD"], [PB(0)])
                yield
                A(P, lambda e: e.activation(out=dec[c][:, :], in_=pb[0][:, :], func=AF.Sigmoid, bias=w0[:, c:c + 1]),
                  [PB(0), "rpD"], [("decD", c)])
                yield
                A(P, lambda e: e.activation(out=dec[c][:, :], in_=dec[c][:, :], func=AF.Exp, scale=-EM05),
                  [("decD", c)], [("decD", c)])
                yield
                PE(P, lambda e: e.matmul(pb[1][:, :], wup[64:128, cs], us[64:128, 6, :], start=True, stop=True),
                   ["wupD", ("usD", 6)], [PB(1)])
                yield
                A(P, lambda e: e.activation(out=asg[c][:, :], in_=pb[1][:, :], func=AF.Sigmoid, bias=a0[:, c:c + 1]),
                  [PB(1), "rpD"], [("asgD", c)])
                yield
                PE(P, lambda e: e.matmul(pb[2][:, :], gup[:, cs], sgd[:, :], start=True, stop=True),
                   ["gupD", "sgdD"], [PB(2)])
                yield
                A(P, lambda e: e.copy(out=gtc[p][c][:, :], in_=pb[2][:, :]), [PB(2)], [("gtcD", c, p)])
                yield
                V(P, lambda e: e.tensor_scalar(out=kk[c][:, :], in0=k_, scalar1=k_k[:, c:c + 1], scalar2=None,
                                               op0=ALU.mult), [kK, "rpD"], [("kkD", c)])
                yield
                G(P, lambda e: e.tensor_tensor(out=kk2[c][:, :], in0=kk[c][:, :], in1=kk[c][:, :], op=ALU.mult),
                  [("kkD", c)], [("kk2D", c)])
                yield
                PE(P, lambda e: e.matmul(pb[3][:, :], bd_ones, kk2[c][:, :], start=True, stop=True),
                   [("kk2D", c), "cst"], [PB(3)])
                yield
                V(P, lambda e: e.tensor_scalar(out=rn[c][:, :], in0=pb[3][:, :], scalar1=1e-24, scalar2=None,
                                               op0=ALU.max), [PB(3)], [("rnD", c)])
                yield
                A(P, lambda e: e.activation(out=rn[c][:, :], in_=rn[c][:, :], func=AF.Sqrt), [("rnD", c)], [("rnD", c)])
                yield
                V(P, lambda e: e.reciprocal(out=rn[c][:, :], in_=rn[c][:, :]), [("rnD", c)], [("rnD", c)])
                yield
                V(P, lambda e: e.tensor_tensor(out=kkn[c][:, :], in0=kk[c][:, :], in1=rn[c][:, :], op=ALU.mult),
                  [("kkD", c), ("rnD", c)], [("kknD", c)])
                yield
                V(P, lambda e: e.tensor_scalar(out=rn[c][:, :], in0=asg[c][:, :], scalar1=k_a[:, c:c + 1],
                                               scalar2=omka[:, c:c + 1], op0=ALU.mult, op1=ALU.add),
                  [("asgD", c), "rpD", "omkaD"], [("rnD", c)])
                yield
                V(P, lambda e: e.tensor_tensor(out=km[c][:, :], in0=k_, in1=rn[c][:, :], op=ALU.mult),
                  [kK, ("rnD", c)], [("kmD", c)])
                yield
                G(P, lambda e: e.tensor_tensor(out=bv[c][:, :], in0=kkn[c][:, :], in1=asg[c][:, :], op=ALU.mult),
                  [("kknD", c), ("asgD", c)], [("bvD", c)])
                yield
                for j in range(4):
                    tk = slice(j * 128, (j + 1) * 128)
                    V(P, lambda e, tk=tk: e.tensor_tensor_scan(out=Gi[p][c][:, tk], data0=dec[c][:, tk],
                                                               data1=zero_t[:, 0:128], initial=1.0, op0=ALU.mult,
                                                               op1=ALU.add), [("decD", c), "zero"], [("GiD", c, p)])
                    yield
                V(P, lambda e: e.reciprocal(out=dec[c][:, :], in_=Gi[p][c][:, :]), [("GiD", c, p)], [("decD", c)])
                yield
                V(P, lambda e: e.tensor_copy(out=v4(Ge[c])[:, :, 1:128], in_=v4(Gi[p][c])[:, :, 0:127]), [("GiD", c, p)],
                  [("GeD", c)])
                yield
                V(P, lambda e: e.memset(v4(Ge[c])[:, :, 0:1], 1.0), [], [("GeD", c)])
                yield
                V(P, lambda e: e.scalar_tensor_tensor(out=ar[p][c][:, :, 0:128], in0=v4(kkn[c]), scalar=-1.0,
                                                      in1=v4(Ge[c]), op0=ALU.mult, op1=ALU.mult),
                  [("kknD", c), ("GeD", c)], [("arD", c, p)])
                yield
                V(P, lambda e: e.tensor_tensor(out=ar[p][c][:, :, 128:256], in0=r_.rearrange("p (j t) -> p j t", j=4),
                                               in1=v4(Gi[p][c]), op=ALU.mult), [kR, ("GiD", c, p)], [("arD", c, p)])
                yield
                G(P, lambda e: e.tensor_tensor(out=bt[p][c][:, :], in0=bv[c][:, :], in1=dec[c][:, :], op=ALU.mult),
                  [("bvD", c), ("decD", c)], [("btD", c, p)])
                yield
                G(P, lambda e: e.tensor_tensor(out=kt[p][c][:, :], in0=km[c][:, :], in1=dec[c][:, :], op=ALU.mult),
                  [("kmD", c), ("decD", c)], [("ktD", c, p)])
                yield
                V(P, lambda e: e.scalar_tensor_tensor(out=kk2[c][:, :], in0=r_, scalar=r_k[:, c:c + 1], in1=km[c][:, :],
                                                      op0=ALU.mult, op1=ALU.mult), [kR, ("kmD", c), "rpD"], [("kk2D", c)])
                yield
                PE(P, lambda e: e.matmul(pb[4][:, :], bd_ones, kk2[c][:, :], start=True, stop=True),
                   [("kk2D", c), "cst"], [PB(4)])
                yield
                V(P, lambda e: e.tensor_tensor(out=bon[p][c][:, :], in0=pb[4][:, :], in1=v_, op=ALU.mult),
                  [PB(4), kV], [("bonD", c, p)])
                yield
                for bi, (src, skey, dst, dkey) in enumerate(((bt[p][c], ("btD", c, p), btT[p][c], ("btTD", c, p)),
                                                             (kt[p][c], ("ktD", c, p), ktT[p][c], ("ktTD", c, p)),
                                                             (v_, kV, vT[p][c], ("vTD", c, p)))):
                    bank = 5 + bi

                    def fn(e, src=src, bank=bank):
                        ins = None
                        for j in range(4):
                            ins = e.transpose(pb[bank][:, j * 128:(j + 1) * 128], src[:, j * 128:(j + 1) * 128], ident)
                        return ins
                    PE(P, fn, [skey, "cst"], [PB(bank)])
                    yield
                    if bi % 2 == 0:
                        A(P, lambda e, dst=dst, bank=bank: e.copy(out=dst[:, :, :],
                                                                  in_=pb[bank][:, :].rearrange("p (j t) -> p j t", j=4)),
                          [PB(bank)], [dkey])
                        yield
                    else:
                        V(P, lambda e, dst=dst, bank=bank: e.tensor_copy(
                            out=dst[:, :, :], in_=pb[bank][:, :].rearrange("p (j t) -> p j t", j=4)), [PB(bank)], [dkey])
                        yield


        def exhaust(g):
            if g is not None:
                for _ in g:
                    pass

        exhaust(prep_gen(0))
        for n in range(NT):
            p = n % 2
            pgen = [prep_gen(n + 1) if n + 1 < NT else None]

            def adv(k, site='a'):
                if pgen[0] is None or site not in os.environ.get('RW_ADV', 'ac'):
                    return
                for _ in range(k):
                    try:
                        next(pgen[0])
                    except StopIteration:
                        pgen[0] = None
                        return
            for j in range(4 if RW_STOP != 'prep' else 0):
                g = n * 4 + j
                cur, nxt = g % 2, (g + 1) % 2
                tk = slice(j * 128, (j + 1) * 128)
                heads = [(c, hh) for c in range(2) for hh in range(2)]
                for (c, hh) in heads:
                    q = c * 2 + hh
                    sl = slice(64 * hh, 64 * hh + 64)
                    PE(P, lambda e, c=c, sl=sl, q=q: e.matmul(pb[q][:, 0:256], bt[p][c][sl, tk], ar[p][c][sl, j, :],
                                                              start=True, stop=True), [("btD", c, p), ("arD", c, p)], [PB(q)])
                    PE(P, lambda e, c=c, sl=sl, q=q: e.matmul(pb[q][:, 256:512], kt[p][c][sl, tk], ar[p][c][sl, j, :],
                                                              start=True, stop=True), [("ktD", c, p), ("arD", c, p)], [PB(q)])
                    V(P, lambda e, q=q: e.tensor_tensor(out=Mm[q][:, :], in0=pb[q][:, :], in1=m_row, op=ALU.mult),
                      [PB(q), "cst"], [("MmD", q)])
                    PE(P, lambda e, c=c, sl=sl, q=q: e.matmul(pb[q][:, 0:128], ar[p][c][sl, j, 0:128], bt[p][c][sl, tk],
                                                              start=True, stop=True), [("btD", c, p), ("arD", c, p)], [PB(q)])
                    V(P, lambda e, q=q: e.tensor_tensor(out=PT0[q][:, :], in0=pb[q][:, 0:128], in1=m_sl, op=ALU.mult),
                      [PB(q), "cst"], [("PT0D", q)])
                    G(P, lambda e, q=q: e.tensor_tensor(out=Xb[q][0][:, :], in0=Mm[q][:, 0:128], in1=ident, op=ALU.add),
                      [("MmD", q), "cst"], [("XD", q, 0)])
                adv(13, 'a')
                for lev in range(6 if RW_STOP != 'M' else 0):
                    adv(3, 'b')
                    for (c, hh) in heads:
                        q = c * 2 + hh
                        curP = Mm[q][:, 0:128] if lev == 0 else Pmb[q][(lev - 1) % 2][:, :]
                        curPk = ("MmD", q) if lev == 0 else ("PmD", q, (lev - 1) % 2)
                        curPT = PT0[q][:, :] if lev == 0 else PTb[q][(lev - 1) % 2][:, :]
                        curPTk = ("PT0D", q) if lev == 0 else ("PTD", q, (lev - 1) % 2)
                        nPT, nPTk = PTb[q][lev % 2], ("PTD", q, lev % 2)
                        nP, nPk = Pmb[q][lev % 2], ("PmD", q, lev % 2)
                        cX, cXk = Xb[q][lev % 2], ("XD", q, lev % 2)
                        if lev < 5:
                            nX, nXk = Xb[q][(lev + 1) % 2], ("XD", q, (lev + 1) % 2)
                        else:
                            nX, nXk = XF[q], ("XFD", q)
                        R32 = (lambda ap: ap) if lev == 0 else R32g
                        if lev < 5:
                            PE(P, lambda e, q=q, curP=curP, curPT=curPT: e.matmul(pb[q][:, 0:128], R32(curPT), R32(curP),
                                                                                  start=True, stop=True),
                               [curPk, curPTk], [PB(q)])
                        PE(P, lambda e, q=q, curP=curP, curPT=curPT: e.matmul(pb[4 + q][:, 0:128], R32(curP), R32(curPT),
                                                                              start=True, stop=True),
                           [curPk, curPTk], [PB(4 + q)])
                        if lev < 5:
                            V(P, lambda e, q=q, nP=nP: e.tensor_copy(out=nP[:, :], in_=pb[q][:, 0:128]), [PB(q)], [nPk])
                        A(P, lambda e, q=q, nPT=nPT: e.copy(out=nPT[:, :], in_=pb[4 + q][:, 0:128]), [PB(4 + q)], [nPTk])
                        PE(P, lambda e, q=q, nPT=nPT, cX=cX: e.matmul(pb[q][:, 256:384], R32g(nPT[:, :]), R32g(cX[:, :]),
                                                                      start=True, stop=True), [nPTk, cXk], [PB(q)])
                        V(P, lambda e, q=q, cX=cX, nX=nX: e.tensor_tensor(out=nX[:, :], in0=pb[q][:, 256:384],
                                                                          in1=cX[:, :], op=ALU.add),
                          [PB(q), cXk], [nXk])
                if RW_STOP in ('inv', 'M'):
                    continue
                adv(13, 'c')
                for (c, hh) in heads:
                    q = c * 2 + hh
                    sl = slice(64 * hh, 64 * hh + 64)
                    bC = 4 + q
                    S0, S0k = ST[c][cur][sl, :], ("ST", c, cur, hh)
                    mm_group(P, pb[bC][:, 0:64], [(ar[p][c][sl, j, 0:128], S0), (Mm[q][:, 256:384], vT[p][c][:, j, sl])],
                             [("arD", c, p), S0k, ("MmD", q), ("vTD", c, p)], [PB(bC)])
                    V(P, lambda e, q=q, bC=bC: e.tensor_copy(out=Wsb[q][:, :], in_=pb[bC][:, 0:64]), [PB(bC)],
                      [("WsbD", q)])
                for (c, hh) in heads:
                    q = c * 2 + hh
                    bC = 4 + q
                    PE(P, lambda e, q=q, bC=bC: e.matmul(pb[bC][:, 64:128], XF[q][:, :], Wsb[q][:, :], start=True,
                                                         stop=True), [("XFD", q), ("WsbD", q)], [PB(bC)])
                    A(P, lambda e, q=q, bC=bC: e.copy(out=Usb[q][:, :], in_=pb[bC][:, 64:128]), [PB(bC)], [("UsbD", q)])
                for (c, hh) in heads:
                    q = c * 2 + hh
                    sl = slice(64 * hh, 64 * hh + 64)
                    bC = 4 + q
                    S0, S0k = ST[c][cur][sl, :], ("ST", c, cur, hh)
                    mm_group(P, pb[bC][sl, 128:192], [(ident[sl, sl], S0), (btT[p][c][:, j, sl], Usb[q][:, :]),
                                                      (ktT[p][c][:, j, sl], vT[p][c][:, j, sl])],
                             [S0k, ("btTD", c, p), ("UsbD", q), ("ktTD", c, p), ("vTD", c, p), "cst"], [PB(bC)])
                    A(P, lambda e, c=c, sl=sl, bC=bC: e.activation(
                        out=ST[c][nxt][sl, :], in_=pb[bC][sl, 128:192], func=AF.Identity,
                        scale=Gi[p][c][sl, j * 128 + 127:j * 128 + 128]), [PB(bC), ("GiD", c, p)], [("ST", c, nxt, hh)])
                    mm_group(P, pb[q][sl, 256:384], [(S0, ar[p][c][sl, j, 128:256]), (Usb[q][:, :], Mm[q][:, 128:256]),
                                                     (vT[p][c][:, j, sl], Mm[q][:, 384:512])],
                             [S0k, ("arD", c, p), ("UsbD", q), ("MmD", q), ("vTD", c, p)], [PB(q)])
                    V(P, lambda e, c=c, sl=sl, q=q: e.tensor_copy(out=ysb[c][sl, tk], in_=pb[q][sl, 256:384]),
                      [PB(q)], [("ysbD", c)])
            exhaust(pgen[0])
            for c in range(2):
                b1, b2 = c * 2, c * 2 + 1
                PE(P, lambda e, b1=b1: e.matmul(pb[b1][:, :], bd_ones, ysb[c][:, :], start=True, stop=True),
                   [("ysbD", c), "cst"], [PB(b1)])
                V(P, lambda e, b1=b1: e.scalar_tensor_tensor(out=kk[c][:, :], in0=pb[b1][:, :], scalar=-1.0 / 64,
                                                             in1=ysb[c][:, :], op0=ALU.mult, op1=ALU.add),
                  [PB(b1), ("ysbD", c)], [("kkD", c)])
                A(P, lambda e: e.activation(out=kkn[c][:, :], in_=kk[c][:, :], func=AF.Square), [("kkD", c)],
                  [("kknD", c)])
                PE(P, lambda e, b2=b2: e.matmul(pb[b2][:, :], bd_ones, kkn[c][:, :], start=True, stop=True),
                   [("kknD", c), "cst"], [PB(b2)])
                A(P, lambda e, b2=b2: e.activation(out=kkn[c][:, :], in_=pb[b2][:, :], func=AF.Sqrt, scale=1.0 / 64,
                                                   bias=GN_EPS), [PB(b2)], [("kknD", c)])
                V(P, lambda e: e.reciprocal(out=kkn[c][:, :], in_=kkn[c][:, :]), [("kknD", c)], [("kknD", c)])
                V(P, lambda e: e.scalar_tensor_tensor(out=kk[c][:, :], in0=kk[c][:, :], scalar=ln_g[:, c:c + 1],
                                                      in1=kkn[c][:, :], op0=ALU.mult, op1=ALU.mult),
                  [("kkD", c), ("kknD", c), "rpD"], [("kkD", c)])
                V(P, lambda e: e.scalar_tensor_tensor(out=kk[c][:, :], in0=kk[c][:, :], scalar=ln_b[:, c:c + 1],
                                                      in1=bon[p][c][:, :], op0=ALU.add, op1=ALU.add),
                  [("kkD", c), ("bonD", c, p), "rpD"], [("kkD", c)])
                V(P, lambda e: e.tensor_tensor(out=yo[c][:, :], in0=kk[c][:, :], in1=gtc[p][c][:, :], op=ALU.mult),
                  [("kkD", c), ("gtcD", c, p)], [("yoD", c)])
                P.dma("sync", mixS[768 + c * 128:768 + (c + 1) * 128, n * 512:(n + 1) * 512], yo[c][:, :],
                      reads=[("yoD", c)], writes=[("mixS", "r", c, n)])
        P.barrier()


def ffn_phase(nc, P, l, E):
    sb, pb, PB, xS, rr, ring = E["sb"], E["pb"], E["PB"], E["xS"], E["rr"], E["ring"]
    gm2, sh2, gt2 = E["gm2"], E["sh2"], E["gt2"]
    rms_h, load_cast, ident, cstt = E["rms_h"], E["load_cast"], E["ident"], E["cstt"]
    is_moe = (l % 2 == 1)
    li = l // 2
    TB = 1024
    NTB = T // TB
    if is_moe:
        experts = [(E["moe_g"][li, e], E["moe_u"][li, e], E["moe_d"][li, e]) for e in range(NE)]
        ff = MFF
    else:
        experts = [(E["ffn_g"][li], E["ffn_u"][li], E["ffn_d"][li])]
        ff = DFF
    nfg = ff // 256
    with contextlib.ExitStack() as st:
        xacc = sb("xacc", [128, 8, TB], stack=st)
        h2 = sb("h2", [128, 8, TB], BF16, stack=st)
        tmp = {"sq": sb("sqF", [128, 8, 512], BF16, stack=st), "rstd": sb("rstdF", [128, 512], stack=st),
               "t2": [sb(f"t2F{i}", [128, 512], stack=st) for i in range(2)]}
        stg = [sb(f"stgF{i}", [128, 2048], stack=st) for i in range(4)]
        wg = [sb(f"wgF{i}", [128, 8, 256], BF16, stack=st) for i in range(2)]
        wu = [sb(f"wuF{i}", [128, 8, 256], BF16, stack=st) for i in range(2)]
        wd = [sb(f"wdF{i}", [128, 2, 1024], BF16, stack=st) for i in range(3)]
        sg = [sb(f"sgF{i}", [128, 512], stack=st) for i in range(2)]
        a_ = [sb(f"aF{i}", [128, 2, 512], BF16, stack=st) for i in range(3)]
        if is_moe:
            hf = sb("hfF", [128, 8, 512], stack=st)
            rt = sb("rtF", [128, 64], stack=st)
            P.dma("sync", rt[:, :], E["moe_r"][li], writes=["rtF"])
            lg = sb("lgF", [128, 4, 8], stack=st)
            mx8 = sb("mx8F", [128, 4, 8], stack=st)
            ngm = sb("ngmF", [128, 4], stack=st)
            ex = sb("exF", [128, 4, 8], stack=st)
            msk = sb("mskF", [128, 4, 8], stack=st)
            den = sb("denF", [128, 4], stack=st)
            gate = sb("gateF", [128, 4, 8], stack=st)
            gT = sb("gTF", [8, TB], stack=st)
            gbc = [sb(f"gbcF{i}", [128, TB], stack=st) for i in range(2)]
            tt = [sb(f"ttF{i}", [128, 512], stack=st) for i in range(2)]
        cnt = 0
        pending = []
        for tb in range(NTB):
            t0 = tb * TB
            for k in range(8):
                for half in range(2):
                    P.dma("sync" if k % 2 == 0 else "gpsimd", xacc[:, k, half * 512:(half + 1) * 512],
                          xS[k * 128:(k + 1) * 128, t0 + half * 512:t0 + (half + 1) * 512],
                          writes=[("xacc", k, half)])
            for half in range(2):
                hs = slice(half * 512, (half + 1) * 512)
                xk = [("xacc", k, half) for k in range(8)]
                if not is_moe:
                    rms_h(xacc[:, :, hs], xk, h2[:, :, hs], ("h2", half), gm2, sh2, tmp)
                else:
                    rms_h(xacc[:, :, hs], xk, h2[:, :, hs], ("h2", half), gm2, sh2, tmp, hf=hf, hfkey="hfF")
                    for jb in range(4):
                        mm_group(P, pb[6][:, jb * 8:(jb + 1) * 8],
                                 [(hf[:, k, jb * 128:(jb + 1) * 128], rt[:, k * 8:(k + 1) * 8]) for k in range(8)],
                                 ["hfF", "rtF"], [PB(6)])
                    V(P, lambda e: e.tensor_copy(out=lg[:, :, :], in_=pb[6][:, 0:32].rearrange("p (j e) -> p j e", j=4)),
                      [PB(6)], ["lgF"])
                    for jb in range(4):
                        V(P, lambda e, jb=jb: e.max(out=mx8[:, jb, :], in_=lg[:, jb, :]), ["lgF"], ["mx8F"])
                    V(P, lambda e: e.tensor_scalar(out=ngm[:, :], in0=mx8[:, :, 0], scalar1=-1.0, scalar2=None,
                                                   op0=ALU.mult), ["mx8F"], ["ngmF"])
                    for jb in range(4):
                        A(P, lambda e, jb=jb: e.activation(out=ex[:, jb, :], in_=lg[:, jb, :], func=AF.Exp,
                                                           bias=ngm[:, jb:jb + 1]), ["lgF", "ngmF"], ["exF"])
                        V(P, lambda e, jb=jb: e.tensor_scalar(out=msk[:, jb, :], in0=lg[:, jb, :],
                                                              scalar1=mx8[:, jb, 1:2], scalar2=None, op0=ALU.is_ge),
                          ["lgF", "mx8F"], ["mskF"])
                    V(P, lambda e: e.tensor_tensor(out=ex[:, :, :], in0=ex[:, :, :], in1=msk[:, :, :], op=ALU.mult),
                      ["exF", "mskF"], ["exF"])
                    V(P, lambda e: e.tensor_reduce(out=den[:, :], in_=ex[:, :, :], axis=mybir.AxisListType.X,
                                                   op=ALU.add), ["exF"], ["denF"])
                    V(P, lambda e: e.reciprocal(out=den[:, :], in_=den[:, :]), ["denF"], ["denF"])
                    for jb in range(4):
                        V(P, lambda e, jb=jb: e.tensor_scalar(out=gate[:, jb, :], in0=ex[:, jb, :],
                                                              scalar1=den[:, jb:jb + 1], scalar2=None, op0=ALU.mult),
                          ["exF", "denF"], ["gateF"])
                    for jb in range(4):
                        PE(P, lambda e, jb=jb: e.transpose(pb[7][0:8, jb * 128:(jb + 1) * 128], gate[:, jb, :], ident),
                           ["gateF", "cst"], [PB(7)])
                    A(P, lambda e, hs=hs: e.copy(out=gT[:, hs], in_=pb[7][0:8, :]), [PB(7)], ["gTF"])
            units = [(e_i, fg) for e_i in range(len(experts)) for fg in range(nfg)]

            def load_unit(u):
                e_l, fg_l = units[u]
                Wg_, Wu_, Wd_ = experts[e_l]
                fs_ = slice(fg_l * 256, (fg_l + 1) * 256)
                load_cast(wg[u % 2], ("wgF", u % 2), Wg_[:, fs_], 8, 256, stg, "stgF", cast_eng="scalar")
                load_cast(wu[u % 2], ("wuF", u % 2), Wu_[:, fs_], 8, 256, stg, "stgF", cast_eng="scalar")
                load_cast(wd[u % 3], ("wdF", u % 3), Wd_[fs_, :], 2, 1024, stg, "stgF", cast_eng="scalar")

            load_unit(0)
            for u, (e_i, fg) in enumerate(units):
                if u + 1 < len(units):
                    load_unit(u + 1)
                if is_moe and fg == 0:
                    gb = gbc[e_i % 2]
                    gk = ("gbcF", e_i % 2)
                    for half in range(2):
                        hs = slice(half * 512, (half + 1) * 512)
                        PE(P, lambda e, hs=hs, e_i=e_i: e.matmul(
                            pb[7][:, :], cstt[0:8, C_SEL8 + e_i * 128:C_SEL8 + (e_i + 1) * 128], gT[:, hs],
                            start=True, stop=True), ["gTF", "cst"], [PB(7)])
                        A(P, lambda e, hs=hs, gb=gb: e.copy(out=gb[:, hs], in_=pb[7][:, :]), [PB(7)], [gk])
                if True:
                    i = u % 2
                    i3 = u % 3
                    for half in range(2):
                        hs = slice(half * 512, (half + 1) * 512)
                        ai = rr("aF", 3)
                        for j in range(2):
                            bg = (cnt % 2) * 2
                            cnt += 1
                            js = slice(j * 128, (j + 1) * 128)
                            mm_group(P, pb[bg][:, :], [(wg[i][:, k, js], h2[:, k, hs]) for k in range(8)],
                                     [("wgF", i), ("h2", half)], [PB(bg)])
                            mm_group(P, pb[bg + 1][:, :], [(wu[i][:, k, js], h2[:, k, hs]) for k in range(8)],
                                     [("wuF", i), ("h2", half)], [PB(bg + 1)])
                            si = rr("sgF", 2)
                            A(P, lambda e, si=si, bg=bg: e.activation(out=sg[si][:, :], in_=pb[bg][:, :], func=AF.Silu),
                              [PB(bg)], [("sgF", si)])
                            if not is_moe:
                                V(P, lambda e, si=si, bg=bg, ai=ai, j=j: e.tensor_tensor(
                                    out=a_[ai][:, j, :], in0=sg[si][:, :], in1=pb[bg + 1][:, :], op=ALU.mult),
                                  [("sgF", si), PB(bg + 1)], [("aF", ai, j)])
                            else:
                                ti = rr("ttF", 2)
                                V(P, lambda e, si=si, bg=bg, ti=ti: e.tensor_tensor(
                                    out=tt[ti][:, :], in0=sg[si][:, :], in1=pb[bg + 1][:, :], op=ALU.mult),
                                  [("sgF", si), PB(bg + 1)], [("ttF", ti)])
                                G(P, lambda e, ti=ti, ai=ai, j=j, hs=hs, gb=gb: e.tensor_tensor(
                                    out=a_[ai][:, j, :], in0=tt[ti][:, :], in1=gb[:, hs], op=ALU.mult),
                                  [("ttF", ti), gk], [("aF", ai, j)])
                            if pending:
                                pending[0](range(4 * j, 4 * j + 4))
                                if j == 1:
                                    pending.pop()
                        def down(ms, i=i3, ai=ai, hs=hs, half=half):
                            for m in ms:
                                bd = 4 + (m % 4)
                                mm_group(P, pb[bd][:, :], [(wd[i][:, j, m * 128:(m + 1) * 128], a_[ai][:, j, :])
                                                           for j in range(2)],
                                         [("wdF", i), ("aF", ai, 0), ("aF", ai, 1)], [PB(bd)])
                                V(P, lambda e, m=m, bd=bd, hs=hs: e.scalar_tensor_tensor(
                                    out=xacc[:, m, hs], in0=pb[bd][:, :], scalar=gt2[:, m:m + 1], in1=xacc[:, m, hs],
                                    op0=ALU.mult, op1=ALU.add), [PB(bd), ("xacc", m, half), "mod"], [("xacc", m, half)])
                        pending.append(down)
            if pending:
                pending.pop()(range(8))
            for k in range(8):
                for half in range(2):
                    P.dma("sync" if k % 2 == 0 else "gpsimd",
                          xS[k * 128:(k + 1) * 128, t0 + half * 512:t0 + (half + 1) * 512],
                          xacc[:, k, half * 512:(half + 1) * 512], reads=[("xacc", k, half)],
                          writes=[("xS", k, tb, half)])
        P.barrier()


F_KEYS = ("ffn_g", "ffn_u", "ffn_d", "moe_r", "moe_g", "moe_u", "moe_d")


def kernel(**I):
    shared = prep_shared(I)
    zshared = {k: np.zeros_like(v) for k, v in shared.items()}
    nc = build()
    active = [0, 1, 4, 5]
    in_maps = []
    for c in range(8):
        if c in active:
            in_maps.append(dict(shared, **prep_core(I, active.index(c))))
        else:
            z = prep_core(I, 0)
            in_maps.append(dict(zshared, **{k: np.zeros_like(v) for k, v in z.items()}))
    res = run_bass_kernel_spmd(nc, in_maps, core_ids=list(range(8)))
    out = np.stack([np.asarray(res.results[c]["outT"]).T for c in active])
    return np.ascontiguousarray(out, np.float32)
```

```python
import os
import contextlib
import numpy as np
import concourse.bass as bass
import concourse.mybir as mybir
from concourse.bass_utils import run_bass_kernel_spmd

F32 = mybir.dt.float32
BF16 = mybir.dt.bfloat16
I32 = mybir.dt.int32
AF = mybir.ActivationFunctionType
ALU = mybir.AluOpType

D = 1024
T = 4096
L = 4
TT = 512
NT = T // TT
DFF = 2816
MFF = 3584
NE = 8
NCH_IN = 16
RMS_EPS = 1e-6
LN_EPS = 1e-5
GN_EPS = 64e-5
SCALE_QK = 96.0 ** -0.5
N_CORES = 4

DBG = os.environ.get("KDBG", "")


class Prog:
    CE = ("vector", "scalar", "gpsimd", "tensor")

    def __init__(self, nc):
        self.nc = nc
        self.eng = {"vector": nc.vector, "scalar": nc.scalar, "gpsimd": nc.gpsimd,
                    "tensor": nc.tensor, "sync": nc.sync}
        self.sem = {e: nc.alloc_semaphore("p_" + e) for e in self.CE}
        self.cnt = {e: 0 for e in self.CE}
        self.dsem = {q: [nc.alloc_semaphore(f"d_{q}{i}") for i in range(8)] for q in ("sync", "gpsimd")}
        self.dcnt = {}
        self.drr = {"sync": 0, "gpsimd": 0}
        self.semobj = {}
        for e in self.CE:
            self.semobj[self.sem[e].name if hasattr(self.sem[e], "name") else e] = self.sem[e]
        self.seen = {e: {} for e in self.eng}
        self.lastw = {}
        self.readers = {}
        self.nops = 0

    def _need(self, eng, deps):
        for (sid, sem, val) in deps:
            if self.seen[eng].get(sid, 0) < val:
                self.eng[eng].wait_ge(sem, val)
                self.seen[eng][sid] = val
                self.nops += 1

    def _collect(self, reads, writes):
        deps = {}
        def add(d):
            if d is None:
                return
            sid, sem, val = d
            if sid not in deps or deps[sid][2] < val:
                deps[sid] = d
        for k in reads:
            add(self.lastw.get(k))
        for k in writes:
            add(self.lastw.get(k))
            for d in self.readers.get(k, {}).values():
                add(d)
        return list(deps.values())

    def _record(self, reads, writes, dep):
        sid = dep[0]
        for k in reads:
            r = self.readers.setdefault(k, {})
            r[sid] = dep
        for k in writes:
            self.lastw[k] = dep
            self.readers[k] = {}

    def op(self, eng, fn, reads=(), writes=()):
        deps = self._collect(reads, writes)
        if eng == "tensor":
            deps = [d for d in deps if d[0] != "tensor"]
        self._need(eng, deps)
        ins = fn(self.eng[eng])
        self.cnt[eng] += 1
        ins.then_inc(self.sem[eng], 1)
        self.nops += 1
        dep = (eng, self.sem[eng], self.cnt[eng])
        self.seen[eng][eng] = max(self.seen[eng].get(eng, 0), 0)
        self._record(reads, writes, dep)
        return dep

    def dma(self, q, out, in_, reads=(), writes=()):
        deps = self._collect(reads, writes)
        i = self.drr[q]
        self.drr[q] = (i + 1) % len(self.dsem[q])
        sem = self.dsem[q][i]
        sid = f"d_{q}{i}"
        n = self.dcnt.get(sid, 0)
        if n > 0:
            deps.append((sid, sem, 16 * n))
        self._need(q, deps)
        self.eng[q].dma_start(out=out, in_=in_).then_inc(sem, 16)
        self.nops += 1
        self.dcnt[sid] = n + 1
        dep = (sid, sem, 16 * (n + 1))
        self._record(reads, writes, dep)
        return dep

    def barrier(self):
        alld = [(e, self.sem[e], self.cnt[e]) for e in self.CE if self.cnt[e] > 0]
        for q in self.dsem:
            for i, sem in enumerate(self.dsem[q]):
                sid = f"d_{q}{i}"
                if self.dcnt.get(sid, 0) > 0:
                    alld.append((sid, sem, 16 * self.dcnt[sid]))
        for e in self.eng:
            self._need(e, [d for d in alld if d[0] != e])
        self.lastw = {}
        self.readers = {}


def V(P, fn, r=(), w=()):
    return P.op("vector", fn, r, w)


def A(P, fn, r=(), w=()):
    return P.op("scalar", fn, r, w)


def G(P, fn, r=(), w=()):
    return P.op("gpsimd", fn, r, w)


def PE(P, fn, r=(), w=()):
    return P.op("tensor", fn, r, w)


class Ctx:
    pass


def mm_group(P, out, pairs, r, w):
    def fn(e):
        ins = None
        n = len(pairs)
        for i, (l, rr) in enumerate(pairs):
            ins = e.matmul(out, l, rr, start=(i == 0), stop=(i == n - 1))
        return ins
    return PE(P, fn, r, w)


C_IDENT, C_ONES, C_BD, C_SU, C_IU, C_SL, C_MROW, C_RMAT, C_SEL65, C_INVF, C_SEL8, C_W = (
    0, 128, 256, 384, 512, 640, 768, 1280, 1376, 1440, 1472, 2496)


def make_consts():
    c = np.zeros((128, C_W), np.float32)
    r = np.arange(128)
    c[:, C_IDENT:C_IDENT + 128] = np.eye(128)
    c[:, C_ONES:C_ONES + 128] = 1.0
    bd = (r[:, None] // 64) == (r[None, :] // 64)
    c[:, C_BD:C_BD + 128] = bd
    su = r[:, None] < r[None, :]
    iu = r[:, None] <= r[None, :]
    c[:, C_SU:C_SU + 128] = su
    c[:, C_IU:C_IU + 128] = iu
    c[:, C_SL:C_SL + 128] = su.T
    c[:, C_MROW:C_MROW + 512] = np.concatenate([su, iu, su, iu], axis=1)
    rm = np.zeros((96, 96), np.float32)
    for i in range(16):
        rm[80 + i, 64 + i] = -1.0
        rm[64 + i, 80 + i] = 1.0
    c[0:96, C_RMAT:C_RMAT + 96] = rm
    c[64, C_SEL65:C_SEL65 + 64] = 1.0
    inv = (10000.0 ** (-np.arange(0, 32, 2, dtype=np.float32) / np.float32(32))).astype(np.float32)
    c[64:80, C_INVF] = inv
    c[80:96, C_INVF] = inv
    for e in range(8):
        c[e, C_SEL8 + e * 128:C_SEL8 + (e + 1) * 128] = 1.0
    return c


def pk(v, n):
    return np.ascontiguousarray(np.asarray(v, np.float32).reshape(n, 128).T)


def prep_shared(I):
    S = {}
    S["ada_w"] = np.ascontiguousarray(I["ada_w"], np.float32)
    S["ada_b"] = pk(I["ada_b"], 48)
    S["ada_lb"] = np.stack([pk(I["ada_layer_bias"][l], 48) for l in range(L)])
    S["n1g"] = np.stack([pk(I["norm1_g"][l], 8) for l in range(L)])
    S["n2g"] = np.stack([pk(I["norm2_g"][l], 8) for l in range(L)])
    w = I["w_in"]
    wi = np.zeros((L, D, NCH_IN * 128), np.float32)
    wi[:, :, 0:512] = w[:, :, 0:512]
    wi[:, :, 512:768] = w[:, :, 512:768]
    wi[:, :, 768:896] = w[:, :, 768:896]
    wi[:, :, 896 + 64:896 + 96] = w[:, :, 896:928]
    rb = 928
    wi[:, :, 1024:1792] = w[:, :, rb:rb + 768]
    wi[:, :, 1792:1920] = w[:, :, rb + 768:rb + 896]
    wi[:, :, 1920:2048] = w[:, :, rb + 896:rb + 1024]
    S["w_in"] = wi
    S["w_out"] = np.ascontiguousarray(I["w_out"], np.float32)
    S["conv_w"] = np.ascontiguousarray(I["conv_w"].reshape(L, 31, 2, 128).transpose(0, 2, 3, 1), np.float32)
    S["conv_p"] = np.stack([np.concatenate([pk(I["conv_b"][l], 2), pk(I["conv_ln_g"][l], 2),
                                            pk(I["conv_ln_b"][l], 2)], axis=1) for l in range(L)])
    mp = np.zeros((L, 128, 5), np.float32)
    for l in range(L):
        mp[l, :, 0:2] = pk(I["mla_q_norm_g"][l], 2)
        mp[l, :, 2] = I["mla_kv_norm_g"][l]
        mp[l, 0:96, 3] = I["qk_norm_q"][l]
        mp[l, 0:96, 4] = I["qk_norm_k"][l]
    S["mla_p"] = mp
    S["w_uq"] = np.ascontiguousarray(I["mla_w_uq"], np.float32)
    wkv = I["mla_w_ukv"].reshape(L, 128, 8, 128)
    S["w_k"] = np.ascontiguousarray(wkv[:, :, :, 0:64].reshape(L, 128, 512), np.float32)
    S["w_v"] = np.ascontiguousarray(wkv[:, :, :, 64:128].reshape(L, 128, 512), np.float32)
    S["rw_mu"] = np.stack([pk(I["rwkv_mu"][l], 8) for l in range(L)])
    S["rw_p"] = np.stack([np.concatenate([pk(I[k][l].reshape(-1), 2) for k in
                                          ("rwkv_w0", "rwkv_a0", "rwkv_k_k", "rwkv_k_a", "rwkv_r_k",
                                           "rwkv_ln_g", "rwkv_ln_b")], axis=1) for l in range(L)])
    S["rw_wup"] = np.ascontiguousarray(np.concatenate([I["rwkv_w_up"], I["rwkv_a_up"]], axis=1), np.float32)
    S["rw_gup"] = np.ascontiguousarray(I["rwkv_g_up"], np.float32)
    S["ffn_g"] = np.ascontiguousarray(I["ffn_w_gate"], np.float32)
    S["ffn_u"] = np.ascontiguousarray(I["ffn_w_up"], np.float32)
    S["ffn_d"] = np.ascontiguousarray(I["ffn_w_down"], np.float32)
    S["moe_r"] = np.ascontiguousarray(I["moe_router"].reshape(2, 8, 128, 8).transpose(0, 2, 1, 3).reshape(2, 128, 64),
                                      np.float32)
    S["moe_g"] = np.ascontiguousarray(I["moe_w_gate"], np.float32)
    S["moe_u"] = np.ascontiguousarray(I["moe_w_up"], np.float32)
    S["moe_d"] = np.ascontiguousarray(I["moe_w_down"], np.float32)
    S["cst"] = make_consts()
    return S


def prep_core(I, b):
    return {"xT": np.ascontiguousarray(I["x"][b].T, np.float32),
            "c_pk": pk(I["c"][b], 8),
            "pos": np.ascontiguousarray(I["positions"][b].reshape(1, T), np.int32)}


def build(n_layers=L, phases="ABCDEF", dbg_out=()):
    nc = bass.Bass("TRN2", target_bir_lowering=False)
    P = Prog(nc)

    def din(name, shape, dt=F32):
        return nc.dram_tensor(name, list(shape), dt, kind="ExternalInput").ap()

    def dscr(name, shape, dt=F32):
        kind = "ExternalOutput" if name in dbg_out else "Internal"
        return nc.dram_tensor(name, list(shape), dt, kind=kind).ap()

    xT_in = din("xT", [D, T])
    c_in = din("c_pk", [128, 8])
    pos_in = din("pos", [1, T], I32)
    ada_w = din("ada_w", [D, 6 * D])
    ada_b = din("ada_b", [128, 48])
    ada_lb = din("ada_lb", [L, 128, 48])
    n1g = din("n1g", [L, 128, 8])
    n2g = din("n2g", [L, 128, 8])
    w_in = din("w_in", [L, D, NCH_IN * 128])
    w_out = din("w_out", [L, D, D])
    conv_w = din("conv_w", [L, 2, 128, 31])
    conv_p = din("conv_p", [L, 128, 6])
    mla_p = din("mla_p", [L, 128, 5])
    w_uq = din("w_uq", [L, 256, 768])
    w_k = din("w_k", [L, 128, 512])
    w_v = din("w_v", [L, 128, 512])
    rw_mu = din("rw_mu", [L, 128, 8])
    rw_p = din("rw_p", [L, 128, 14])
    rw_wup = din("rw_wup", [L, 128, 256])
    rw_gup = din("rw_gup", [L, 128, 256])
    if "F" in phases:
        ffn_g = din("ffn_g", [2, D, DFF])
        ffn_u = din("ffn_u", [2, D, DFF])
        ffn_d = din("ffn_d", [2, DFF, D])
        moe_r = din("moe_r", [2, 128, 64])
        moe_g = din("moe_g", [2, NE, D, MFF])
        moe_u = din("moe_u", [2, NE, D, MFF])
        moe_d = din("moe_d", [2, NE, MFF, D])
    cst = din("cst", [128, C_W])

    xS = nc.dram_tensor("outT", [D, T], F32, kind="ExternalOutput").ap()
    uS = dscr("uS", [NCH_IN * 128, T])
    mixS = dscr("mixS", [D, T], BF16)
    csS = dscr("csS", [2, 96, T])
    qS = dscr("qS", [8, 96, T], BF16)
    kS = dscr("kS", [8, 96, T], BF16)

    es = contextlib.ExitStack()

    uid = [0]

    def sb(name, shape, dt=F32, stack=None):
        uid[0] += 1
        return (stack or es).enter_context(nc.sbuf_tensor(f"{name}_{uid[0]}", list(shape), dt))

    pb = [es.enter_context(nc.psum_tensor(f"pb{i}", [128, 512], F32)) for i in range(8)]

    def PB(i):
        return ("pb", i)

    cstt = sb("cstt", [128, C_W])
    P.dma("sync", cstt[:, :], cst[:, :], writes=["cst"])
    ident = cstt[:, C_IDENT:C_IDENT + 128]
    ones_f = cstt[:, C_ONES:C_ONES + 128]
    bd_ones = cstt[:, C_BD:C_BD + 128]
    m_sl = cstt[:, C_SL:C_SL + 128]
    m_row = cstt[:, C_MROW:C_MROW + 512]
    rmat = cstt[0:96, C_RMAT:C_RMAT + 96]
    sel65 = cstt[0:65, C_SEL65:C_SEL65 + 64]
    invf = cstt[0:96, C_INVF:C_INVF + 1]
    cbf = sb("cbf", [128, 384], BF16)
    V(P, lambda e: e.tensor_copy(out=cbf[:, 0:256], in_=cstt[:, 0:256]), ["cst"], ["cbf"])
    V(P, lambda e: e.tensor_copy(out=cbf[:, 256:384], in_=cstt[:, C_IU:C_IU + 128]), ["cst"], ["cbf"])
    ident_b = cbf[:, 0:128]
    ones_b = cbf[:, 128:256]
    tri_b = cbf[:, 256:384]
    zero_t = sb("zero_t", [128, 128])
    V(P, lambda e: e.memset(zero_t[:, :], 0.0), [], ["zero"])

    mod = sb("mod", [128, L, 48])
    modp = sb("modp", [128, L, 16])

    with contextlib.ExitStack() as st:
        cpk = sb("cpk", [128, 8], stack=st)
        scs = sb("scs", [128, 8], stack=st)
        P.dma("sync", cpk[:, :], c_in[:, :], writes=["cpk"])
        A(P, lambda e: e.activation(out=scs[:, :], in_=cpk[:, :], func=AF.Silu), ["cpk"], ["scs"])
        aw = [sb(f"aw{i}", [128, 6 * D], stack=st) for i in range(2)]
        for k in range(8):
            t = aw[k % 2]
            kk = f"aw{k % 2}"
            for j in range(4):
                P.dma("sync" if j % 2 == 0 else "gpsimd", t[:, j * 1536:(j + 1) * 1536],
                      ada_w[k * 128:(k + 1) * 128, j * 1536:(j + 1) * 1536], writes=[kk])

            def fn(e, t=t, k=k):
                ins = None
                for m in range(48):
                    ins = e.matmul(pb[0][:, k * 48 + m:k * 48 + m + 1], t[:, m * 128:(m + 1) * 128],
                                   scs[:, k:k + 1], start=True, stop=True)
                return ins
            PE(P, fn, [kk, "scs"], [PB(0)])
        cond = sb("cond", [128, 48], stack=st)
        ab = sb("ab", [128, 48], stack=st)
        alb = sb("alb", [128, L, 48], stack=st)
        P.dma("sync", ab[:, :], ada_b[:, :], writes=["ab"])
        for l in range(L):
            P.dma("sync", alb[:, l, :], ada_lb[l], writes=["alb"])
        V(P, lambda e: e.tensor_tensor(out=cond[:, :], in0=pb[0][:, 0:48], in1=ab[:, :], op=ALU.add),
          [PB(0), "ab"], ["cond"])
        for k in range(1, 8):
            V(P, lambda e, k=k: e.tensor_tensor(out=cond[:, :], in0=pb[0][:, k * 48:(k + 1) * 48], in1=cond[:, :],
                                                op=ALU.add), [PB(0), "cond"], ["cond"])
        g1t = sb("g1t", [128, L, 8], stack=st)
        g2t = sb("g2t", [128, L, 8], stack=st)
        for l in range(L):
            P.dma("sync", g1t[:, l, :], n1g[l], writes=["g1t"])
            P.dma("sync", g2t[:, l, :], n2g[l], writes=["g2t"])
        for l in range(L):
            V(P, lambda e, l=l: e.tensor_tensor(out=mod[:, l, :], in0=cond[:, :], in1=alb[:, l, :], op=ALU.add),
              ["cond", "alb"], ["mod"])
            V(P, lambda e, l=l: e.scalar_tensor_tensor(out=modp[:, l, 0:8], in0=mod[:, l, 8:16], scalar=1.0,
                                                       in1=g1t[:, l, :], op0=ALU.add, op1=ALU.mult),
              ["mod", "g1t"], ["modp"])
            V(P, lambda e, l=l: e.scalar_tensor_tensor(out=modp[:, l, 8:16], in0=mod[:, l, 32:40], scalar=1.0,
                                                       in1=g2t[:, l, :], op0=ALU.add, op1=ALU.mult),
              ["mod", "g2t"], ["modp"])
        posi = sb("posi", [96, T], I32, stack=st)
        ang = sb("ang", [96, T], stack=st)
        tb = sb("tb", [96, T], stack=st)
        P.dma("sync", posi[:, :], pos_in[0:1, :].partition_broadcast(96), writes=["posi"])
        V(P, lambda e: e.tensor_copy(out=ang[:, :], in_=posi[:, :]), ["posi"], ["ang"])
        V(P, lambda e: e.tensor_scalar(out=ang[:, :], in0=ang[:, :], scalar1=invf, scalar2=None, op0=ALU.mult),
          ["ang", "cst"], ["ang"])
        TWO_PI = 2.0 * np.pi
        ki = sb("ki", [96, T], I32, stack=st)
        kf = sb("kf", [96, T], stack=st)
        for ci, shift in ((1, 0.0), (0, np.pi / 2)):
            V(P, lambda e, shift=shift: e.tensor_scalar(out=tb[:, :], in0=ang[:, :], scalar1=float(shift),
                                                        scalar2=float(1.0 / TWO_PI), op0=ALU.add, op1=ALU.mult),
              ["ang"], ["tb"])
            V(P, lambda e: e.tensor_copy(out=ki[:, :], in_=tb[:, :]), ["tb"], ["ki"])
            V(P, lambda e: e.tensor_copy(out=kf[:, :], in_=ki[:, :]), ["ki"], ["kf"])
            V(P, lambda e, shift=shift: e.tensor_scalar(out=tb[:, :], in0=ang[:, :], scalar1=float(shift),
                                                        scalar2=None, op0=ALU.add), ["ang"], ["tb"])
            V(P, lambda e: e.scalar_tensor_tensor(out=tb[:, :], in0=kf[:, :], scalar=float(-TWO_PI), in1=tb[:, :],
                                                  op0=ALU.mult, op1=ALU.add), ["kf", "tb"], ["tb"])
            V(P, lambda e: e.tensor_scalar(out=kf[:, :], in0=tb[:, :], scalar1=float(np.pi), scalar2=None,
                                           op0=ALU.is_gt), ["tb"], ["kf"])
            V(P, lambda e: e.scalar_tensor_tensor(out=tb[:, :], in0=kf[:, :], scalar=float(-TWO_PI), in1=tb[:, :],
                                                  op0=ALU.mult, op1=ALU.add), ["kf", "tb"], ["tb"])
            V(P, lambda e: e.tensor_scalar(out=kf[:, :], in0=tb[:, :], scalar1=float(-np.pi), scalar2=None,
                                           op0=ALU.is_lt), ["tb"], ["kf"])
            V(P, lambda e: e.scalar_tensor_tensor(out=tb[:, :], in0=kf[:, :], scalar=float(TWO_PI), in1=tb[:, :],
                                                  op0=ALU.mult, op1=ALU.add), ["kf", "tb"], ["tb"])
            V(P, lambda e: e.tensor_scalar(out=tb[:, :], in0=tb[:, :], scalar1=3.1415925, scalar2=-3.1415925,
                                           op0=ALU.min, op1=ALU.max), ["tb"], ["tb"])
            A(P, lambda e: e.activation(out=tb[:, :], in_=tb[:, :], func=AF.Sin), ["tb"], ["tb"])
            P.dma("sync", csS[ci], tb[:, :], reads=["tb"], writes=[("csS", ci)])
        xc = [sb(f"xc{i}", [128, T], stack=st) for i in range(2)]
        for k in range(8):
            t = xc[k % 2]
            q = "sync" if k % 2 == 0 else "gpsimd"
            P.dma(q, t[:, :], xT_in[k * 128:(k + 1) * 128, :], writes=[f"xc{k % 2}"])
            P.dma(q, xS[k * 128:(k + 1) * 128, :], t[:, :], reads=[f"xc{k % 2}"], writes=[("xS", k)])
        P.barrier()

    ring = {}

    def rr(name, n):
        i = ring.get(name, 0)
        ring[name] = i + 1
        return i % n

    def load_cast(dst3, dkey, src2d, nk, ncols, stg, stgname, cast_eng="gpsimd", dma_q=None):
        per = max(1, 2048 // ncols)
        k = 0
        while k < nk:
            kk = min(per, nk - k)
            i = rr(stgname, len(stg))
            s = stg[i]
            skey = (stgname, i)
            sv = s[:, 0:kk * ncols].rearrange("p (k c) -> p k c", k=kk)
            q = dma_q or ("sync" if (ring[stgname] % 2 == 0) else "gpsimd")
            P.dma(q, sv, src2d[k * 128:(k + kk) * 128, :].rearrange("(k p) c -> p k c", p=128), writes=[skey])
            eng = cast_eng if cast_eng != "alt" else ("gpsimd" if ring[stgname] % 2 == 0 else "vector")
            if eng == "scalar":
                A(P, lambda e, sv=sv, k=k, kk=kk: e.copy(out=dst3[:, k:k + kk, :], in_=sv), [skey], [dkey])
            else:
                P.op(eng, lambda e, sv=sv, k=k, kk=kk: e.tensor_copy(out=dst3[:, k:k + kk, :], in_=sv), [skey], [dkey])
            k += kk

    def rms_h(xt, xkey, hb, hkey, gm, sh, tmp, hf=None, hfkey=None):
        sq, rstd, t2 = tmp["sq"], tmp["rstd"], tmp["t2"]
        xkeys = xkey if isinstance(xkey, list) else [xkey] * 8
        A(P, lambda e: e.activation(out=sq[:, :, :], in_=xt, func=AF.Square), list(set(xkeys)), ["sq"])
        mm_group(P, pb[7][:, :], [(ones_b, sq[:, k, :]) for k in range(8)], ["sq", "cbf"], [PB(7)])
        A(P, lambda e: e.activation(out=rstd[:, :], in_=pb[7][:, :], func=AF.Sqrt, scale=1.0 / D, bias=RMS_EPS),
          [PB(7)], ["rstd"])
        V(P, lambda e: e.reciprocal(out=rstd[:, :], in_=rstd[:, :]), ["rstd"], ["rstd"])
        for k in range(8):
            i = rr("t2", 2)
            V(P, lambda e, k=k, i=i: e.scalar_tensor_tensor(out=t2[i][:, :], in0=xt[:, k, :], scalar=gm[:, k:k + 1],
                                                            in1=rstd[:, :], op0=ALU.mult, op1=ALU.mult),
              [xkeys[k], "rstd", "modp"], [("t2", i)])
            if hf is None:
                A(P, lambda e, k=k, i=i: e.activation(out=hb[:, k, :], in_=t2[i][:, :], func=AF.Identity,
                                                      bias=sh[:, k:k + 1]), [("t2", i), "mod"], [hkey])
            else:
                A(P, lambda e, k=k, i=i: e.activation(out=hf[:, k, :], in_=t2[i][:, :], func=AF.Identity,
                                                      bias=sh[:, k:k + 1]), [("t2", i), "mod"], [hfkey])
                G(P, lambda e, k=k: e.tensor_copy(out=hb[:, k, :], in_=hf[:, k, :]), [hfkey], [hkey])

    EPS_T = {}

    def eps_tile(val):
        return float(val)

    for l in range(n_layers):
        gm1, gm2 = modp[:, l, 0:8], modp[:, l, 8:16]
        sh1, gt1 = mod[:, l, 0:8], mod[:, l, 16:24]
        sh2, gt2 = mod[:, l, 24:32], mod[:, l, 40:48]

        if "A" in phases:
            with contextlib.ExitStack() as st:
                wA = sb("wA", [128, 8, 2048], BF16, stack=st)
                stg = [sb(f"stgA{i}", [128, 2048], stack=st) for i in range(3)]
                load_cast(wA, "wA", w_in[l], 8, 2048, stg, "stgA", cast_eng="alt")
                xtb = [sb(f"xtA{i}", [128, 8, 512], stack=st) for i in range(2)]
                hb = [sb(f"hbA{i}", [128, 8, 512], BF16, stack=st) for i in range(2)]
                tmp = {"sq": sb("sqA", [128, 8, 512], BF16, stack=st), "rstd": sb("rstdA", [128, 512], stack=st),
                       "t2": [sb(f"t2A{i}", [128, 512], stack=st) for i in range(2)]}
                uo = [sb(f"uoA{i}", [128, 512], stack=st) for i in range(4)]
                for n in range(NT):
                    xt = xtb[n % 2]
                    xkey = f"xtA{n % 2}"
                    for k in range(8):
                        P.dma("sync" if k % 2 == 0 else "gpsimd", xt[:, k, :],
                              xS[k * 128:(k + 1) * 128, n * 512:(n + 1) * 512], writes=[xkey])
                    h = hb[n % 2]
                    hkey = f"hbA{n % 2}"
                    rms_h(xt[:, :, :], xkey, h, hkey, gm1, sh1, tmp)
                    for m in range(NCH_IN):
                        bi = m % 4
                        mm_group(P, pb[bi][:, :], [(wA[:, k, m * 128:(m + 1) * 128], h[:, k, :]) for k in range(8)],
                                 ["wA", hkey], [PB(bi)])
                        oi = rr("uo", 4)
                        if m % 2 == 0:
                            V(P, lambda e, bi=bi, oi=oi: e.tensor_copy(out=uo[oi][:, :], in_=pb[bi][:, :]),
                              [PB(bi)], [("uo", oi)])
                        else:
                            A(P, lambda e, bi=bi, oi=oi: e.copy(out=uo[oi][:, :], in_=pb[bi][:, :]),
                              [PB(bi)], [("uo", oi)])
                        P.dma("sync", uS[m * 128:(m + 1) * 128, n * 512:(n + 1) * 512], uo[oi][:, :],
                              reads=[("uo", oi)], writes=[("uS", m, n)])
                P.barrier()

        if "B" in phases:
            with contextlib.ExitStack() as st:
                cw = sb("cw", [128, 2, 31], stack=st)
                cp = sb("cp", [128, 6], stack=st)
                for cc in range(2):
                    P.dma("sync", cw[:, cc, :], conv_w[l, cc], writes=["cw"])
                P.dma("sync", cp[:, :], conv_p[l], writes=["cp"])
                dg = sb("dg", [128, 62, 128], BF16, stack=st)
                for cc in range(2):
                    for k in range(31):
                        P.op("gpsimd" if k % 2 else "vector",
                             lambda e, cc=cc, k=k: e.tensor_scalar(out=dg[:, cc * 31 + k, :], in0=ident,
                                                                   scalar1=cw[:, cc, k:k + 1], scalar2=None,
                                                                   op0=ALU.mult), ["cw", "cst"], [("dg", cc, k)])
                at = [sb(f"atB{i}", [128, 542], stack=st) for i in range(2)]
                gtl = [sb(f"gtB{i}", [128, 542], stack=st) for i in range(2)]
                zb = [sb(f"zbB{i}", [128, 542], BF16, stack=st) for i in range(2)]
                zc = sb("zcB", [128, 2, 512], stack=st)
                xcB = sb("xcB", [128, 2, 512], stack=st)
                sqB = sb("sqB", [128, 2, 512], stack=st)
                rsB = sb("rsB", [128, 512], stack=st)
                tB = sb("tB", [128, 512], stack=st)
                yb = [sb(f"ybB{i}", [128, 512], BF16, stack=st) for i in range(2)]
                for n in range(NT):
                    for cc in range(2):
                        i = rr("atB", 2)
                        a_, g_, z_ = at[i], gtl[i], zb[i]
                        ak, gk, zk = ("atB", i), ("gtB", i), ("zbB", i)
                        if n == 0:
                            V(P, lambda e, a_=a_: e.memset(a_[:, 0:30], 0.0), [], [ak])
                            V(P, lambda e, g_=g_: e.memset(g_[:, 0:30], 0.0), [], [gk])
                            P.dma("sync", a_[:, 30:542], uS[cc * 128:(cc + 1) * 128, 0:512], writes=[ak])
                            P.dma("gpsimd", g_[:, 30:542], uS[(2 + cc) * 128:(3 + cc) * 128, 0:512], writes=[gk])
                        else:
                            P.dma("sync", a_[:, :], uS[cc * 128:(cc + 1) * 128, n * 512 - 30:(n + 1) * 512], writes=[ak])
                            P.dma("gpsimd", g_[:, :], uS[(2 + cc) * 128:(3 + cc) * 128, n * 512 - 30:(n + 1) * 512],
                                  writes=[gk])
                        A(P, lambda e, g_=g_: e.activation(out=g_[:, :], in_=g_[:, :], func=AF.Sigmoid), [gk], [gk])
                        V(P, lambda e, a_=a_, g_=g_, z_=z_: e.tensor_tensor(out=z_[:, :], in0=a_[:, :], in1=g_[:, :],
                                                                            op=ALU.mult), [ak, gk], [zk])
                        mm_group(P, pb[cc][:, :], [(dg[:, cc * 31 + k, :], z_[:, k:k + 512]) for k in range(31)],
                                 [zk] + [("dg", cc, k) for k in range(31)], [PB(cc)])
                        A(P, lambda e, cc=cc: e.activation(out=zc[:, cc, :], in_=pb[cc][:, :], func=AF.Identity,
                                                           bias=cp[:, cc:cc + 1]), [PB(cc), "cp"], ["zcB"])
                    mm_group(P, pb[2][:, :], [(ones_f, zc[:, 0, :]), (ones_f, zc[:, 1, :])], ["zcB", "cst"], [PB(2)])
                    for cc in range(2):
                        V(P, lambda e, cc=cc: e.scalar_tensor_tensor(out=xcB[:, cc, :], in0=pb[2][:, :],
                                                                     scalar=-1.0 / 256, in1=zc[:, cc, :],
                                                                     op0=ALU.mult, op1=ALU.add),
                          [PB(2), "zcB"], ["xcB"])
                    A(P, lambda e: e.activation(out=sqB[:, :, :], in_=xcB[:, :, :], func=AF.Square), ["xcB"], ["sqB"])
                    mm_group(P, pb[3][:, :], [(ones_f, sqB[:, 0, :]), (ones_f, sqB[:, 1, :])], ["sqB", "cst"], [PB(3)])
                    A(P, lambda e: e.activation(out=rsB[:, :], in_=pb[3][:, :], func=AF.Sqrt, scale=1.0 / 256,
                                                bias=LN_EPS), [PB(3)], ["rsB"])
                    V(P, lambda e: e.reciprocal(out=rsB[:, :], in_=rsB[:, :]), ["rsB"], ["rsB"])
                    for cc in range(2):
                        V(P, lambda e, cc=cc: e.scalar_tensor_tensor(out=tB[:, :], in0=xcB[:, cc, :],
                                                                     scalar=cp[:, 2 + cc:3 + cc], in1=rsB[:, :],
                                                                     op0=ALU.mult, op1=ALU.mult),
                          ["xcB", "rsB", "cp"], ["tB"])
                        yi = rr("ybB", 2)
                        A(P, lambda e, cc=cc, yi=yi: e.activation(out=yb[yi][:, :], in_=tB[:, :], func=AF.Silu,
                                                                  bias=cp[:, 4 + cc:5 + cc]), ["tB", "cp"], [("ybB", yi)])
                        P.dma("sync", mixS[cc * 128:(cc + 1) * 128, n * 512:(n + 1) * 512], yb[yi][:, :],
                              reads=[("ybB", yi)], writes=[("mixS", cc, n)])
                P.barrier()

        if "C" in phases:
            with contextlib.ExitStack() as st:
                Vres = sb("Vres", [128, 32, 8, 65], BF16, stack=st)
                V(P, lambda e: e.memset(Vres[:, :, :, 64:65], 1.0), [], ["Vres"])
                with contextlib.ExitStack() as st1:
                    mp = sb("mp", [128, 5], stack=st1)
                    P.dma("sync", mp[:, :], mla_p[l], writes=["mp"])
                    wq = sb("wq", [128, 2, 768], BF16, stack=st1)
                    wk = sb("wk", [128, 1, 512], BF16, stack=st1)
                    wv = sb("wv", [128, 1, 512], BF16, stack=st1)
                    stg = [sb(f"stgC{i}", [128, 2048], stack=st1) for i in range(2)]
                    load_cast(wq, "wq", w_uq[l], 2, 768, stg, "stgC")
                    load_cast(wk, "wk", w_k[l], 1, 512, stg, "stgC")
                    load_cast(wv, "wv", w_v[l], 1, 512, stg, "stgC")
                    cq = sb("cq", [128, 2, 512], stack=st1)
                    ckv = sb("ckv", [128, 512], stack=st1)
                    krt = sb("krt", [128, 512], stack=st1)
                    cst_ = sb("cosC", [96, 512], stack=st1)
                    snt_ = sb("sinC", [96, 512], stack=st1)
                    sqC = sb("sqC", [128, 3, 512], BF16, stack=st1)
                    rqC = sb("rqC", [128, 2, 512], stack=st1)
                    cqn = sb("cqn", [128, 2, 512], BF16, stack=st1)
                    ckvn = sb("ckvn", [128, 512], BF16, stack=st1)
                    raw = [sb(f"rawC{i}", [96, 512], stack=st1) for i in range(4)]
                    sqh = [sb(f"sqhC{i}", [96, 512], stack=st1) for i in range(4)]
                    rsh = [sb(f"rshC{i}", [96, 512], stack=st1) for i in range(4)]
                    qn = [sb(f"qnC{i}", [96, 512], stack=st1) for i in range(4)]
                    t1 = [sb(f"t1C{i}", [96, 512], stack=st1) for i in range(4)]
                    qf = [sb(f"qfC{i}", [96, 512], BF16, stack=st1) for i in range(4)]

                    def norm_rope_multi(chains):
                        for (i, gcol, dst, dkey) in chains:
                            A(P, lambda e: e.activation(out=sqh[i][:, :], in_=raw[i][:, :], func=AF.Square),
                              [("rawC", i)], [("sqhC", i)])
                        for (i, gcol, dst, dkey) in chains:
                            bi = 4 + i
                            PE(P, lambda e: e.matmul(pb[bi][0:96, :], ones_f[0:96, 0:96], sqh[i][:, :], start=True,
                                                     stop=True), [("sqhC", i), "cst"], [PB(bi)])
                        for (i, gcol, dst, dkey) in chains:
                            bi = 4 + i
                            A(P, lambda e: e.activation(out=rsh[i][:, :], in_=pb[bi][0:96, :], func=AF.Sqrt,
                                                        scale=1.0 / 96, bias=RMS_EPS), [PB(bi)], [("rshC", i)])
                        for (i, gcol, dst, dkey) in chains:
                            V(P, lambda e: e.reciprocal(out=rsh[i][:, :], in_=rsh[i][:, :]), [("rshC", i)], [("rshC", i)])
                            V(P, lambda e: e.scalar_tensor_tensor(out=qn[i][:, :], in0=raw[i][:, :], scalar=gcol,
                                                                  in1=rsh[i][:, :], op0=ALU.mult, op1=ALU.mult),
                              [("rawC", i), ("rshC", i), "mp"], [("qnC", i)])
                        for (i, gcol, dst, dkey) in chains:
                            bi = 4 + i
                            PE(P, lambda e: e.matmul(pb[bi][0:96, :], rmat, qn[i][:, :], start=True, stop=True),
                               [("qnC", i), "cst"], [PB(bi)])
                        for (i, gcol, dst, dkey) in chains:
                            bi = 4 + i
                            V(P, lambda e: e.tensor_tensor(out=t1[i][:, :], in0=pb[bi][0:96, :], in1=snt_[:, :],
                                                           op=ALU.mult), [PB(bi), "sinC"], [("t1C", i)])
                            G(P, lambda e: e.tensor_tensor(out=qn[i][:, :], in0=qn[i][:, :], in1=cst_[:, :], op=ALU.mult),
                              [("qnC", i), "cosC"], [("qnC", i)])
                        for (i, gcol, dst, dkey) in chains:
                            G(P, lambda e: e.tensor_tensor(out=qf[i][:, :], in0=qn[i][:, :], in1=t1[i][:, :], op=ALU.add),
                              [("qnC", i), ("t1C", i)], [("qfC", i)])
                            P.dma("sync" if i % 2 == 0 else "gpsimd", dst, qf[i][:, :], reads=[("qfC", i)], writes=[dkey])

                    for n in range(NT):
                        tsl = slice(n * 512, (n + 1) * 512)
                        for k in range(2):
                            P.dma("sync", cq[:, k, :], uS[(4 + k) * 128:(5 + k) * 128, tsl], writes=["cq"])
                        P.dma("gpsimd", ckv[:, :], uS[6 * 128:7 * 128, tsl], writes=["ckv"])
                        P.dma("gpsimd", krt[:, :], uS[7 * 128:8 * 128, tsl], writes=["krt"])
                        P.dma("sync", cst_[:, :], csS[0][:, tsl], writes=["cosC"])
                        P.dma("sync", snt_[:, :], csS[1][:, tsl], writes=["sinC"])
                        A(P, lambda e: e.activation(out=sqC[:, 0:2, :], in_=cq[:, :, :], func=AF.Square), ["cq"], ["sqC"])
                        A(P, lambda e: e.activation(out=sqC[:, 2, :], in_=ckv[:, :], func=AF.Square), ["ckv"], ["sqC"])
                        mm_group(P, pb[0][:, :], [(ones_b, sqC[:, 0, :]), (ones_b, sqC[:, 1, :])], ["sqC", "cbf"], [PB(0)])
                        mm_group(P, pb[1][:, :], [(ones_b, sqC[:, 2, :])], ["sqC", "cbf"], [PB(1)])
                        A(P, lambda e: e.activation(out=rqC[:, 0, :], in_=pb[0][:, :], func=AF.Sqrt, scale=1.0 / 256,
                                                    bias=RMS_EPS), [PB(0)], ["rqC"])
                        A(P, lambda e: e.activation(out=rqC[:, 1, :], in_=pb[1][:, :], func=AF.Sqrt, scale=1.0 / 128,
                                                    bias=RMS_EPS), [PB(1)], ["rqC"])
                        V(P, lambda e: e.reciprocal(out=rqC[:, :, :], in_=rqC[:, :, :]), ["rqC"], ["rqC"])
                        for k in range(2):
                            V(P, lambda e, k=k: e.scalar_tensor_tensor(out=cqn[:, k, :], in0=cq[:, k, :],
                                                                       scalar=mp[:, k:k + 1], in1=rqC[:, 0, :],
                                                                       op0=ALU.mult, op1=ALU.mult),
                              ["cq", "rqC", "mp"], ["cqn"])
                        V(P, lambda e: e.scalar_tensor_tensor(out=ckvn[:, :], in0=ckv[:, :], scalar=mp[:, 2:3],
                                                              in1=rqC[:, 1, :], op0=ALU.mult, op1=ALU.mult),
                          ["ckv", "rqC", "mp"], ["ckvn"])
                        for j in range(4):
                            bi = 2 + (j % 2)
                            PE(P, lambda e, j=j, bi=bi: e.matmul(pb[bi][:, :], ckvn[:, j * 128:(j + 1) * 128], wv[:, 0, :],
                                                                 start=True, stop=True), ["ckvn", "wv"], [PB(bi)])
                            A(P, lambda e, j=j, bi=bi, n=n: e.copy(
                                out=Vres[:, n * 4 + j, :, 0:64],
                                in_=pb[bi][:, :].rearrange("p (h d) -> p h d", h=8)), [PB(bi)], ["Vres"])
                        for hp in range(4):
                            chains = []
                            for hh in range(2):
                                h = 2 * hp + hh
                                iq, ik = 2 * hh, 2 * hh + 1
                                bq, bk = hh, 2 + hh
                                mm_group(P, pb[bq][0:96, :], [(wq[:, k, 96 * h:96 * h + 96], cqn[:, k, :]) for k in range(2)],
                                         ["wq", "cqn"], [PB(bq)])
                                PE(P, lambda e, h=h, bk=bk: e.matmul(pb[bk][0:64, :], wk[:, 0, 64 * h:64 * h + 64], ckvn[:, :],
                                                                     start=True, stop=True), ["wk", "ckvn"], [PB(bk)])
                                A(P, lambda e, iq=iq, bq=bq: e.copy(out=raw[iq][:, :], in_=pb[bq][0:96, :]), [PB(bq)],
                                  [("rawC", iq)])
                                V(P, lambda e, ik=ik, bk=bk: e.tensor_copy(out=raw[ik][0:64, :], in_=pb[bk][0:64, :]), [PB(bk)],
                                  [("rawC", ik)])
                                A(P, lambda e, ik=ik: e.copy(out=raw[ik][64:96, :], in_=krt[64:96, :]), ["krt"], [("rawC", ik)])
                                chains.append((iq, mp[0:96, 3:4], qS[h][:, tsl], ("qS", h, n)))
                                chains.append((ik, mp[0:96, 4:5], kS[h][:, tsl], ("kS", h, n)))
                            norm_rope_multi(chains)
                    P.barrier()
                with contextlib.ExitStack() as st2:
                    KT = [sb(f"KT{i}", [96, T], BF16, stack=st2) for i in range(2)]
                    QT = [sb(f"QT{i}", [96, T], BF16, stack=st2) for i in range(2)]
                    pt = [sb(f"ptC{i}", [128, 512], BF16, stack=st2) for i in range(4)]
                    osb = [sb(f"osb{i}", [65, 512], stack=st2) for i in range(2)]
                    rc = [sb(f"rcC{i}", [64, 512], stack=st2) for i in range(2)]
                    yo = [sb(f"yoC{i}", [64, 512], BF16, stack=st2) for i in range(2)]
                    for h in range(8):
                        kt_, qt_ = KT[h % 2], QT[h % 2]
                        kk_, qk_ = f"KT{h % 2}", f"QT{h % 2}"
                        for half in range(2):
                            hs = slice(half * 2048, (half + 1) * 2048)
                            P.dma("sync", kt_[:, hs], kS[h][:, hs], writes=[kk_])
                            P.dma("gpsimd", qt_[:, hs], qS[h][:, hs], writes=[qk_])
                        blocks = [(qi, kb) for qi in range(NT) for kb in range(4 * qi + 4)]

                        def geom(qi, kb):
                            d = kb - 4 * qi
                            return d, max(0, d) * 128

                        def emit_S(bi_):
                            qi, kb = blocks[bi_]
                            d, c0 = geom(qi, kb)
                            sbk = bi_ % 3
                            PE(P, lambda e: e.matmul(pb[sbk][:, c0:512], kt_[:, kb * 128:(kb + 1) * 128],
                                                     qt_[:, qi * 512 + c0:(qi + 1) * 512], start=True, stop=True),
                               [kk_, qk_], [PB(sbk)])

                        emit_S(0)
                        emit_S(1)
                        for bi_, (qi, kb) in enumerate(blocks):
                            d, c0 = geom(qi, kb)
                            nkb = 4 * qi + 4
                            ob = 4 + (qi % 2)
                            sbk = bi_ % 3
                            pi = bi_ % 4
                            A(P, lambda e: e.activation(out=pt[pi][:, c0:512], in_=pb[sbk][:, c0:512], func=AF.Exp,
                                                        scale=SCALE_QK), [PB(sbk)], [("ptC", pi)])
                            if d >= 0:
                                G(P, lambda e: e.tensor_tensor(out=pt[pi][:, c0:c0 + 128], in0=pt[pi][:, c0:c0 + 128],
                                                               in1=tri_b, op=ALU.mult), [("ptC", pi), "cbf"], [("ptC", pi)])
                            if bi_ + 2 < len(blocks):
                                emit_S(bi_ + 2)
                            PE(P, lambda e: e.matmul(pb[ob][0:65, c0:512], Vres[:, kb, h, :], pt[pi][:, c0:512],
                                                     start=(kb == 0), stop=(kb == nkb - 1)), [("ptC", pi), "Vres"], [PB(ob)])
                            if kb == nkb - 1:
                                oi = qi % 2
                                A(P, lambda e: e.copy(out=osb[oi][:, :], in_=pb[ob][0:65, :]), [PB(ob)], [("osb", oi)])
                                PE(P, lambda e: e.matmul(pb[6 + oi][0:64, :], sel65, osb[oi][:, :], start=True, stop=True),
                                   [("osb", oi), "cst"], [PB(6 + oi)])
                                V(P, lambda e: e.reciprocal(out=rc[oi][:, :], in_=pb[6 + oi][0:64, :]), [PB(6 + oi)],
                                  [("rcC", oi)])
                                V(P, lambda e: e.tensor_tensor(out=yo[oi][:, :], in0=osb[oi][0:64, :], in1=rc[oi][:, :],
                                                               op=ALU.mult), [("osb", oi), ("rcC", oi)], [("yoC", oi)])
                                P.dma("sync", mixS[256 + 64 * h:256 + 64 * (h + 1), qi * 512:(qi + 1) * 512], yo[oi][:, :],
                                      reads=[("yoC", oi)], writes=[("mixS", "m", h, qi)])
                    P.barrier()

        if "D" in phases:
            rwkv_phase(nc, P, l, locals())

        if "E" in phases:
            with contextlib.ExitStack() as st:
                wO = sb("wO", [128, 8, 1024], BF16, stack=st)
                stg = [sb(f"stgE{i}", [128, 2048], stack=st) for i in range(3)]
                load_cast(wO, "wO", w_out[l], 8, 1024, stg, "stgE", cast_eng="alt")
                xtb = [sb(f"xtE{i}", [128, 8, 512], stack=st) for i in range(2)]
                mtb = [sb(f"mtE{i}", [128, 8, 512], BF16, stack=st) for i in range(2)]
                for n in range(NT):
                    xt, mt = xtb[n % 2], mtb[n % 2]
                    xkey, mkey = f"xtE{n % 2}", f"mtE{n % 2}"
                    tsl = slice(n * 512, (n + 1) * 512)
                    for k in range(8):
                        P.dma("sync", xt[:, k, :], xS[k * 128:(k + 1) * 128, tsl], reads=[("xS", k, n)], writes=[(xkey, k)])
                        P.dma("gpsimd", mt[:, k, :], mixS[k * 128:(k + 1) * 128, tsl], writes=[mkey])
                    for m in range(8):
                        bi = m % 4
                        mm_group(P, pb[bi][:, :], [(wO[:, k, m * 128:(m + 1) * 128], mt[:, k, :]) for k in range(8)],
                                 ["wO", mkey], [PB(bi)])
                        V(P, lambda e, m=m, bi=bi, xt=xt: e.scalar_tensor_tensor(
                            out=xt[:, m, :], in0=pb[bi][:, :], scalar=gt1[:, m:m + 1], in1=xt[:, m, :],
                            op0=ALU.mult, op1=ALU.add), [PB(bi), (xkey, m), "mod"], [(xkey, m)])
                        P.dma("sync", xS[m * 128:(m + 1) * 128, tsl], xt[:, m, :], reads=[(xkey, m)],
                              writes=[("xS", m, n)])
                P.barrier()

        if "F" in phases:
            ffn_phase(nc, P, l, locals())

    P.barrier()
    es.close()
    return nc


RW_STOP = os.environ.get('RW_STOP', '')
RW_LVL = int(os.environ.get('RW_LVL', '99'))


USE_R32 = os.environ.get('USE_R32', '1') == '1'


def R32g(ap):
    return ap.bitcast(mybir.dt.float32r) if USE_R32 else ap


def rwkv_phase(nc, P, l, E):
    sb, pb, PB, uS, mixS, rr = E["sb"], E["pb"], E["PB"], E["uS"], E["mixS"], E["rr"]
    ident, ones_f, bd_ones, m_sl, m_row, zero_t = (E["ident"], E["ones_f"], E["bd_ones"], E["m_sl"], E["m_row"],
                                                   E["zero_t"])
    EM05 = float(np.exp(-0.5))
    X_ = mybir.AxisListType.X
    with contextlib.ExitStack() as st:
        mu = sb("muD", [128, 8], stack=st)
        rp = sb("rpD", [128, 14], stack=st)
        wup = sb("wupD", [128, 256], stack=st)
        gup = sb("gupD", [128, 256], stack=st)
        P.dma("sync", mu[:, :], E["rw_mu"][l], writes=["muD"])
        P.dma("sync", rp[:, :], E["rw_p"][l], writes=["rpD"])
        P.dma("sync", wup[:, :], E["rw_wup"][l], writes=["wupD"])
        P.dma("sync", gup[:, :], E["rw_gup"][l], writes=["gupD"])
        omka = sb("omkaD", [128, 2], stack=st)
        V(P, lambda e: e.tensor_scalar(out=omka[:, :], in0=rp[:, 6:8], scalar1=-1.0, scalar2=1.0, op0=ALU.mult,
                                       op1=ALU.add), ["rpD"], ["omkaD"])
        w0, a0, k_k, k_a, r_k, ln_g, ln_b = [rp[:, 2 * i:2 * i + 2] for i in range(7)]
        ST = [[sb(f"ST{c}{i}", [128, 64], stack=st) for i in range(2)] for c in range(2)]
        for c in range(2):
            V(P, lambda e, c=c: e.memset(ST[c][0][:, :], 0.0), [], [("ST", c, 0, 0), ("ST", c, 0, 1)])
        ur = sb("urD", [128, 8, 513], stack=st)
        us = sb("usD", [128, 8, 512], stack=st)
        dl = [sb(f"dlD{i}", [128, 512], stack=st) for i in range(2)]
        tw = sb("twD", [128, 512], stack=st)
        sgd = sb("sgdD", [128, 512], stack=st)

        def t2(name, shape=(128, 512)):
            return [sb(f"{name}{c}", list(shape), stack=st) for c in range(2)]
        def t22(name, shape=(128, 512)):
            return [[sb(f"{name}{pp}{c}", list(shape), stack=st) for c in range(2)] for pp in range(2)]
        dec, asg, gtc, kk, kk2, rn, kkn, tkk, km, bv = (t2("decD"), t2("asgD"), t22("gtcD"), t2("kkD"), t2("kk2D"),
                                                        t2("rnD"), t2("kknD"), None, t2("kmD"), t2("bvD"))
        Gi, Ginv, Ge, bt, kt, rk, bon, ysb, yc, sqy = (t22("GiD"), None, t2("GeD"), t22("btD"), t22("ktD"),
                                                       None, t22("bonD"), t2("ysbD"), None, None)
        ar = t22("arD", (128, 4, 256))
        btT, ktT, vT = t22("btTD", (128, 4, 128)), t22("ktTD", (128, 4, 128)), t22("vTD", (128, 4, 128))
        yo = [sb(f"yoD{c}", [128, 512], BF16, stack=st) for c in range(2)]
        Mm = [sb(f"MmD{q}", [128, 512], stack=st) for q in range(4)]
        FR = mybir.dt.float32r if USE_R32 else F32
        PT0 = [sb(f"PT0D{q}", [128, 128], stack=st) for q in range(4)]
        PTb = [[sb(f"PTD{q}{i}", [128, 128], FR, stack=st) for i in range(2)] for q in range(4)]
        Pmb = [[sb(f"PmD{q}{i}", [128, 128], FR, stack=st) for i in range(2)] for q in range(4)]
        Xb = [[sb(f"XD{q}{i}", [128, 128], FR, stack=st) for i in range(2)] for q in range(4)]
        XF = [sb(f"XFD{q}", [128, 128], stack=st) for q in range(4)]
        Wsb = [sb(f"WsbD{q}", [128, 64], stack=st) for q in range(4)]
        Usb = [sb(f"UsbD{q}", [128, 64], stack=st) for q in range(4)]

        def v4(t):
            return t[:, :].rearrange("p (j t) -> p j t", j=4)

        def prep_gen(n):
            p = n % 2
            if n == 0:
                V(P, lambda e: e.memset(ur[:, :, 0:1], 0.0), [], ["urD"])
                yield
            for j in range(8):
                q_ = "sync" if j % 2 == 0 else "gpsimd"
                if n == 0:
                    P.dma(q_, ur[:, j, 1:513], uS[(8 + j) * 128:(9 + j) * 128, 0:512], writes=["urD"])
                    yield
                else:
                    P.dma(q_, ur[:, j, :], uS[(8 + j) * 128:(9 + j) * 128, n * 512 - 1:(n + 1) * 512], writes=["urD"])
                    yield
            for j in range(8):
                eng = "vector" if j % 2 == 0 else "gpsimd"
                di = j % 2
                P.op(eng, lambda e, j=j, di=di: e.tensor_tensor(out=dl[di][:, :], in0=ur[:, j, 0:512], in1=ur[:, j, 1:513],
                                                                op=ALU.subtract), ["urD"], [("dlD", di)])
                yield
                P.op("vector", lambda e, j=j, di=di: e.scalar_tensor_tensor(out=us[:, j, :], in0=dl[di][:, :],
                                                                       scalar=mu[:, j:j + 1], in1=ur[:, j, 1:513],
                                                                       op0=ALU.mult, op1=ALU.add),
                     [("dlD", di), "urD", "muD"], [("usD", j)])
                yield
            A(P, lambda e: e.activation(out=tw[0:64, :], in_=us[0:64, 6, :], func=AF.Tanh), [("usD", 6)], ["twD"])
            yield
            A(P, lambda e: e.activation(out=sgd[:, :], in_=us[:, 7, :], func=AF.Sigmoid), [("usD", 7)], ["sgdD"])
            yield
            for c in range(2):
                cs = slice(c * 128, (c + 1) * 128)
                kR, kK, kV = ("usD", c), ("usD", 2 + c), ("usD", 4 + c)
                r_, k_, v_ = us[:, c, :], us[:, 2 + c, :], us[:, 4 + c, :]
                PE(P, lambda e: e.matmul(pb[0][:, :], wup[0:64, cs], tw[0:64, :], start=True, stop=True),
                   ["wupD", "twD"], [PB(0)])
                yield
                A(P, lambda e: e.activation(out=dec[c][:, :], in_=pb[0][:, :], func=AF.Sigmoid, bias=w0[:, c:c + 1]),
                  [PB(0), "rpD"], [("decD", c)])
                yield
                A(P, lambda e: e.activation(out=dec[c][:, :], in_=dec[c][:, :], func=AF.Exp, scale=-EM05),
                  [("decD", c)], [("decD", c)])
                yield
                PE(P, lambda e: e.matmul(pb[1][:, :], wup[64:128, cs], us[64:128, 6, :], start=True, stop=True),
                   ["wupD", ("usD", 6)], [PB(1)])
                yield
                A(P, lambda e: e.activation(out=asg[c][:, :], in_=pb[1][:, :], func=AF.Sigmoid, bias=a0[:, c:c + 1]),
                  [PB(1), "rpD"], [("asgD", c)])
                yield
                PE(P, lambda e: e.matmul(pb[2][:, :], gup[:, cs], sgd[:, :], start=True, stop=True),
                   ["gupD", "sgdD"], [PB(2)])
                yield
                A(P, lambda e: e.copy(out=gtc[p][c][:, :], in_=pb[2][:, :]), [PB(2)], [("gtcD", c, p)])
                yield
                V(P, lambda e: e.tensor_scalar(out=kk[c][:, :], in0=k_, scalar1=k_k[:, c:c + 1], scalar2=None,
                                               op0=ALU.mult), [kK, "rpD"], [("kkD", c)])
                yield
                G(P, lambda e: e.tensor_tensor(out=kk2[c][:, :], in0=kk[c][:, :], in1=kk[c][:, :], op=ALU.mult),
                  [("kkD", c)], [("kk2D", c)])
                yield
                PE(P, lambda e: e.matmul(pb[3][:, :], bd_ones, kk2[c][:, :], start=True, stop=True),
                   [("kk2D", c), "cst"], [PB(3)])
                yield
                V(P, lambda e: e.tensor_scalar(out=rn[c][:, :], in0=pb[3][:, :], scalar1=1e-24, scalar2=None,
                                               op0=ALU.max), [PB(3)], [("rnD", c)])
                yield
                A(P, lambda e: e.activation(out=rn[c][:, :], in_=rn[c][:, :], func=AF.Sqrt), [("rnD", c)], [("rnD", c)])
                yield
                V(P, lambda e: e.reciprocal(out=rn[c][:, :], in_=rn[c][:, :]), [("rnD", c)], [("rnD", c)])
                yield
                V(P, lambda e: e.tensor_tensor(out=kkn[c][:, :], in0=kk[c][:, :], in1=rn[c][:, :], op=ALU.mult),
                  [("kkD", c), ("rnD", c)], [("kknD", c)])
                yield
                V(P, lambda e: e.tensor_scalar(out=rn[c][:, :], in0=asg[c][:, :], scalar1=k_a[:, c:c + 1],
                                               scalar2=omka[:, c:c + 1], op0=ALU.mult, op1=ALU.add),
                  [("asgD", c), "rpD", "omkaD"], [("rnD", c)])
                yield
                V(P, lambda e: e.tensor_tensor(out=km[c][:, :], in0=k_, in1=rn[c][:, :], op=ALU.mult),
                  [kK, ("rnD", c)], [("kmD", c)])
                yield
                G(P, lambda e: e.tensor_tensor(out=bv[c][:, :], in0=kkn[c][:, :], in1=asg[c][:, :], op=ALU.mult),
                  [("kknD", c), ("asgD", c)], [("bvD", c)])
                yield
                for j in range(4):
                    tk = slice(j * 128, (j + 1) * 128)
                    V(P, lambda e, tk=tk: e.tensor_tensor_scan(out=Gi[p][c][:, tk], data0=dec[c][:, tk],
                                                               data1=zero_t[:, 0:128], initial=1.0, op0=ALU.mult,
                                                               op1=ALU.add), [("decD", c), "zero"], [("GiD", c, p)])
                    yield
                V(P, lambda e: e.reciprocal(out=dec[c][:, :], in_=Gi[p][c][:, :]), [("GiD", c, p)], [("decD", c)])
                yield
                V(P, lambda e: e.tensor_copy(out=v4(Ge[c])[:, :, 1:128], in_=v4(Gi[p][c])[:, :, 0:127]), [("GiD", c, p)],
                  [("GeD", c)])
                yield
                V(P, lambda e: e.memset(v4(Ge[c])[:, :, 0:1], 1.0), [], [("GeD", c)])
                yield
                V(P, lambda e: e.scalar_tensor_tensor(out=ar[p][c][:, :, 0:128], in0=v4(kkn[c]), scalar=-1.0,
                                                      in1=v4(Ge[c]), op0=ALU.mult, op1=ALU.mult),
                  [("kknD", c), ("GeD", c)], [("arD", c, p)])
                yield
                V(P, lambda e: e.tensor_tensor(out=ar[p][c][:, :, 128:256], in0=r_.rearrange("p (j t) -> p j t", j=4),
                                               in1=v4(Gi[p][c]), op=ALU.mult), [kR, ("GiD", c, p)], [("arD", c, p)])
                yield
                G(P, lambda e: e.tensor_tensor(out=bt[p][c][:, :], in0=bv[c][:, :], in1=dec[c][:, :], op=ALU.mult),
                  [("bvD", c), ("decD", c)], [("btD", c, p)])
                yield
                G(P, lambda e: e.tensor_tensor(out=kt[p][c][:, :], in0=km[c][:, :], in1=dec[c][:, :], op=ALU.mult),
                  [("kmD", c), ("decD", c)], [("ktD", c, p)])
                yield
                V(P, lambda e: e.scalar_tensor_tensor(out=kk2[c][:, :], in0=r_, scalar=r_k[:, c:c + 1], in1=km[c][:, :],
                                                      op0=ALU.mult, op1=ALU.mult), [kR, ("kmD", c), "rpD"], [("kk2D", c)])
                yield
                PE(P, lambda e: e.matmul(pb[4][:, :], bd_ones, kk2[c][:, :], start=True, stop=True),
                   [("kk2D", c), "cst"], [PB(4)])
                yield
                V(P, lambda e: e.tensor_tensor(out=bon[p][c][:, :], in0=pb[4][:, :], in1=v_, op=ALU.mult),
                  [PB(4), kV], [("bonD", c, p)])
                yield
                for bi, (src, skey, dst, dkey) in enumerate(((bt[p][c], ("btD", c, p), btT[p][c], ("btTD", c, p)),
                                                             (kt[p][c], ("ktD", c, p), ktT[p][c], ("ktTD", c, p)),
                                                             (v_, kV, vT[p][c], ("vTD", c, p)))):
                    bank = 5 + bi

                    def fn(e, src=src, bank=bank):
                        ins = None
                        for j in range(4):
                            ins = e.transpose(pb[bank][:, j * 128:(j + 1) * 128], src[:, j * 128:(j + 1) * 128], ident)
                        return ins
                    PE(P, fn, [skey, "cst"], [PB(bank)])
                    yield
                    if bi % 2 == 0:
                        A(P, lambda e, dst=dst, bank=bank: e.copy(out=dst[:, :, :],
                                                                  in_=pb[bank][:, :].rearrange("p (j t) -> p j t", j=4)),
                          [PB(bank)], [dkey])
                        yield
                    else:
                        V(P, lambda e, dst=dst, bank=bank: e.tensor_copy(
                            out=dst[:, :, :], in_=pb[bank][:, :].rearrange("p (j t) -> p j t", j=4)), [PB(bank)], [dkey])
                        yield


        def exhaust(g):
            if g is not None:
                for _ in g:
                    pass

        exhaust(prep_gen(0))
        for n in range(NT):
            p = n % 2
            pgen = [prep_gen(n + 1) if n + 1 < NT else None]

            def adv(k, site='a'):
                if pgen[0] is None or site not in os.environ.get('RW_ADV', 'ac'):
                    return
                for _ in range(k):
                    try:
                        next(pgen[0])
                    except StopIteration:
                        pgen[0] = None
                        return
            for j in range(4 if RW_STOP != 'prep' else 0):
                g = n * 4 + j
                cur, nxt = g % 2, (g + 1) % 2
                tk = slice(j * 128, (j + 1) * 128)
                heads = [(c, hh) for c in range(2) for hh in range(2)]
                for (c, hh) in heads:
                    q = c * 2 + hh
                    sl = slice(64 * hh, 64 * hh + 64)
                    PE(P, lambda e, c=c, sl=sl, q=q: e.matmul(pb[q][:, 0:256], bt[p][c][sl, tk], ar[p][c][sl, j, :],
                                                              start=True, stop=True), [("btD", c, p), ("arD", c, p)], [PB(q)])
                    PE(P, lambda e, c=c, sl=sl, q=q: e.matmul(pb[q][:, 256:512], kt[p][c][sl, tk], ar[p][c][sl, j, :],
                                                              start=True, stop=True), [("ktD", c, p), ("arD", c, p)], [PB(q)])
                    V(P, lambda e, q=q: e.tensor_tensor(out=Mm[q][:, :], in0=pb[q][:, :], in1=m_row, op=ALU.mult),
                      [PB(q), "cst"], [("MmD", q)])
                    PE(P, lambda e, c=c, sl=sl, q=q: e.matmul(pb[q][:, 0:128], ar[p][c][sl, j, 0:128], bt[p][c][sl, tk],
                                                              start=True, stop=True), [("btD", c, p), ("arD", c, p)], [PB(q)])
                    V(P, lambda e, q=q: e.tensor_tensor(out=PT0[q][:, :], in0=pb[q][:, 0:128], in1=m_sl, op=ALU.mult),
                      [PB(q), "cst"], [("PT0D", q)])
                    G(P, lambda e, q=q: e.tensor_tensor(out=Xb[q][0][:, :], in0=Mm[q][:, 0:128], in1=ident, op=ALU.add),
                      [("MmD", q), "cst"], [("XD", q, 0)])
                adv(13, 'a')
                for lev in range(6 if RW_STOP != 'M' else 0):
                    adv(3, 'b')
                    for (c, hh) in heads:
                        q = c * 2 + hh
                        curP = Mm[q][:, 0:128] if lev == 0 else Pmb[q][(lev - 1) % 2][:, :]
                        curPk = ("MmD", q) if lev == 0 else ("PmD", q, (lev - 1) % 2)
                        curPT = PT0[q][:, :] if lev == 0 else PTb[q][(lev - 1) % 2][:, :]
                        curPTk = ("PT0D", q) if lev == 0 else ("PTD", q, (lev - 1) % 2)
                        nPT, nPTk = PTb[q][lev % 2], ("PTD", q, lev % 2)
                        nP, nPk = Pmb[q][lev % 2], ("PmD", q, lev % 2)
                        cX, cXk = Xb[q][lev % 2], ("XD", q, lev % 2)
                        if lev < 5:
                            nX, nXk = Xb[q][(lev + 1) % 2], ("XD", q, (lev + 1) % 2)
                        else:
                            nX, nXk = XF[q], ("XFD", q)
                        R32 = (lambda ap: ap) if lev == 0 else R32g
                        if lev < 5:
                            PE(P, lambda e, q=q, curP=curP, curPT=curPT: e.matmul(pb[q][:, 0:128], R32(curPT), R32(curP),
                                                                                  start=True, stop=True),
                               [curPk, curPTk], [PB(q)])
                        PE(P, lambda e, q=q, curP=curP, curPT=curPT: e.matmul(pb[4 + q][:, 0:128], R32(curP), R32(curPT),
                                                                              start=True, stop=True),
                           [curPk, curPTk], [PB(4 + q)])
                        if lev < 5:
                            V(P, lambda e, q=q, nP=nP: e.tensor_copy(out=nP[:, :], in_=pb[q][:, 0:128]), [PB(q)], [nPk])
                        A(P, lambda e, q=q, nPT=nPT: e.copy(out=nPT[:, :], in_=pb[4 + q][:, 0:128]), [PB(4 + q)], [nPTk])
                        PE(P, lambda e, q=q, nPT=nPT, cX=cX: e.matmul(pb[q][:, 256:384], R32g(nPT[:, :]), R32g(cX[:, :]),
                                                                      start=True, stop=True), [nPTk, cXk], [PB(q)])
                        V(P, lambda e, q=q, cX=cX, nX=nX: e.tensor_tensor(out=nX[:, :], in0=pb[q][:, 256:384],
                                                                          in1=cX[:, :], op=ALU.add),
                          [PB(q), cXk], [nXk])
                if RW_STOP in ('inv', 'M'):
                    continue
                adv(13, 'c')
                for (c, hh) in heads:
                    q = c * 2 + hh
                    sl = slice(64 * hh, 64 * hh + 64)
                    bC = 4 + q
                    S0, S0k = ST[c][cur][sl, :], ("ST", c, cur, hh)
                    mm_group(P, pb[bC][:, 0:64], [(ar[p][c][sl, j, 0:128], S0), (Mm[q][:, 256:384], vT[p][c][:, j, sl])],
                             [("arD", c, p), S0k, ("MmD", q), ("vTD", c, p)], [PB(bC)])
                    V(P, lambda e, q=q, bC=bC: e.tensor_copy(out=Wsb[q][:, :], in_=pb[bC][:, 0:64]), [PB(bC)],
                      [("WsbD", q)])
                for (c, hh) in heads:
                    q = c * 2 + hh
                    bC = 4 + q
                    PE(P, lambda e, q=q, bC=bC: e.matmul(pb[bC][:, 64:128], XF[q][:, :], Wsb[q][:, :], start=True,
                                                         stop=True), [("XFD", q), ("WsbD", q)], [PB(bC)])
                    A(P, lambda e, q=q, bC=bC: e.copy(out=Usb[q][:, :], in_=pb[bC][:, 64:128]), [PB(bC)], [("UsbD", q)])
                for (c, hh) in heads:
                    q = c * 2 + hh
                    sl = slice(64 * hh, 64 * hh + 64)
                    bC = 4 + q
                    S0, S0k = ST[c][cur][sl, :], ("ST", c, cur, hh)
                    mm_group(P, pb[bC][sl, 128:192], [(ident[sl, sl], S0), (btT[p][c][:, j, sl], Usb[q][:, :]),
                                                      (ktT[p][c][:, j, sl], vT[p][c][:, j, sl])],
                             [S0k, ("btTD", c, p), ("UsbD", q), ("ktTD", c, p), ("vTD", c, p), "cst"], [PB(bC)])
                    A(P, lambda e, c=c, sl=sl, bC=bC: e.activation(
                        out=ST[c][nxt][sl, :], in_=pb[bC][sl, 128:192], func=AF.Identity,
                        scale=Gi[p][c][sl, j * 128 + 127:j * 128 + 128]), [PB(bC), ("GiD", c, p)], [("ST", c, nxt, hh)])
                    mm_group(P, pb[q][sl, 256:384], [(S0, ar[p][c][sl, j, 128:256]), (Usb[q][:, :], Mm[q][:, 128:256]),
                                                     (vT[p][c][:, j, sl], Mm[q][:, 384:512])],
                             [S0k, ("arD", c, p), ("UsbD", q), ("MmD", q), ("vTD", c, p)], [PB(q)])
                    V(P, lambda e, c=c, sl=sl, q=q: e.tensor_copy(out=ysb[c][sl, tk], in_=pb[q][sl, 256:384]),
                      [PB(q)], [("ysbD", c)])
            exhaust(pgen[0])
            for c in range(2):
                b1, b2 = c * 2, c * 2 + 1
                PE(P, lambda e, b1=b1: e.matmul(pb[b1][:, :], bd_ones, ysb[c][:, :], start=True, stop=True),
                   [("ysbD", c), "cst"], [PB(b1)])
                V(P, lambda e, b1=b1: e.scalar_tensor_tensor(out=kk[c][:, :], in0=pb[b1][:, :], scalar=-1.0 / 64,
                                                             in1=ysb[c][:, :], op0=ALU.mult, op1=ALU.add),
                  [PB(b1), ("ysbD", c)], [("kkD", c)])
                A(P, lambda e: e.activation(out=kkn[c][:, :], in_=kk[c][:, :], func=AF.Square), [("kkD", c)],
                  [("kknD", c)])
                PE(P, lambda e, b2=b2: e.matmul(pb[b2][:, :], bd_ones, kkn[c][:, :], start=True, stop=True),
                   [("kknD", c), "cst"], [PB(b2)])
                A(P, lambda e, b2=b2: e.activation(out=kkn[c][:, :], in_=pb[b2][:, :], func=AF.Sqrt, scale=1.0 / 64,
                                                   bias=GN_EPS), [PB(b2)], [("kknD", c)])
                V(P, lambda e: e.reciprocal(out=kkn[c][:, :], in_=kkn[c][:, :]), [("kknD", c)], [("kknD", c)])
                V(P, lambda e: e.scalar_tensor_tensor(out=kk[c][:, :], in0=kk[c][:, :], scalar=ln_g[:, c:c + 1],
                                                      in1=kkn[c][:, :], op0=ALU.mult, op1=ALU.mult),
                  [("kkD", c), ("kknD", c), "rpD"], [("kkD", c)])
                V(P, lambda e: e.scalar_tensor_tensor(out=kk[c][:, :], in0=kk[c][:, :], scalar=ln_b[:, c:c + 1],
                                                      in1=bon[p][c][:, :], op0=ALU.add, op1=ALU.add),
                  [("kkD", c), ("bonD", c, p), "rpD"], [("kkD", c)])
                V(P, lambda e: e.tensor_tensor(out=yo[c][:, :], in0=kk[c][:, :], in1=gtc[p][c][:, :], op=ALU.mult),
                  [("kkD", c), ("gtcD", c, p)], [("yoD", c)])
                P.dma("sync", mixS[768 + c * 128:768 + (c + 1) * 128, n * 512:(n + 1) * 512], yo[c][:, :],
                      reads=[("yoD", c)], writes=[("mixS", "r", c, n)])
        P.barrier()


def ffn_phase(nc, P, l, E):
    sb, pb, PB, xS, rr, ring = E["sb"], E["pb"], E["PB"], E["xS"], E["rr"], E["ring"]
    gm2, sh2, gt2 = E["gm2"], E["sh2"], E["gt2"]
    rms_h, load_cast, ident, cstt = E["rms_h"], E["load_cast"], E["ident"], E["cstt"]
    is_moe = (l % 2 == 1)
    li = l // 2
    TB = 1024
    NTB = T // TB
    if is_moe:
        experts = [(E["moe_g"][li, e], E["moe_u"][li, e], E["moe_d"][li, e]) for e in range(NE)]
        ff = MFF
    else:
        experts = [(E["ffn_g"][li], E["ffn_u"][li], E["ffn_d"][li])]
        ff = DFF
    nfg = ff // 256
    with contextlib.ExitStack() as st:
        xacc = sb("xacc", [128, 8, TB], stack=st)
        h2 = sb("h2", [128, 8, TB], BF16, stack=st)
        tmp = {"sq": sb("sqF", [128, 8, 512], BF16, stack=st), "rstd": sb("rstdF", [128, 512], stack=st),
               "t2": [sb(f"t2F{i}", [128, 512], stack=st) for i in range(2)]}
        stg = [sb(f"stgF{i}", [128, 2048], stack=st) for i in range(4)]
        wg = [sb(f"wgF{i}", [128, 8, 256], BF16, stack=st) for i in range(2)]
        wu = [sb(f"wuF{i}", [128, 8, 256], BF16, stack=st) for i in range(2)]
        wd = [sb(f"wdF{i}", [128, 2, 1024], BF16, stack=st) for i in range(3)]
        sg = [sb(f"sgF{i}", [128, 512], stack=st) for i in range(2)]
        a_ = [sb(f"aF{i}", [128, 2, 512], BF16, stack=st) for i in range(3)]
        if is_moe:
            hf = sb("hfF", [128, 8, 512], stack=st)
            rt = sb("rtF", [128, 64], stack=st)
            P.dma("sync", rt[:, :], E["moe_r"][li], writes=["rtF"])
            lg = sb("lgF", [128, 4, 8], stack=st)
            mx8 = sb("mx8F", [128, 4, 8], stack=st)
            ngm = sb("ngmF", [128, 4], stack=st)
            ex = sb("exF", [128, 4, 8], stack=st)
            msk = sb("mskF", [128, 4, 8], stack=st)
            den = sb("denF", [128, 4], stack=st)
            gate = sb("gateF", [128, 4, 8], stack=st)
            gT = sb("gTF", [8, TB], stack=st)
            gbc = [sb(f"gbcF{i}", [128, TB], stack=st) for i in range(2)]
            tt = [sb(f"ttF{i}", [128, 512], stack=st) for i in range(2)]
        cnt = 0
        pending = []
        for tb in range(NTB):
            t0 = tb * TB
            for k in range(8):
                for half in range(2):
                    P.dma("sync" if k % 2 == 0 else "gpsimd", xacc[:, k, half * 512:(half + 1) * 512],
                          xS[k * 128:(k + 1) * 128, t0 + half * 512:t0 + (half + 1) * 512],
                          writes=[("xacc", k, half)])
            for half in range(2):
                hs = slice(half * 512, (half + 1) * 512)
                xk = [("xacc", k, half) for k in range(8)]
                if not is_moe:
                    rms_h(xacc[:, :, hs], xk, h2[:, :, hs], ("h2", half), gm2, sh2, tmp)
                else:
                    rms_h(xacc[:, :, hs], xk, h2[:, :, hs], ("h2", half), gm2, sh2, tmp, hf=hf, hfkey="hfF")
                    for jb in range(4):
                        mm_group(P, pb[6][:, jb * 8:(jb + 1) * 8],
                                 [(hf[:, k, jb * 128:(jb + 1) * 128], rt[:, k * 8:(k + 1) * 8]) for k in range(8)],
                                 ["hfF", "rtF"], [PB(6)])
                    V(P, lambda e: e.tensor_copy(out=lg[:, :, :], in_=pb[6][:, 0:32].rearrange("p (j e) -> p j e", j=4)),
                      [PB(6)], ["lgF"])
                    for jb in range(4):
                        V(P, lambda e, jb=jb: e.max(out=mx8[:, jb, :], in_=lg[:, jb, :]), ["lgF"], ["mx8F"])
                    V(P, lambda e: e.tensor_scalar(out=ngm[:, :], in0=mx8[:, :, 0], scalar1=-1.0, scalar2=None,
                                                   op0=ALU.mult), ["mx8F"], ["ngmF"])
                    for jb in range(4):
                        A(P, lambda e, jb=jb: e.activation(out=ex[:, jb, :], in_=lg[:, jb, :], func=AF.Exp,
                                                           bias=ngm[:, jb:jb + 1]), ["lgF", "ngmF"], ["exF"])
                        V(P, lambda e, jb=jb: e.tensor_scalar(out=msk[:, jb, :], in0=lg[:, jb, :],
                                                              scalar1=mx8[:, jb, 1:2], scalar2=None, op0=ALU.is_ge),
                          ["lgF", "mx8F"], ["mskF"])
                    V(P, lambda e: e.tensor_tensor(out=ex[:, :, :], in0=ex[:, :, :], in1=msk[:, :, :], op=ALU.mult),
                      ["exF", "mskF"], ["exF"])
                    V(P, lambda e: e.tensor_reduce(out=den[:, :], in_=ex[:, :, :], axis=mybir.AxisListType.X,
                                                   op=ALU.add), ["exF"], ["denF"])
                    V(P, lambda e: e.reciprocal(out=den[:, :], in_=den[:, :]), ["denF"], ["denF"])
                    for jb in range(4):
                        V(P, lambda e, jb=jb: e.tensor_scalar(out=gate[:, jb, :], in0=ex[:, jb, :],
                                                              scalar1=den[:, jb:jb + 1], scalar2=None, op0=ALU.mult),
                          ["exF", "denF"], ["gateF"])
                    for jb in range(4):
                        PE(P, lambda e, jb=jb: e.transpose(pb[7][0:8, jb * 128:(jb + 1) * 128], gate[:, jb, :], ident),
                           ["gateF", "cst"], [PB(7)])
                    A(P, lambda e, hs=hs: e.copy(out=gT[:, hs], in_=pb[7][0:8, :]), [PB(7)], ["gTF"])
            units = [(e_i, fg) for e_i in range(len(experts)) for fg in range(nfg)]

            def load_unit(u):
                e_l, fg_l = units[u]
                Wg_, Wu_, Wd_ = experts[e_l]
                fs_ = slice(fg_l * 256, (fg_l + 1) * 256)
                load_cast(wg[u % 2], ("wgF", u % 2), Wg_[:, fs_], 8, 256, stg, "stgF", cast_eng="gpsimd", dma_q="sync")
                load_cast(wu[u % 2], ("wuF", u % 2), Wu_[:, fs_], 8, 256, stg, "stgF", cast_eng="gpsimd", dma_q="sync")
                load_cast(wd[u % 3], ("wdF", u % 3), Wd_[fs_, :], 2, 1024, stg, "stgF", cast_eng="gpsimd", dma_q="sync")

            load_unit(0)
            for u, (e_i, fg) in enumerate(units):
                if u + 1 < len(units):
                    load_unit(u + 1)
                if is_moe and fg == 0:
                    gb = gbc[e_i % 2]
                    gk = ("gbcF", e_i % 2)
                    for half in range(2):
                        hs = slice(half * 512, (half + 1) * 512)
                        PE(P, lambda e, hs=hs, e_i=e_i: e.matmul(
                            pb[7][:, :], cstt[0:8, C_SEL8 + e_i * 128:C_SEL8 + (e_i + 1) * 128], gT[:, hs],
                            start=True, stop=True), ["gTF", "cst"], [PB(7)])
                        A(P, lambda e, hs=hs, gb=gb: e.copy(out=gb[:, hs], in_=pb[7][:, :]), [PB(7)], [gk])
                if True:
                    i = u % 2
                    i3 = u % 3
                    for half in range(2):
                        hs = slice(half * 512, (half + 1) * 512)
                        ai = rr("aF", 3)
                        for j in range(2):
                            bg = (cnt % 2) * 2
                            cnt += 1
                            js = slice(j * 128, (j + 1) * 128)
                            mm_group(P, pb[bg][:, :], [(wg[i][:, k, js], h2[:, k, hs]) for k in range(8)],
                                     [("wgF", i), ("h2", half)], [PB(bg)])
                            mm_group(P, pb[bg + 1][:, :], [(wu[i][:, k, js], h2[:, k, hs]) for k in range(8)],
                                     [("wuF", i), ("h2", half)], [PB(bg + 1)])
                            si = rr("sgF", 2)
                            A(P, lambda e, si=si, bg=bg: e.activation(out=sg[si][:, :], in_=pb[bg][:, :], func=AF.Silu),
                              [PB(bg)], [("sgF", si)])
                            if not is_moe:
                                V(P, lambda e, si=si, bg=bg, ai=ai, j=j: e.tensor_tensor(
                                    out=a_[ai][:, j, :], in0=sg[si][:, :], in1=pb[bg + 1][:, :], op=ALU.mult),
                                  [("sgF", si), PB(bg + 1)], [("aF", ai, j)])
                            else:
                                ti = rr("ttF", 2)
                                V(P, lambda e, si=si, bg=bg, ti=ti: e.tensor_tensor(
                                    out=tt[ti][:, :], in0=sg[si][:, :], in1=pb[bg + 1][:, :], op=ALU.mult),
                                  [("sgF", si), PB(bg + 1)], [("ttF", ti)])
                                V(P, lambda e, ti=ti, ai=ai, j=j, hs=hs, gb=gb: e.tensor_tensor(
                                    out=a_[ai][:, j, :], in0=tt[ti][:, :], in1=gb[:, hs], op=ALU.mult),
                                  [("ttF", ti), gk], [("aF", ai, j)])
                            if pending:
                                pending[0](range(4 * j, 4 * j + 4))
                                if j == 1:
                                    pending.pop()
                        def down(ms, i=i3, ai=ai, hs=hs, half=half):
                            for m in ms:
                                bd = 4 + (m % 4)
                                mm_group(P, pb[bd][:, :], [(wd[i][:, j, m * 128:(m + 1) * 128], a_[ai][:, j, :])
                                                           for j in range(2)],
                                         [("wdF", i), ("aF", ai, 0), ("aF", ai, 1)], [PB(bd)])
                                V(P, lambda e, m=m, bd=bd, hs=hs: e.scalar_tensor_tensor(
                                    out=xacc[:, m, hs], in0=pb[bd][:, :], scalar=gt2[:, m:m + 1], in1=xacc[:, m, hs],
                                    op0=ALU.mult, op1=ALU.add), [PB(bd), ("xacc", m, half), "mod"], [("xacc", m, half)])
                        pending.append(down)
            if pending:
                pending.pop()(range(8))
            for k in range(8):
                for half in range(2):
                    P.dma("sync" if k % 2 == 0 else "gpsimd",
                          xS[k * 128:(k + 1) * 128, t0 + half * 512:t0 + (half + 1) * 512],
                          xacc[:, k, half * 512:(half + 1) * 512], reads=[("xacc", k, half)],
                          writes=[("xS", k, tb, half)])
        P.barrier()


F_KEYS = ("ffn_g", "ffn_u", "ffn_d", "moe_r", "moe_g", "moe_u", "moe_d")


def kernel(**I):
    shared = prep_shared(I)
    zshared = {k: np.zeros_like(v) for k, v in shared.items()}
    nc = build()
    active = [0, 1, 4, 5]
    in_maps = []
    for c in range(8):
        if c in active:
            in_maps.append(dict(shared, **prep_core(I, active.index(c))))
        else:
            z = prep_core(I, 0)
            in_maps.append(dict(zshared, **{k: np.zeros_like(v) for k, v in z.items()}))
    res = run_bass_kernel_spmd(nc, in_maps, core_ids=list(range(8)))
    out = np.stack([np.asarray(res.results[c]["outT"]).T for c in active])
    return np.ascontiguousarray(out, np.float32)
```
